# Optimizing a Trainium2 kernel written in Bass

```python
import jax, jax.numpy as jnp
from jax import lax
import numpy as np

D_MODEL = 1024
BATCH = 8
SEQ = 2048
DEPTH = 1
DEC_BATCH = 32
DEC_SEQ = 64
PAST_LEN = 4096

CHUNK = 64
HEAD_DIM = 64
SBA_HEADS = 8
SBA_W = SBA_HEADS * HEAD_DIM
CONV_W = 256
CONV_K = 31
MEM_HEADS = 4
MEM_W = MEM_HEADS * HEAD_DIM
N_MEM = 256
MIX_W = SBA_W + CONV_W + MEM_W
IN_W = 3 * SBA_W + 2 * CONV_W + MEM_W
QBLOCK = 128
N_GROUPS = 4
EXPERTS_PER_GROUP = 8
N_EXPERTS = N_GROUPS * EXPERTS_PER_GROUP
TOP_K = 2
D_EXPERT = 256
DEEPNORM_ALPHA = (2 * DEPTH) ** 0.25
DEEPNORM_BETA = (8 * DEPTH) ** -0.25
SBA_SCALE = HEAD_DIM ** -0.5
MEM_SCALE = HEAD_DIM ** -0.5
LN_EPS = 1e-5

kernel_name = 'stickbreak_conformer_memxattn_hmoe_stream_step'


def _ln(x, g, b):
    xf = x.astype(jnp.float32)
    mu = jnp.mean(xf, -1, keepdims=True)
    var = jnp.mean(jnp.square(xf - mu), -1, keepdims=True)
    return ((xf - mu) * lax.rsqrt(var + LN_EPS) * g.astype(jnp.float32) + b.astype(jnp.float32)).astype(x.dtype)


def _in_proj(h, w_in):
    B, T, _ = h.shape
    p = jnp.einsum('btd,de->bte', h, w_in)
    q, k, v, cv, cg, mq = jnp.split(p, [SBA_W, 2 * SBA_W, 3 * SBA_W, 3 * SBA_W + CONV_W, 3 * SBA_W + 2 * CONV_W], axis=-1)
    hd = (B, T, SBA_HEADS, HEAD_DIM)
    glu = cv * jax.nn.sigmoid(cg)
    return q.reshape(hd), k.reshape(hd), v.reshape(hd), glu, mq.reshape(B, T, MEM_HEADS, HEAD_DIM)


def _stick_breaking(q, q_pos, k, v, k_pos):
    z = jnp.einsum('bqhd,bshd->bhqs', q.astype(jnp.float32), k.astype(jnp.float32)) * SBA_SCALE
    causal = k_pos[None, :] < q_pos[:, None]
    log_keep = jnp.where(causal, jax.nn.log_sigmoid(-z), 0.0)
    suffix = lax.cumsum(log_keep, axis=3, reverse=True) - log_keep
    w = jnp.where(causal, jnp.exp(jax.nn.log_sigmoid(z) + suffix), 0.0)
    o = jnp.einsum('bhqs,bshd->bqhd', w, v.astype(jnp.float32))
    return o.astype(q.dtype)


def _sba_prompt(q, k, v):
    B, T, H, Dh = q.shape
    nb = T // QBLOCK
    k_pos = jnp.arange(T)
    qb = jnp.moveaxis(q.reshape(B, nb, QBLOCK, H, Dh), 1, 0)

    def block(args):
        qi, bi = args
        return _stick_breaking(qi, bi * QBLOCK + jnp.arange(QBLOCK), k, v, k_pos)

    o = lax.map(block, (qb, jnp.arange(nb)))
    return jnp.moveaxis(o, 0, 1).reshape(B, T, H, Dh)


def _conv_module(conv_in, w_dw, b_dw, lnc_g, lnc_b, w_cpw):
    dw = lax.conv_general_dilated(conv_in, w_dw[:, None, :].astype(conv_in.dtype), window_strides=(1,),
                                  padding='VALID', dimension_numbers=('NWC', 'WIO', 'NWC'),
                                  feature_group_count=CONV_W) + b_dw
    u = _ln(dw, lnc_g, lnc_b)
    return jnp.einsum('btc,ce->bte', jax.nn.silu(u), w_cpw)


def _mem_kv(mem, w):
    B, M, _ = mem.shape
    return jnp.einsum('bmd,de->bme', mem, w).reshape(B, M, MEM_HEADS, HEAD_DIM)


def _mem_attn(mq, mk, mv):
    s = jnp.einsum('bqhd,bmhd->bhqm', mq.astype(jnp.float32), mk.astype(jnp.float32)) * MEM_SCALE
    p = jax.nn.softmax(s, axis=-1)
    return jnp.einsum('bhqm,bmhd->bqhd', p, mv.astype(jnp.float32)).astype(mq.dtype)


def _hier_moe(h, w_rg, b_rg, w_re, b_re, w_eg, w_eu, w_ed):
    B, T, D = h.shape
    t = h.reshape(B * T, D)
    g_logits = jnp.einsum('nd,dg->ng', t, w_rg).astype(jnp.float32) + b_rg.astype(jnp.float32)
    g_prob = jax.nn.softmax(g_logits, axis=-1)
    g_idx = jnp.argmax(g_logits, axis=-1)
    g_w = jnp.max(g_prob, axis=-1, keepdims=True)
    e_logits = (jnp.einsum('nd,de->ne', t, w_re).astype(jnp.float32) + b_re.astype(jnp.float32))
    e_logits = e_logits.reshape(-1, N_GROUPS, EXPERTS_PER_GROUP)
    e_sel = jnp.einsum('nge,ng->ne', e_logits, jax.nn.one_hot(g_idx, N_GROUPS, dtype=jnp.float32))
    top_v, top_i = lax.top_k(e_sel, TOP_K)
    top_w = jax.nn.softmax(top_v, axis=-1) * g_w
    expert_id = g_idx[:, None] * EXPERTS_PER_GROUP + top_i
    gate = jnp.einsum('nke,nk->ne', jax.nn.one_hot(expert_id, N_EXPERTS, dtype=jnp.float32), top_w)
    a = jnp.einsum('nd,edf->nef', t, w_eg)
    u = jnp.einsum('nd,edf->nef', t, w_eu)
    hid = jax.nn.silu(a) * u * gate.astype(t.dtype)[:, :, None]
    y = jnp.einsum('nef,efd->nd', hid, w_ed)
    return y.reshape(B, T, D)


def _post(x, sba, conv, mem, w_out, ln1_g, ln1_b, w_rg, b_rg, w_re, b_re, w_eg, w_eu, w_ed, ln2_g, ln2_b):
    B, T, _ = x.shape
    mix = jnp.concatenate([sba.reshape(B, T, SBA_W), conv, mem.reshape(B, T, MEM_W)], axis=-1)
    x = _ln(DEEPNORM_ALPHA * x + jnp.einsum('bte,ed->btd', mix, w_out), ln1_g, ln1_b)
    f = _hier_moe(x, w_rg, b_rg, w_re, b_re, w_eg, w_eu, w_ed)
    return _ln(DEEPNORM_ALPHA * x + f, ln2_g, ln2_b)


def setup_inputs(seed: int = 0) -> dict:
    key = jax.random.key(seed)
    ks = jax.random.split(key, 40)
    L = DEPTH

    def nrm(k, shape, scale=1.0):
        return jax.random.normal(k, shape, jnp.float32) * scale

    return {
        'x_prompt': nrm(ks[0], (BATCH, SEQ, D_MODEL)),
        'x_sample': nrm(ks[1], (DEC_BATCH, DEC_SEQ, D_MODEL)),
        'mem_prompt': nrm(ks[2], (BATCH, N_MEM, D_MODEL)),
        'cache_sba_k': nrm(ks[3], (L, DEC_BATCH, PAST_LEN, SBA_HEADS, HEAD_DIM)),
        'cache_sba_v': nrm(ks[4], (L, DEC_BATCH, PAST_LEN, SBA_HEADS, HEAD_DIM)),
        'cache_conv': nrm(ks[5], (L, DEC_BATCH, CONV_K - 1, CONV_W), 0.5),
        'cache_mem_k': nrm(ks[6], (L, DEC_BATCH, N_MEM, MEM_HEADS, HEAD_DIM)),
        'cache_mem_v': nrm(ks[7], (L, DEC_BATCH, N_MEM, MEM_HEADS, HEAD_DIM)),
        'ln0_g': 1.0 + nrm(ks[8], (D_MODEL,), 0.02),
        'ln0_b': nrm(ks[9], (D_MODEL,), 0.02),
        'w_in': nrm(ks[10], (L, D_MODEL, IN_W), D_MODEL ** -0.5),
        'w_dw': nrm(ks[11], (L, CONV_K, CONV_W), CONV_K ** -0.5),
        'b_dw': nrm(ks[12], (L, CONV_W), 0.02),
        'lnc_g': 1.0 + nrm(ks[13], (L, CONV_W), 0.02),
        'lnc_b': nrm(ks[14], (L, CONV_W), 0.02),
        'w_cpw': nrm(ks[15], (L, CONV_W, CONV_W), CONV_W ** -0.5),
        'w_mk': nrm(ks[16], (L, D_MODEL, MEM_W), D_MODEL ** -0.5),
        'w_mv': nrm(ks[17], (L, D_MODEL, MEM_W), D_MODEL ** -0.5),
        'w_out': nrm(ks[18], (L, MIX_W, D_MODEL), MIX_W ** -0.5 * DEEPNORM_BETA),
        'ln1_g': 1.0 + nrm(ks[19], (L, D_MODEL), 0.02),
        'ln1_b': nrm(ks[20], (L, D_MODEL), 0.02),
        'w_rg': nrm(ks[21], (L, D_MODEL, N_GROUPS), D_MODEL ** -0.5),
        'b_rg': nrm(ks[22], (L, N_GROUPS), 0.01),
        'w_re': nrm(ks[23], (L, D_MODEL, N_EXPERTS), D_MODEL ** -0.5),
        'b_re': nrm(ks[24], (L, N_EXPERTS), 0.01),
        'w_eg': nrm(ks[25], (L, N_EXPERTS, D_MODEL, D_EXPERT), D_MODEL ** -0.5),
        'w_eu': nrm(ks[26], (L, N_EXPERTS, D_MODEL, D_EXPERT), D_MODEL ** -0.5),
        'w_ed': nrm(ks[27], (L, N_EXPERTS, D_EXPERT, D_MODEL), D_EXPERT ** -0.5 * DEEPNORM_BETA),
        'ln2_g': 1.0 + nrm(ks[28], (L, D_MODEL), 0.02),
        'ln2_b': nrm(ks[29], (L, D_MODEL), 0.02),
    }


def reference(x_prompt, x_sample, mem_prompt, cache_sba_k, cache_sba_v, cache_conv, cache_mem_k, cache_mem_v,
              ln0_g, ln0_b, w_in, w_dw, b_dw, lnc_g, lnc_b, w_cpw, w_mk, w_mv, w_out, ln1_g, ln1_b,
              w_rg, b_rg, w_re, b_re, w_eg, w_eu, w_ed, ln2_g, ln2_b):
    xp = _ln(x_prompt, ln0_g, ln0_b)
    xs = _ln(x_sample, ln0_g, ln0_b)
    past = cache_sba_k.shape[2]
    tn = x_sample.shape[1]
    q_pos_s = past + jnp.arange(tn)
    k_pos_s = jnp.arange(past + tn)
    kp_l, vp_l, cp_l, mkp_l, mvp_l, ks_l, vs_l, cs_l = [], [], [], [], [], [], [], []
    for l in range(DEPTH):
        moe = (w_out[l], ln1_g[l], ln1_b[l], w_rg[l], b_rg[l], w_re[l], b_re[l], w_eg[l], w_eu[l], w_ed[l], ln2_g[l], ln2_b[l])
        conv_w = (w_dw[l], b_dw[l], lnc_g[l], lnc_b[l], w_cpw[l])
        q, k, v, glu, mq = _in_proj(xp, w_in[l])
        sba = _sba_prompt(q, k, v)
        conv_in = jnp.pad(glu, ((0, 0), (CONV_K - 1, 0), (0, 0)))
        conv = _conv_module(conv_in, *conv_w)
        mk = _mem_kv(mem_prompt, w_mk[l])
        mv = _mem_kv(mem_prompt, w_mv[l])
        mem = _mem_attn(mq, mk, mv)
        xp = _post(xp, sba, conv, mem, *moe)
        kp_l.append(k); vp_l.append(v); cp_l.append(conv_in[:, -(CONV_K - 1):]); mkp_l.append(mk); mvp_l.append(mv)
        q, k, v, glu, mq = _in_proj(xs, w_in[l])
        k_all = jnp.concatenate([cache_sba_k[l], k], axis=1)
        v_all = jnp.concatenate([cache_sba_v[l], v], axis=1)
        sba = _stick_breaking(q, q_pos_s, k_all, v_all, k_pos_s)
        conv_in = jnp.concatenate([cache_conv[l], glu], axis=1)
        conv = _conv_module(conv_in, *conv_w)
        mem = _mem_attn(mq, cache_mem_k[l], cache_mem_v[l])
        xs = _post(xs, sba, conv, mem, *moe)
        ks_l.append(k); vs_l.append(v); cs_l.append(conv_in[:, -(CONV_K - 1):])
    return (xp, xs, jnp.stack(kp_l), jnp.stack(vp_l), jnp.stack(cp_l), jnp.stack(mkp_l), jnp.stack(mvp_l),
            jnp.stack(ks_l), jnp.stack(vs_l), jnp.stack(cs_l))
```

```python
import numpy as np
from contextlib import ExitStack
import concourse.bass as bass
import concourse.mybir as mybir
from concourse.bass_utils import run_bass_kernel_spmd

F32 = mybir.dt.float32
BF16 = mybir.dt.bfloat16
I32 = mybir.dt.int32
AF = mybir.ActivationFunctionType
ALU = mybir.AluOpType

ENGS = ["pe", "act", "dve", "pool", "sp"]
NCORES = 8
ALPHA = 2.0 ** 0.25
EPS = 1e-5
SCALE = 0.125
NT = 2304
NP = 2048
BIG = 1.0e30


class Prog:
    def __init__(self, nc):
        self.nc = nc
        self.ins = {e: [] for e in ENGS}
        self.last_w = {}
        self.readers = {}
        self.dma_cnt = {}

    def _tok_deps(self, reads, writes):
        deps = []
        for r in reads:
            t = self.last_w.get(r)
            if t is not None:
                deps.append(t)
        for w in writes:
            t = self.last_w.get(w)
            if t is not None:
                deps.append(t)
            deps.extend(self.readers.get(w, ()))
        return deps

    def _add_waits(self, eng, deps):
        waits = []
        for t in deps:
            if t[0] == "dma":
                waits.append((("dma", t[1]), t[2] * 16))
            else:
                _, e2, idx = t
                if e2 == eng and eng == "pe":
                    continue
                self.ins[e2][idx]["flag"] = True
                waits.append((("eng", e2), idx))
        return waits

    def op(self, eng, fn, reads=(), writes=()):
        deps = self._tok_deps(reads, writes)
        waits = self._add_waits(eng, deps)
        idx = len(self.ins[eng])
        self.ins[eng].append(dict(fn=fn, waits=waits, flag=False, dma=None))
        tok = ("eng", eng, idx)
        for r in reads:
            lst = self.readers.setdefault(r, [])
            lst[:] = [t for t in lst if not (t[0] == "eng" and t[1] == eng)]
            lst.append(tok)
        for w in writes:
            self.last_w[w] = tok
            self.readers[w] = []
        return tok

    def dma(self, eng, fn, semkey, reads=(), writes=()):
        deps = self._tok_deps(reads, writes)
        waits = self._add_waits(eng, deps)
        cnt = self.dma_cnt.get(semkey, 0) + 1
        self.dma_cnt[semkey] = cnt
        self.ins[eng].append(dict(fn=fn, waits=waits, flag=False, dma=semkey))
        tok = ("dma", semkey, cnt)
        for r in reads:
            self.readers.setdefault(r, []).append(tok)
        for w in writes:
            self.last_w[w] = tok
            self.readers[w] = []
        return tok

    def barrier(self):
        lasts = {}
        for e in ENGS:
            for idx in range(len(self.ins[e]) - 1, -1, -1):
                ins = self.ins[e][idx]
                if ins["fn"] is not None and ins["dma"] is None:
                    lasts[e] = idx
                    break
        dmas = dict(self.dma_cnt)
        for e in ENGS:
            waits = []
            for e2, idx in lasts.items():
                if e2 != e:
                    self.ins[e2][idx]["flag"] = True
                    waits.append((("eng", e2), idx))
            for k, c in dmas.items():
                waits.append((("dma", k), c * 16))
            self.ins[e].append(dict(fn=None, waits=waits, flag=False, dma=None))

    def emit(self):
        nc = self.nc
        with ExitStack() as st:
            sems = {}
            for e in ENGS:
                sems[("eng", e)] = st.enter_context(nc.semaphore("s_" + e))
            for i, k in enumerate(self.dma_cnt):
                sems[("dma", k)] = st.enter_context(nc.semaphore("d%d" % i))
            cnts = {}
            for e in ENGS:
                c = 0
                arr = []
                for ins in self.ins[e]:
                    if ins["flag"]:
                        c += 1
                    arr.append(c)
                cnts[e] = arr
            block = st.enter_context(nc.Block())
            final_dma = [(sems[("dma", k)], c * 16) for k, c in self.dma_cnt.items()]

            def run(e, engobj):
                waited = {}
                for ins in self.ins[e]:
                    for key, v in ins["waits"]:
                        if key[0] == "eng":
                            v = cnts[key[1]][v]
                        if waited.get(key, 0) >= v:
                            continue
                        waited[key] = v
                        engobj.wait_ge(sems[key], v)
                    if ins["fn"] is None:
                        continue
                    r = ins["fn"](engobj)
                    if ins["dma"] is not None:
                        r.then_inc(sems[("dma", ins["dma"])], 16)
                    elif ins["flag"]:
                        r.then_inc(sems[("eng", e)], 1)
                if e == "sp":
                    for s, v in final_dma:
                        engobj.wait_ge(s, v)

            @block.tensor
            def _(eng):
                run("pe", eng)

            @block.scalar
            def _(eng):
                run("act", eng)

            @block.vector
            def _(eng):
                run("dve", eng)

            @block.gpsimd
            def _(eng):
                run("pool", eng)

            @block.sync
            def _(eng):
                run("sp", eng)


def build(nc, stop_after=99):
    P = Prog(nc)

    def din(name, shape):
        return nc.dram_tensor(name, list(shape), F32, kind="ExternalInput").ap()

    def dout(name, shape):
        return nc.dram_tensor(name, list(shape), F32, kind="ExternalOutput").ap()

    xp = din("xp", [NP, 1024]); xs = din("xs", [256, 1024]); memp = din("memp", [256, 1024])
    ck = din("ck", [4, 4096, 512]); cv = din("cv", [4, 4096, 512])
    cconv = din("cconv", [4, 30, 256]); cmk = din("cmk", [4, 256, 256]); cmv = din("cmv", [4, 256, 256])
    ln0_g = din("ln0_g", [1024]); ln0_b = din("ln0_b", [1024])
    w_in = din("w_in", [1024, 2304]); w_dw = din("w_dw", [31, 256]); b_dw = din("b_dw", [256])
    lnc_g = din("lnc_g", [256]); lnc_b = din("lnc_b", [256]); w_cpw = din("w_cpw", [256, 256])
    w_mk = din("w_mk", [1024, 256]); w_mv = din("w_mv", [1024, 256]); w_out = din("w_out", [1024, 1024])
    ln1_g = din("ln1_g", [1024]); ln1_b = din("ln1_b", [1024])
    w_rg = din("w_rg", [1024, 4]); b_rg = din("b_rg", [4]); w_re = din("w_re", [1024, 32]); b_re = din("b_re", [32])
    w_eg = din("w_eg", [32, 1024, 256]); w_eu = din("w_eu", [32, 1024, 256]); w_ed = din("w_ed", [32, 256, 1024])
    ln2_g = din("ln2_g", [1024]); ln2_b = din("ln2_b", [1024])

    yp = dout("yp", [NP, 1024]); ys = dout("ys", [256, 1024])
    kp = dout("kp", [NP, 512]); vp = dout("vp", [NP, 512]); convp = dout("convp", [30, 256])
    mkp = dout("mkp", [256, 256]); mvp = dout("mvp", [256, 256])
    ksn = dout("ksn", [256, 512]); vsn = dout("vsn", [256, 512]); convs = dout("convs", [4, 30, 256])
    XS = nc.dram_tensor("XS_scr", [50 * 256, 1028], BF16, kind="Internal").ap()
    YK = nc.dram_tensor("YK_scr", [2 * NT, 1024], F32, kind="Internal").ap()
    XNS = nc.dram_tensor("XN_scr", [NT, 1024], F32, kind="Internal").ap()
    WGB = nc.dram_tensor("WGB_scr", [4096, 2048], BF16, kind="Internal").ap()
    WUB = nc.dram_tensor("WUB_scr", [4096, 2048], BF16, kind="Internal").ap()
    WDB = nc.dram_tensor("WDB_scr", [4096, 2048], BF16, kind="Internal").ap()

    class Arena:
        def __init__(self, base, size):
            self.base, self.size, self.off = base, size, 0

        def t(self, name, shape, dtype):
            el = 2 if dtype == BF16 else 4
            n = 1
            for s in shape[1:]:
                n *= s
            sz = (n * el + 31) // 32 * 32
            assert self.off + sz <= self.size, (name, self.off, sz, self.size)
            h = nc.alloc_sbuf_tensor_at(name, list(shape), dtype, offset=self.base + self.off)
            self.off += sz
            return h.ap()

        def reset(self):
            self.off = 0

    BASE = 16640
    aXT = Arena(BASE, 36864)
    aMIX = Arena(BASE + 36864, 36864)
    CSZ = 11776
    aCONST = Arena(BASE + 73728, CSZ)
    aR = Arena(BASE + 73728 + CSZ, 73728)
    aFREE = Arena(BASE + 73728 + CSZ + 73728, 228640 - (BASE + 73728 + CSZ + 73728))

    XT = aXT.t("XT", [128, 8, NT], BF16)
    MIXT = aMIX.t("MIXT", [128, 8, NT], BF16)
    R = aR.t("R", [128, 18, 1024], F32)
    GATE = aCONST.t("GATE", [128, 18, 32], F32)
    ident = aCONST.t("ident", [128, 128], BF16)
    identF = aCONST.t("identF", [128, 128], F32)
    Uinc = aCONST.t("Uinc", [128, 128], BF16)
    ones = aCONST.t("ones", [128, 128], BF16)
    onesF = aCONST.t("onesF", [128, 128], F32)
    tri = aCONST.t("tri", [128, 128], F32)
    tri8 = aCONST.t("tri8", [128, 8, 64], F32)
    wdw = aCONST.t("wdw", [128, 2, 31], F32)
    cpar = aCONST.t("cpar", [128, 3, 2], F32)
    W_CPW = aCONST.t("W_CPW", [128, 2, 256], BF16)
    W_R = aCONST.t("W_R", [128, 8, 36], BF16)
    RB = aCONST.t("RB", [128, 36], F32)
    small = aCONST.t("small", [128, 64], F32)
    small2 = aCONST.t("small2", [128, 64], F32)
    IDXW = aCONST.t("IDXW", [128, 50], I32)

    ps = [nc.alloc_psum_tensor("ps%d" % i, [128, 512], F32) for i in range(8)]
    psf = [p.ap() for p in ps]
    psb = [p.bitcast(BF16).ap() for p in ps]

    def PS(i):
        return ("ps", i)

    def pool(fn, reads=(), writes=()):
        return P.op("pool", fn, reads, writes)

    pool(lambda e: e.memset(ident, 0.0), writes=["ident"])
    pool(lambda e: e.affine_select(out=ident, in_=ident, pattern=[[-1, 128]], compare_op=ALU.not_equal,
                                   fill=1.0, base=0, channel_multiplier=1), reads=["ident"], writes=["ident"])
    pool(lambda e: e.memset(identF, 0.0), writes=["identF"])
    pool(lambda e: e.affine_select(out=identF, in_=identF, pattern=[[-1, 128]], compare_op=ALU.not_equal,
                                   fill=1.0, base=0, channel_multiplier=1), reads=["identF"], writes=["identF"])
    aR.reset()
    W_IN = aR.t("W_IN", [128, 8, 2304], BF16)
    FILLR = aR.t("FILLR", [128, 1028], BF16)
    for c in range(8):
        P.dma("pool", lambda e, c=c: e.dma_start(out=W_IN[:, c, :], in_=w_in[c * 128:(c + 1) * 128, :]), "W_IN", writes=["W_IN"])
    WIN_ALL = [("W_IN", c) for c in range(8)]

    pool(lambda e: e.memset(ones, 1.0), writes=["ones"])
    pool(lambda e: e.memset(onesF, 1.0), writes=["onesF"])
    pool(lambda e: e.memset(Uinc, 1.0), writes=["Uinc"])
    pool(lambda e: e.affine_select(out=Uinc, in_=Uinc, pattern=[[-1, 128]], compare_op=ALU.is_ge,
                                   fill=0.0, base=0, channel_multiplier=1), reads=["Uinc"], writes=["Uinc"])
    pool(lambda e: e.memset(tri, 1.0), writes=["tri"])
    pool(lambda e: e.affine_select(out=tri, in_=tri, pattern=[[1, 128]], compare_op=ALU.is_gt,
                                   fill=0.0, base=0, channel_multiplier=-1), reads=["tri"], writes=["tri"])
    pool(lambda e: e.memset(tri8, 1.0), writes=["tri8"])
    pool(lambda e: e.affine_select(out=tri8[0:64], in_=tri8[0:64], pattern=[[0, 8], [1, 64]], compare_op=ALU.is_gt,
                                   fill=0.0, base=0, channel_multiplier=-1), reads=["tri8"], writes=["tri8"])
    P.dma("pool", lambda e: e.dma_start(out=tri8[64:128], in_=tri8[0:64]), "tri8", reads=["tri8"], writes=["tri8"])

    def xrows(t):
        return xp[t * 128:(t + 1) * 128, :] if t < 16 else xs[(t - 16) * 128:(t - 15) * 128, :]

    def tcols(t):
        return slice(t * 128, (t + 1) * 128)

    ln_ctr = [0]

    def layernorm(tag, src, src_res, dst, dst_res, G, B, gres, out_scale=None):
        k = ln_ctr[0] % 2
        ln_ctr[0] += 1
        sm = small if k == 0 else small2
        smr = ("small", k)
        st6 = sm[:, 0:12].rearrange("p (a b) -> p a b", b=6)
        mv = sm[:, 12:14]
        rs = sm[:, 14:15]
        P.op("dve", lambda e: e.bn_stats(out=st6[:, 0, :], in_=src[:, 0:512]), reads=[src_res], writes=[smr])
        P.op("dve", lambda e: e.bn_stats(out=st6[:, 1, :], in_=src[:, 512:1024]), reads=[src_res], writes=[smr])
        P.op("dve", lambda e: e.bn_aggr(out=mv, in_=st6), reads=[smr], writes=[smr])
        P.op("dve", lambda e: e.tensor_scalar(out=rs, in0=mv[:, 1:2], scalar1=EPS, scalar2=None, op0=ALU.add),
             reads=[smr], writes=[smr])
        P.op("act", lambda e: e.activation(out=rs, in_=rs, func=AF.Ln), reads=[smr], writes=[smr])
        P.op("act", lambda e: e.activation(out=rs, in_=rs, func=AF.Exp, scale=-0.5), reads=[smr], writes=[smr])
        P.op("dve", lambda e: e.scalar_tensor_tensor(out=src, in0=src, scalar=mv[:, 0:1], in1=G, op0=ALU.subtract, op1=ALU.mult),
             reads=[smr, src_res] + gres, writes=[src_res])
        P.op("dve", lambda e: e.scalar_tensor_tensor(out=dst, in0=src, scalar=rs, in1=B, op0=ALU.mult, op1=ALU.add),
             reads=[smr, src_res] + gres, writes=[dst_res])

    aFREE.reset()
    aMIX.reset()
    XIN = [aMIX.t("XIN%d" % i, [128, 1024], F32) for i in range(3)]
    XNB = [aMIX.t("XNB%d" % i, [128, 1024], BF16) for i in range(2)]
    LNG = aMIX.t("LNG", [128, 1024], F32)
    LNB = aMIX.t("LNB", [128, 1024], F32)
    P.dma("sp", lambda e: e.dma_start(out=LNG, in_=ln0_g.partition_broadcast(128)), "LNG", writes=["LNG"])
    P.dma("sp", lambda e: e.dma_start(out=LNB, in_=ln0_b.partition_broadcast(128)), "LNB", writes=["LNB"])
    def transpose_to_XT(t, srcb, src_res, bank):
        for c in range(8):
            P.op("pe", lambda e, c=c: e.transpose(psb[bank][:, c * 128:(c + 1) * 128], srcb[:, c * 128:(c + 1) * 128], ident),
                 reads=[src_res, "ident"], writes=[PS(bank)])
        P.op("act", lambda e: e.copy(out=XT[:, :, tcols(t)], in_=psb[bank].rearrange("p (c n) -> p c n", n=128)),
             reads=[PS(bank)], writes=[("XT", t)])

    KST = [aMIX.t("KST%d" % i, [128, 512], F32) for i in range(2)]
    VST = [aMIX.t("VST%d" % i, [128, 512], F32) for i in range(2)]
    aFREE.reset()
    V_ALL = aFREE.t("V_ALL", [128, 18, 512], BF16)
    aA_base = aFREE.off

    def p1_load(t):
        s3 = t % 3
        P.dma("sp", lambda e: e.dma_start(out=XIN[s3], in_=xrows(t)), ("XIN", s3), writes=[("XIN", s3)])

    def p2a_mm(t):
        s = t % 2
        for (which, col0, bank) in (("k", 512, 0 + s), ("v", 1024, 2 + s)):
            for c in range(8):
                P.op("pe", lambda e, c=c, col0=col0, bank=bank: e.matmul(
                    psf[bank], lhsT=XT[:, c, tcols(t)], rhs=W_IN[:, c, col0:col0 + 512], start=(c == 0), stop=(c == 7)),
                    reads=[("XT", t), "W_IN"], writes=[PS(bank)])

    def p2a_evac(t):
        s = t % 2
        P.op("act", lambda e: e.copy(out=KST[s], in_=psf[0 + s]), reads=[PS(0 + s)], writes=[("KST", s)])
        P.op("act", lambda e: e.copy(out=VST[s], in_=psf[2 + s]), reads=[PS(2 + s)], writes=[("VST", s)])
        P.op("act", lambda e: e.copy(out=V_ALL[:, t, :], in_=psf[2 + s]), reads=[PS(2 + s)], writes=[("V_ALL", t)])
        kdst = kp[t * 128:(t + 1) * 128, :] if t < 16 else ksn[(t - 16) * 128:(t - 15) * 128, :]
        vdst = vp[t * 128:(t + 1) * 128, :] if t < 16 else vsn[(t - 16) * 128:(t - 15) * 128, :]
        P.dma("pool", lambda e: e.dma_start(out=kdst, in_=KST[s]), ("st", "KST", s), reads=[("KST", s)])
        P.dma("pool", lambda e: e.dma_start(out=vdst, in_=VST[s]), ("st", "VST", s), reads=[("VST", s)])

    p1_load(0)
    p1_load(1)
    WDWT = aCONST.t("WDWT", [31, 256], F32)
    P.dma("sp", lambda e: e.dma_start(out=WDWT, in_=w_dw), "WDWT", writes=["WDWT"])
    for cc in range(2):
        P.op("pe", lambda e, cc=cc: e.transpose(psf[0][:, cc * 32:cc * 32 + 31], WDWT[:, cc * 128:(cc + 1) * 128], identF[0:31, 0:31]),
             reads=["WDWT", "identF"], writes=[PS(0)])
    P.op("act", lambda e: e.copy(out=wdw, in_=psf[0][:, 0:64].rearrange("p (c n) -> p c n", n=32)[:, :, 0:31]),
         reads=[PS(0)], writes=["wdw"])
    for i, v in enumerate([b_dw, lnc_g, lnc_b]):
        P.dma("sp", lambda e, i=i, v=v: e.dma_start(out=cpar[:, i, :], in_=v.rearrange("(c p) -> p c", p=128),
                                                     allow_slow_non_contiguous=True), "cpar", writes=["cpar"])
    P.dma("pool", lambda e: e.dma_start(out=W_CPW, in_=w_cpw.rearrange("(c p) f -> p c f", p=128)), "W_CPW", writes=["W_CPW"])
    P.dma("pool", lambda e: e.dma_start(out=W_R[:, :, 0:4], in_=w_rg.rearrange("(c p) f -> p c f", p=128)), "W_R", writes=["W_R"])
    P.dma("pool", lambda e: e.dma_start(out=W_R[:, :, 4:36], in_=w_re.rearrange("(c p) f -> p c f", p=128)), "W_R", writes=["W_R"])
    P.dma("sp", lambda e: e.dma_start(out=RB[:, 0:4], in_=b_rg.partition_broadcast(128)), "RB", writes=["RB"])
    P.dma("sp", lambda e: e.dma_start(out=RB[:, 4:36], in_=b_re.partition_broadcast(128)), "RB", writes=["RB"])

    for t in range(20):
        if t < 18:
            s = t % 2
            s3 = t % 3
            layernorm("ln0", XIN[s3], ("XIN", s3), XIN[s3], ("XIN", s3), LNG, LNB, ["LNG", "LNB"])
        if t >= 2:
            p2a_evac(t - 2)
        if t < 18:
            P.op("act", lambda e, s=s, s3=s3: e.copy(out=XNB[s], in_=XIN[s3]), reads=[("XIN", s3)], writes=[("XNB", s)])
            P.dma("pool", lambda e, t=t, s3=s3: e.dma_start(out=XNS[t * 128:(t + 1) * 128, :], in_=XIN[s3]), ("st", "XIN", s3), reads=[("XIN", s3)], writes=[("XNS", t)])
            transpose_to_XT(t, XNB[s], ("XNB", s), 6 + s)
            if t + 2 < 18:
                p1_load(t + 2)
        if 1 <= t < 19:
            p2a_mm(t - 1)
    P.op("pool", lambda e: e.memset(FILLR, 0.0), writes=["FILLR"])
    P.op("pool", lambda e: e.memset(FILLR[:, 1024:1026].bitcast(I32), 1 << 24), reads=["FILLR"], writes=["FILLR"])
    for a_ in range(10):
        P.dma("sp", lambda e, a_=a_: e.dma_start(out=XS[a_ * 1280:(a_ + 1) * 1280, :].rearrange("(a p) f -> p a f", p=128),
                                                 in_=FILLR.unsqueeze(1).to_broadcast([128, 10, 1028])), "XSfill", reads=["FILLR"], writes=["XSfill"])
    XT_ALL = [("XT", t) for t in range(18)]
    if stop_after <= 2:
        P.emit(); return nc

    QT = aR.t("QT", [128, 4, NP], BF16)
    KT = aR.t("KT", [128, 4, NP], BF16)
    QS = aFREE.t("QS", [128, 4, 256], BF16)
    KS = aFREE.t("KS", [128, 4, 256], BF16)
    aA_base = aFREE.off
    GW = 2078 + 4 * 94
    GLU = aFREE.t("GLU", [128, 2, GW], F32)
    MQT = aFREE.t("MQT", [128, 2, NT], BF16)
    SIG = [aMIX.t("SIG%d" % i, [128, 512], F32) for i in range(2)]
    blocks = [(0, 512), (512, 1024), (1024, 1536), (1536, 2048), (2048, 2304)]
    bankrr = [0]

    def inproj_fm(j, n0, n1, bank):
        N = n1 - n0
        for c in range(8):
            P.op("pe", lambda e, c=c: e.matmul(psf[bank][:, 0:N], lhsT=W_IN[:, c, j * 128:(j + 1) * 128], rhs=XT[:, c, n0:n1],
                                               start=(c == 0), stop=(c == 7)),
                 reads=["W_IN"] + [("XT", t) for t in range(n0 // 128, n1 // 128)], writes=[PS(bank)])

    def sample_glu_view(cc):
        return GLU[:, cc, 2078:2078 + 376].rearrange("p (s w) -> p s w", w=94)[:, :, 30:94]

    for bi, (n0, n1) in enumerate(blocks):
        N = n1 - n0
        for j in list(range(8)) + [16, 17]:
            bank = bankrr[0] % 4
            bankrr[0] += 1
            inproj_fm(j, n0, n1, bank)
            if j < 4:
                dst, dres = (QT[:, j, n0:n1], ("QT", j, bi)) if bi < 4 else (QS[:, j, :], ("QS", j))
            elif j < 8:
                dst, dres = (KT[:, j - 4, n0:n1], ("KT", j - 4, bi)) if bi < 4 else (KS[:, j - 4, :], ("KS", j - 4))
            else:
                dst, dres = MQT[:, j - 16, n0:n1], ("MQT", j - 16, bi)
            if (bankrr[0] % 2) == 0:
                P.op("act", lambda e, dst=dst, bank=bank, N=N: e.copy(out=dst, in_=psf[bank][:, 0:N]), reads=[PS(bank)], writes=[dres])
            else:
                P.op("dve", lambda e, dst=dst, bank=bank, N=N: e.tensor_copy(out=dst, in_=psf[bank][:, 0:N]), reads=[PS(bank)], writes=[dres])
        for cc in range(2):
            b1, b2 = 4 + 2 * (cc % 2), 5 + 2 * (cc % 2)
            inproj_fm(12 + cc, n0, n1, b1)
            inproj_fm(14 + cc, n0, n1, b2)
            sg = SIG[cc]
            P.op("act", lambda e, sg=sg, b2=b2, N=N: e.activation(out=sg[:, 0:N], in_=psf[b2][:, 0:N], func=AF.Sigmoid),
                 reads=[PS(b2)], writes=[("SIG", cc)])
            if bi < 4:
                P.op("dve", lambda e, sg=sg, b1=b1, cc=cc, n0=n0, N=N: e.tensor_tensor(
                    out=GLU[:, cc, 30 + n0:30 + n0 + N], in0=psf[b1][:, 0:N], in1=sg[:, 0:N], op=ALU.mult),
                    reads=[PS(b1), ("SIG", cc)], writes=[("GLU", cc)])
            else:
                P.op("dve", lambda e, sg=sg, b1=b1, cc=cc: e.tensor_tensor(
                    out=sample_glu_view(cc), in0=psf[b1][:, 0:256].rearrange("p (s w) -> p s w", w=64),
                    in1=sg[:, 0:256].rearrange("p (s w) -> p s w", w=64), op=ALU.mult),
                    reads=[PS(b1), ("SIG", cc)], writes=[("GLU", cc)])
    if stop_after <= 3:
        P.emit(); return nc

    P.barrier()
    aMIX.reset()
    DIAG = aMIX.t("DIAG", [128, 62, 128], BF16)
    assert aMIX.off <= 18432, aMIX.off
    for cc in range(2):
        for j in range(31):
            P.op("dve", lambda e, cc=cc, j=j: e.tensor_scalar(out=DIAG[:, cc * 31 + j, :], in0=identF, scalar1=wdw[:, cc, j:j + 1],
                                                              scalar2=None, op0=ALU.mult), reads=["identF", "wdw"], writes=["DIAG"])
    aW = Arena(aR.base, 36864)
    MEMB = aW.t("MEMB", [128, 2, 1024], BF16)
    MEMT = aW.t("MEMT", [128, 8, 256], BF16)
    W_MK = aW.t("W_MK", [128, 8, 256], BF16)
    W_MV = aW.t("W_MV", [128, 8, 256], BF16)
    MKT = aW.t("MKT", [128, 2, 256], BF16)
    MV = aW.t("MV", [128, 2, 256], BF16)
    MST = [aW.t("MST%d" % i, [128, 256], F32) for i in range(2)]
    PT = [aW.t("PT%d" % i, [128, 512], BF16) for i in range(2)]
    RDEN = aW.t("RDEN", [128, 512], F32)
    P.dma("pool", lambda e: e.dma_start(out=MEMB, in_=memp.rearrange("(m p) d -> p m d", p=128)), "MEMB", writes=["MEMB"])
    P.dma("pool", lambda e: e.dma_start(out=W_MK, in_=w_mk.rearrange("(c p) f -> p c f", p=128)), "W_MK", writes=["W_MK"])
    P.dma("pool", lambda e: e.dma_start(out=W_MV, in_=w_mv.rearrange("(c p) f -> p c f", p=128)), "W_MV", writes=["W_MV"])
    for mt in range(2):
        for c in range(8):
            P.op("pe", lambda e, mt=mt, c=c: e.transpose(psb[6][:, c * 128:(c + 1) * 128], MEMB[:, mt, c * 128:(c + 1) * 128], ident),
                 reads=["MEMB", "ident"], writes=[PS(6)])
        P.op("act", lambda e, mt=mt: e.copy(out=MEMT[:, :, mt * 128:(mt + 1) * 128], in_=psb[6].rearrange("p (c n) -> p c n", n=128)),
             reads=[PS(6)], writes=["MEMT"])
    for wi, (W, wres, dstout) in enumerate(((W_MK, "W_MK", mkp), (W_MV, "W_MV", mvp))):
        for mt in range(2):
            bank = (wi * 2 + mt) % 4
            for c in range(8):
                P.op("pe", lambda e, W=W, mt=mt, c=c, bank=bank: e.matmul(psf[bank][:, 0:256], lhsT=MEMT[:, c, mt * 128:(mt + 1) * 128],
                                                                           rhs=W[:, c, :], start=(c == 0), stop=(c == 7)),
                     reads=["MEMT", wres], writes=[PS(bank)])
            s = mt
            P.op("act", lambda e, s=s, bank=bank: e.copy(out=MST[s], in_=psf[bank][:, 0:256]), reads=[PS(bank)], writes=[("MST", s)])
            if wi == 1:
                P.op("pool", lambda e, mt=mt, s=s: e.tensor_copy(out=MV[:, mt, :], in_=MST[s]), reads=[("MST", s)], writes=["MV"])
            P.dma("sp", lambda e, s=s, dstout=dstout, mt=mt: e.dma_start(out=dstout[mt * 128:(mt + 1) * 128, :], in_=MST[s]),
                  ("st", "MST", s), reads=[("MST", s)])
    for fc in range(2):
        bank = 4 + fc
        for c in range(8):
            P.op("pe", lambda e, fc=fc, c=c, bank=bank: e.matmul(psf[bank][:, 0:256], lhsT=W_MK[:, c, fc * 128:(fc + 1) * 128],
                                                                  rhs=MEMT[:, c, :], start=(c == 0), stop=(c == 7)),
                 reads=["MEMT", "W_MK"], writes=[PS(bank)])
        P.op("act", lambda e, fc=fc, bank=bank: e.copy(out=MKT[:, fc, :], in_=psf[bank][:, 0:256]), reads=[PS(bank)], writes=["MKT"])

    def mem_attn(mkt, mkt_res, mvv, mv_res, n0, N, mq_res):
        for fc in range(2):
            bO, bD = 4 + fc, 6 + fc
            for hh in range(2):
                h = 2 * fc + hh
                pr = slice(64 * hh, 64 * hh + 64)
                for mt in range(2):
                    bS = mt
                    P.op("pe", lambda e, mt=mt, bS=bS, pr=pr, fc=fc: e.matmul(psf[bS][:, 0:N], lhsT=mkt[pr, fc, mt * 128:(mt + 1) * 128],
                                                                       rhs=MQT[pr, fc, n0:n0 + N], start=True, stop=True),
                         reads=[mkt_res] + mq_res, writes=[PS(bS)])
                    P.op("act", lambda e, mt=mt, bS=bS: e.activation(out=PT[mt][:, 0:N], in_=psf[bS][:, 0:N], func=AF.Exp, scale=SCALE),
                         reads=[PS(bS)], writes=[("PT", mt)])
                for mt in range(2):
                    P.op("pe", lambda e, mt=mt, pr=pr, h=h, bO=bO: e.matmul(psf[bO][pr, 0:N], lhsT=mvv[:, mt, h * 64:(h + 1) * 64], rhs=PT[mt][:, 0:N],
                                                                     start=(mt == 0), stop=(mt == 1)),
                         reads=[mv_res, ("PT", mt)], writes=[PS(bO)])
                for mt in range(2):
                    P.op("pe", lambda e, mt=mt, pr=pr, bD=bD: e.matmul(psf[bD][pr, 0:N], lhsT=ones[:, 0:64], rhs=PT[mt][:, 0:N],
                                                                start=(mt == 0), stop=(mt == 1)),
                         reads=["ones", ("PT", mt)], writes=[PS(bD)])
            P.op("act", lambda e, bD=bD: e.activation(out=RDEN[:, 0:N], in_=psf[bD][:, 0:N], func=AF.Ln), reads=[PS(bD)], writes=["RDEN"])
            P.op("act", lambda e: e.activation(out=RDEN[:, 0:N], in_=RDEN[:, 0:N], func=AF.Exp, scale=-1.0), reads=["RDEN"], writes=["RDEN"])
            P.op("dve", lambda e, bO=bO, fc=fc: e.tensor_tensor(out=MIXT[:, 6 + fc, n0:n0 + N], in0=psf[bO][:, 0:N], in1=RDEN[:, 0:N], op=ALU.mult),
                 reads=[PS(bO), "RDEN"], writes=[("MIXT", 6 + fc, n0)])

    for bi in range(4):
        mem_attn(MKT, "MKT", MV, "MV", bi * 512, 512, [("MQT", 0, bi), ("MQT", 1, bi)])
    if stop_after <= 3.2:
        P.emit(); return nc
    CMK = [aW.t("CMK%d" % i, [128, 2, 256], BF16) for i in range(2)]
    CMV = [aW.t("CMV%d" % i, [128, 2, 256], BF16) for i in range(2)]
    CMKT = [aW.t("CMKT%d" % i, [128, 2, 256], BF16) for i in range(2)]
    for s in range(4):
        sl = s % 2
        P.dma("pool", lambda e, s=s, sl=sl: e.dma_start(out=CMK[sl], in_=cmk[s].rearrange("(m p) f -> p m f", p=128)), ("CMK", sl), writes=[("CMK", sl)])
        P.dma("pool", lambda e, s=s, sl=sl: e.dma_start(out=CMV[sl], in_=cmv[s].rearrange("(m p) f -> p m f", p=128)), ("CMV", sl), writes=[("CMV", sl)])
        for mt in range(2):
            for fc in range(2):
                P.op("pe", lambda e, mt=mt, fc=fc, sl=sl: e.transpose(psb[3][:, (mt * 2 + fc) * 128:(mt * 2 + fc + 1) * 128],
                                                                      CMK[sl][:, mt, fc * 128:(fc + 1) * 128], ident),
                     reads=[("CMK", sl), "ident"], writes=[PS(3)])
        P.op("act", lambda e, sl=sl: e.copy(out=CMKT[sl].rearrange("p f (m n) -> p m f n", n=128),
                                            in_=psb[3][:, 0:512].rearrange("p (m f n) -> p m f n", f=2, n=128)),
             reads=[PS(3)], writes=[("CMKT", sl)])
        mem_attn(CMKT[sl], ("CMKT", sl), CMV[sl], ("CMV", sl), 2048 + 64 * s, 64, [("MQT", 0, 4), ("MQT", 1, 4)])

    if stop_after <= 3.4:
        P.emit(); return nc
    P.barrier()
    aW.reset()
    GLUB = aW.t("GLUB", [128, 2, GW], BF16)
    CT = aW.t("CT", [30, 256], F32)
    CST = [aW.t("CST%d" % i, [30, 256], F32) for i in range(2)]
    DW = [aW.t("DW%d" % i, [128, 512], F32) for i in range(4)]
    SQ = [aW.t("SQ%d" % i, [128, 512], F32) for i in range(2)]
    MEANT = aW.t("MEANT", [128, 512], F32)
    RSTD = aW.t("RSTD", [128, 512], F32)
    SU = [aW.t("SU%d" % i, [128, 512], BF16) for i in range(4)]
    cblk = [0]
    segs = [(0, 2048)] + [(2078 + 94 * s, 64) for s in range(4)]
    P.op("pool", lambda e: e.memset(GLU[:, :, 0:30], 0.0), writes=[("GLU", 0), ("GLU", 1)])
    for s in range(4):
        P.dma("sp", lambda e, s=s: e.dma_start(out=CT, in_=cconv[s]), "CT", writes=["CT"])
        for cc in range(2):
            P.op("pe", lambda e, cc=cc: e.transpose(psf[0][:, cc * 32:cc * 32 + 30], CT[:, cc * 128:(cc + 1) * 128], identF[0:30, 0:30]),
                 reads=["CT", "identF"], writes=[PS(0)])
        P.op("act", lambda e, s=s: e.copy(out=GLU[:, :, 2078 + 94 * s:2078 + 94 * s + 30],
                                          in_=psf[0][:, 0:64].rearrange("p (c n) -> p c n", n=32)[:, :, 0:30]),
             reads=[PS(0)], writes=[("GLU", 0), ("GLU", 1)])
    P.op("act", lambda e: e.copy(out=GLUB[:, 0, :], in_=GLU[:, 0, :]), reads=[("GLU", 0)], writes=[("GLUB", 0)])
    P.op("pool", lambda e: e.tensor_copy(out=GLUB[:, 1, :], in_=GLU[:, 1, :]), reads=[("GLU", 1)], writes=[("GLUB", 1)])
    for si, (gb, ln) in enumerate(segs):
        sl = si % 2
        for cc in range(2):
            P.op("pe", lambda e, cc=cc, gb=gb, ln=ln: e.transpose(psf[1][0:30, cc * 128:(cc + 1) * 128], GLU[:, cc, gb + ln:gb + ln + 30], identF),
                 reads=[("GLU", cc), "identF"], writes=[PS(1)])
        P.op("act", lambda e, sl=sl: e.copy(out=CST[sl], in_=psf[1][0:30, 0:256]), reads=[PS(1)], writes=[("CST", sl)])
        dst = convp if si == 0 else convs[si - 1]
        P.dma("sp", lambda e, sl=sl, dst=dst: e.dma_start(out=dst, in_=CST[sl]), ("st", "CST", sl), reads=[("CST", sl)])

    if stop_after <= 3.6:
        P.emit(); return nc

    def conv_s1(gb, t0, N, tok0):
        pb = cblk[0] % 2
        cblk[0] += 1
        DWl = [DW[2 * pb], DW[2 * pb + 1]]
        SUl = [SU[2 * pb], SU[2 * pb + 1]]
        for cc in range(2):
            dw = DWl[cc]
            for j in range(31):
                P.op("pe", lambda e, cc=cc, j=j: e.matmul(psf[cc][:, 0:N], lhsT=DIAG[:, cc * 31 + j, :], rhs=GLUB[:, cc, gb + t0 + j:gb + t0 + j + N],
                                                          start=(j == 0), stop=(j == 30)), reads=["DIAG", ("GLUB", cc)], writes=[PS(cc)])
            P.op("act", lambda e, cc=cc, dw=dw: e.activation(out=dw[:, 0:N], in_=psf[cc][:, 0:N], func=AF.Identity, bias=cpar[:, 0, cc:cc + 1]),
                 reads=[PS(cc), "cpar"], writes=[("DW", cc, pb)])
            P.op("act", lambda e, cc=cc, dw=dw: e.activation(out=SQ[cc][:, 0:N], in_=dw[:, 0:N], func=AF.Square),
                 reads=[("DW", cc, pb)], writes=[("SQ", cc)])
        return (pb, DWl, SUl)

    def conv_s2(ctx, gb, t0, N, tok0):
        pb, DWl, SUl = ctx
        for cc in range(2):
            P.op("pe", lambda e, cc=cc: e.matmul(psf[2][:, 0:N], lhsT=onesF, rhs=DWl[cc][:, 0:N], start=(cc == 0), stop=(cc == 1)),
                 reads=["onesF", ("DW", cc, pb)], writes=[PS(2)])
        for cc in range(2):
            P.op("pe", lambda e, cc=cc: e.matmul(psf[3][:, 0:N], lhsT=onesF, rhs=SQ[cc][:, 0:N], start=(cc == 0), stop=(cc == 1)),
                 reads=["onesF", ("SQ", cc)], writes=[PS(3)])
        P.op("dve", lambda e: e.tensor_scalar(out=MEANT[:, 0:N], in0=psf[2][:, 0:N], scalar1=1.0 / 256, scalar2=None, op0=ALU.mult),
             reads=[PS(2)], writes=["MEANT"])
        P.op("dve", lambda e: e.tensor_tensor(out=RSTD[:, 0:N], in0=MEANT[:, 0:N], in1=MEANT[:, 0:N], op=ALU.mult),
             reads=["MEANT"], writes=["RSTD"])
        P.op("dve", lambda e: e.scalar_tensor_tensor(out=RSTD[:, 0:N], in0=psf[3][:, 0:N], scalar=1.0 / 256, in1=RSTD[:, 0:N],
                                                     op0=ALU.mult, op1=ALU.subtract), reads=[PS(3), "RSTD"], writes=["RSTD"])
        P.op("dve", lambda e: e.tensor_scalar(out=RSTD[:, 0:N], in0=RSTD[:, 0:N], scalar1=EPS, scalar2=None, op0=ALU.add),
             reads=["RSTD"], writes=["RSTD"])
        P.op("act", lambda e: e.activation(out=RSTD[:, 0:N], in_=RSTD[:, 0:N], func=AF.Ln), reads=["RSTD"], writes=["RSTD"])
        P.op("act", lambda e: e.activation(out=RSTD[:, 0:N], in_=RSTD[:, 0:N], func=AF.Exp, scale=-0.5), reads=["RSTD"], writes=["RSTD"])
        for cc in range(2):
            dw = DWl[cc]
            P.op("dve", lambda e, dw=dw: e.tensor_tensor(out=dw[:, 0:N], in0=dw[:, 0:N], in1=MEANT[:, 0:N], op=ALU.subtract),
                 reads=[("DW", cc, pb), "MEANT"], writes=[("DW", cc, pb)])
            P.op("pool", lambda e, dw=dw: e.tensor_tensor(out=dw[:, 0:N], in0=dw[:, 0:N], in1=RSTD[:, 0:N], op=ALU.mult),
                 reads=[("DW", cc, pb), "RSTD"], writes=[("DW", cc, pb)])
            P.op("act", lambda e, dw=dw, cc=cc: e.activation(out=SUl[cc][:, 0:N], in_=dw[:, 0:N], func=AF.Silu,
                                                             scale=cpar[:, 1, cc:cc + 1], bias=cpar[:, 2, cc:cc + 1]),
                 reads=[("DW", cc, pb), "cpar"], writes=[("SU", cc, pb)])

    def conv_s3(ctx, gb, t0, N, tok0):
        pb, DWl, SUl = ctx
        for oc in range(2):
            bank = 4 + oc
            for cc in range(2):
                P.op("pe", lambda e, oc=oc, cc=cc, bank=bank: e.matmul(psf[bank][:, 0:N], lhsT=W_CPW[:, cc, oc * 128:(oc + 1) * 128],
                                                                        rhs=SUl[cc][:, 0:N], start=(cc == 0), stop=(cc == 1)),
                     reads=["W_CPW", ("SU", cc, pb)], writes=[PS(bank)])
            P.op("act", lambda e, oc=oc, bank=bank: e.copy(out=MIXT[:, 4 + oc, tok0:tok0 + N], in_=psf[bank][:, 0:N]),
                 reads=[PS(bank)], writes=[("MIXT", 4 + oc, tok0)])

    cblocks = [(0, b_ * 512, 512, b_ * 512) for b_ in range(4)] + [(2078 + 94 * s_, 0, 64, 2048 + 64 * s_) for s_ in range(4)]
    cctx = {}
    for k_ in range(len(cblocks) + 2):
        if 0 <= k_ - 1 < len(cblocks):
            conv_s2(cctx[k_ - 1], *cblocks[k_ - 1])
        if k_ < len(cblocks):
            cctx[k_] = conv_s1(*cblocks[k_])
        if 0 <= k_ - 2 < len(cblocks):
            conv_s3(cctx[k_ - 2], *cblocks[k_ - 2])
    if stop_after <= 4:
        P.emit(); return nc

    P.barrier()
    WOUT_OFF = 24576
    assert WOUT_OFF >= aA_base and WOUT_OFF + 16384 <= aFREE.off
    W_OUT = nc.alloc_sbuf_tensor_at("W_OUT", [128, 8, 1024], BF16, offset=aFREE.base + WOUT_OFF).ap()
    for c in range(8):
        P.dma("pool", lambda e, c=c: e.dma_start(out=W_OUT[:, c, :], in_=w_out[c * 128:(c + 1) * 128, :]), "W_OUT", writes=["W_OUT"])
    aA = Arena(aR.base, 36864)
    NSL = 4
    E = [aA.t("E%d" % i, [128, 512], BF16) for i in range(NSL)]
    SPb = [aA.t("SP%d" % i, [128, 512], BF16) for i in range(NSL)]
    Xb = [aA.t("X%d" % i, [128, 512], BF16) for i in range(NSL)]
    Wb = Xb
    SACC = [aA.t("SACC%d" % i, [128, 512], BF16) for i in range(4)]
    KC = [aA.t("KC%d" % i, [128, 512], BF16) for i in range(5)]
    VC = [aA.t("VC%d" % i, [128, 512], BF16) for i in range(10)]
    KCT = [aA.t("KCT%d" % i, [128, 4, 128], BF16) for i in range(2)]
    QBD = [aA.t("QBD%d" % i, [128, 4, 128], BF16) for i in range(2)]
    FSRC = aA.t("FSRC", [128, 512], BF16)
    P.op("dve", lambda e: e.memset(FSRC, 1.0), writes=["FSRC"])

    items = []
    gi = 0
    for hp in range(4):
        for Q in range(4):
            for hh in range(2):
                nkb = 4 * Q + 4
                for kb in range(nkb - 1, -1, -1):
                    items.append(dict(kind="p", hp=hp, Q=Q, hh=hh, kb=kb, first=(kb == nkb - 1), last=(kb == 0),
                                      grp=gi, sacc=(gi * 2 + hh) % 2, obank=4 + (gi % 2), endgrp=(hh == 1 and kb == 0)))
            gi += 1
    for s in range(4):
        for kb in range(32, -1, -1):
            items.append(dict(kind="s", s=s, kb=kb, first=(kb == 32), last=(kb == 0), grp=gi, sacc=gi % 2,
                              obank=4 + (gi % 2), endgrp=(kb == 0)))
        gi += 1
    gcount = {}
    for it in items:
        n = gcount.get((it["grp"], it.get("hh", 0)), 0)
        gcount[(it["grp"], it.get("hh", 0))] = n + 1
        gpar = it["hh"] if it["kind"] == "p" else it["grp"] % 2
        it["sa_cur"] = gpar * 2 + (n % 2)
        it["sa_nxt"] = gpar * 2 + ((n + 1) % 2)
        it["gpar"] = gpar
    for i, it in enumerate(items):
        it["i"] = i
        it["sl"] = i % NSL
        it["zb"] = i % 2
        it["sb"] = 2 + (i % 2)

    def build_qbd(s_):
        q = QBD[s_ % 2]
        P.op("pool", lambda e: e.memset(q, 0.0), writes=[("QBD", s_ % 2)])
        for hh in range(2):
            pr = slice(64 * hh, 64 * hh + 64)
            P.op("pool", lambda e, pr=pr, hh=hh: e.tensor_copy(out=q[pr, :, 64 * hh:64 * hh + 64], in_=QS[pr, :, 64 * s_:64 * s_ + 64]),
                 reads=[("QS", j) for j in range(4)] + [("QBD", s_ % 2)], writes=[("QBD", s_ % 2)])

    def rng(it):
        if it["kind"] == "p":
            c0 = max(0, it["kb"] - 4 * it["Q"]) * 128
            return slice(0, 128), c0, 512
        if it["kb"] == 32:
            o = 64 * (it["s"] % 2)
            return slice(o, o + 64), 0, 512
        return slice(0, 128), 0, 512

    wgv0 = w_eg.rearrange("e (p c) f -> (e p) (c f)", c=8)
    wuv0 = w_eu.rearrange("e (p c) f -> (e p) (c f)", c=8)
    wdv0 = w_ed.rearrange("e (p c) d -> (e p) (c d)", c=2)
    cast_jobs = [(src, dst, g) for g in range(8) for (src, dst) in ((wgv0, WGB), (wuv0, WUB), (wdv0, WDB))]

    def st_load(it):
        if it["kind"] == "p" and it["i"] % 12 == 0 and cast_jobs:
            src, dst, g = cast_jobs.pop(0)
            P.dma("pool", lambda e: e.dma_start(out=dst[g * 512:(g + 1) * 512, :], in_=src[g * 512:(g + 1) * 512, :]), "WCAST", writes=["WCAST"])
        if it["kind"] != "s" or it["kb"] == 32:
            return
        s, kb = it["s"], it["kb"]
        sl3 = it["i"] % 5
        P.dma("pool", lambda e: e.dma_start(out=KC[sl3], in_=ck[s, kb * 128:(kb + 1) * 128, :]), ("KC", sl3), writes=[("KC", sl3)])
        sl8 = it["i"] % 10
        P.dma("pool", lambda e: e.dma_start(out=VC[sl8], in_=cv[s, kb * 128:(kb + 1) * 128, :]), ("VC", sl8), writes=[("VC", sl8)])

    def st_T(it):
        if it["kind"] != "s" or it["kb"] == 32:
            return
        sl3 = it["i"] % 5
        s2 = it["i"] % 2
        for hp in range(4):
            P.op("pe", lambda e, hp=hp: e.transpose(psb[6][:, hp * 128:(hp + 1) * 128], KC[sl3][:, hp * 128:(hp + 1) * 128], ident),
                 reads=[("KC", sl3), "ident"], writes=[PS(6)])
        P.op("dve", lambda e: e.tensor_copy(out=KCT[s2], in_=psb[6][:, 0:512].rearrange("p (h n) -> p h n", n=128)),
             reads=[PS(6)], writes=[("KCT", s2)])

    def st_A(it):
        pr, c0, ce = rng(it)
        zb = it["zb"]
        if it["kind"] == "p":
            hp, Q, hh, kb = it["hp"], it["Q"], it["hh"], it["kb"]
            hr = slice(64 * hh, 64 * hh + 64)
            P.op("pe", lambda e: e.matmul(psf[zb][:, c0:512], lhsT=KT[hr, hp, kb * 128:(kb + 1) * 128], rhs=QT[hr, hp, Q * 512 + c0:Q * 512 + 512],
                                          start=True, stop=True),
                 reads=[("KT", hp, kb // 4), ("QT", hp, Q)], writes=[PS(zb)])
        else:
            s, kb = it["s"], it["kb"]
            if kb == 32:
                build_qbd(s)
            for hp in range(4):
                if kb == 32:
                    P.op("pe", lambda e, hp=hp: e.matmul(psf[zb][pr, hp * 128:(hp + 1) * 128], lhsT=KS[:, hp, 64 * s:64 * s + 64], rhs=QBD[s % 2][:, hp, :],
                                                         start=True, stop=True),
                         reads=[("KS", hp), ("QBD", s % 2)], writes=[PS(zb)])
                else:
                    s2 = it["i"] % 2
                    P.op("pe", lambda e, hp=hp: e.matmul(psf[zb][:, hp * 128:(hp + 1) * 128], lhsT=KCT[s2][:, hp, :], rhs=QBD[s % 2][:, hp, :],
                                                         start=True, stop=True),
                         reads=[("KCT", s2), ("QBD", s % 2)], writes=[PS(zb)])

    def st_B(it):
        pr, c0, ce = rng(it)
        zb, sl = it["zb"], it["sl"]
        P.op("act", lambda e: e.activation(out=E[sl][pr, c0:512], in_=psf[zb][pr, c0:512], func=AF.Exp, scale=SCALE),
             reads=[PS(zb)], writes=[("E", sl)])
        if it["kind"] == "p" and it["kb"] >= 4 * it["Q"]:
            P.op("dve", lambda e: e.tensor_tensor(out=E[sl][:, c0:c0 + 128], in0=E[sl][:, c0:c0 + 128], in1=tri, op=ALU.mult),
                 reads=[("E", sl), "tri"], writes=[("E", sl)])
        if it["kind"] == "s" and it["kb"] == 32:
            P.op("dve", lambda e: e.tensor_tensor(out=E[sl][pr, :], in0=E[sl][pr, :], in1=tri8[pr].rearrange("p h c -> p (h c)"), op=ALU.mult),
                 reads=[("E", sl), "tri8"], writes=[("E", sl)])
        P.op("act", lambda e: e.activation(out=SPb[sl][pr, c0:512], in_=E[sl][pr, c0:512], func=AF.Ln, bias=1.0),
             reads=[("E", sl)], writes=[("SP", sl)])

    def st_C(it):
        pr, c0, ce = rng(it)
        sb, sl, sa, sn = it["sb"], it["sl"], it["sa_cur"], it["sa_nxt"]
        if it["first"]:
            P.op("dve", lambda e: e.memset(SACC[sa], 0.0), writes=[("SACC", sa)])
            P.op("dve", lambda e: e.memset(SACC[sn], 0.0), writes=[("SACC", sn)])
        P.op("pe", lambda e: e.matmul(psf[sb][pr, c0:512], lhsT=Uinc[pr, pr], rhs=SPb[sl][pr, c0:512], start=True, stop=it["first"]),
             reads=["Uinc", ("SP", sl)], writes=[PS(sb)])
        if not it["first"]:
            P.op("pe", lambda e: e.matmul(psf[sb][pr, c0:512], lhsT=ones[:, pr], rhs=SACC[sa][:, c0:512], start=False, stop=True),
                 reads=["ones", ("SACC", sa)], writes=[PS(sb)])
        if not it["last"]:
            P.op("dve", lambda e: e.tensor_tensor(out=SACC[sn][pr, c0:512], in0=SACC[sa][pr, c0:512], in1=SPb[sl][pr, c0:512], op=ALU.add),
                 reads=[("SACC", sa), ("SP", sl)], writes=[("SACC", sn)])

    def st_D(it):
        pr, c0, ce = rng(it)
        sb, sl, sa = it["sb"], it["sl"], it["sacc"]
        P.op("act", lambda e: e.activation(out=Xb[sl][pr, c0:512], in_=psf[sb][pr, c0:512], func=AF.Exp, scale=-1.0),
             reads=[PS(sb)], writes=[("X", sl), ("W", sl)])
        P.op("dve", lambda e: e.tensor_tensor(out=Wb[sl][pr, c0:512], in0=E[sl][pr, c0:512], in1=Xb[sl][pr, c0:512], op=ALU.mult),
             reads=[("E", sl), ("X", sl)], writes=[("W", sl)])

    def st_E(it):
        pr, c0, ce = rng(it)
        sl, ob = it["sl"], it["obank"]
        if it["kind"] == "p":
            hp, Q, hh, kb = it["hp"], it["Q"], it["hh"], it["kb"]
            h = 2 * hp + hh
            hr = slice(64 * hh, 64 * hh + 64)
            P.op("pe", lambda e: e.matmul(psf[ob][hr, c0:512], lhsT=V_ALL[:, kb, h * 64:(h + 1) * 64], rhs=Wb[sl][:, c0:512],
                                          start=it["first"], stop=it["last"], skip_group_check=True),
                 reads=[("V_ALL", kb), ("W", sl)], writes=[PS(ob)])
            if it["endgrp"]:
                P.op("act", lambda e: e.copy(out=MIXT[:, hp, Q * 512:(Q + 1) * 512], in_=psf[ob]), reads=[PS(ob)], writes=[("MIXT", hp, Q * 512)])
        else:
            s, kb = it["s"], it["kb"]
            sl3 = it["i"] % 5
            for h in range(8):
                hp, hh = h // 2, h % 2
                hr = slice(64 * hh, 64 * hh + 64)
                if kb == 32:
                    vt = V_ALL[pr, 16 + s // 2, h * 64:(h + 1) * 64]
                    vres = ("V_ALL", 16 + s // 2)
                else:
                    vt = VC[it["i"] % 10][:, h * 64:(h + 1) * 64]
                    vres = ("VC", it["i"] % 10)
                P.op("pe", lambda e, h=h, hp=hp, hr=hr, vt=vt: e.matmul(psf[ob][hr, hp * 64:(hp + 1) * 64], lhsT=vt, rhs=Wb[sl][pr, h * 64:(h + 1) * 64],
                                                                          start=(it["first"] and hp == 0), stop=it["last"], skip_group_check=True),
                     reads=[vres, ("W", sl)], writes=[PS(ob)])
            if it["endgrp"]:
                P.op("act", lambda e: e.copy(out=MIXT[:, 0:4, 2048 + 64 * s:2048 + 64 * s + 64],
                                             in_=psf[ob][:, 0:256].rearrange("p (h n) -> p h n", n=64)),
                     reads=[PS(ob)], writes=[("MIXT", "s", s)])

    n_it = len(items)
    NFILL = 1
    for step in range(n_it + 10):
        def g(k):
            j = step - k
            return items[j] if 0 <= j < n_it else None
        for k, fnst in ((0, st_load), (4, st_T), (5, st_A), (6, st_B), (8, st_C), (8, st_D), (9, st_E)):
            it = g(k)
            if it is not None:
                fnst(it)
            if k in (5, 8) and fnst is not st_D and NFILL and it is not None and it["kind"] == "p":
                P.op("pe", lambda e: e.matmul(psf[7], lhsT=ones, rhs=FSRC, start=True, stop=True), reads=["FSRC", "ones"])
    if stop_after <= 5:
        P.emit(); return nc

    P.barrier()
    aFREE.reset()
    RT = aFREE.t("RT", [128, 18, 88], F32)
    off_rt = aFREE.off
    LNG1 = aFREE.t("LNG1", [128, 1024], F32)
    LNB1 = aFREE.t("LNB1", [128, 1024], F32)
    XIN5 = [aFREE.t("XIN5_%d" % i, [128, 1024], F32) for i in range(2)]
    assert aFREE.off <= WOUT_OFF
    aFREE.off = WOUT_OFF + 16384
    XIN5.append(aFREE.t("XIN5_2", [128, 1024], F32))
    H5 = XIN5
    X1B = [aFREE.t("X1B_%d" % i, [128, 1024], BF16) for i in range(2)]
    for (tl, src) in ((LNG1, ln1_g), (LNB1, ln1_b)):
        P.dma("sp", lambda e, tl=tl, src=src: e.dma_start(out=tl, in_=src.partition_broadcast(128)), ("LNP", id(tl)), writes=[("LNP", id(tl))])
    LN1R = [("LNP", id(LNG1)), ("LNP", id(LNB1))]

    def mix_res(t):
        n0 = t * 128
        if t < 16:
            q = (n0 // 512) * 512
            return [("MIXT", c, q) for c in range(8)]
        r = []
        for s in (2 * (t - 16), 2 * (t - 16) + 1):
            r += [("MIXT", "s", s)] + [("MIXT", c, 2048 + 64 * s) for c in range(4, 8)]
        return r

    def router_mm(t):
        bank = 4 + (t % 2)
        for c in range(8):
            P.op("pe", lambda e, c=c: e.matmul(psf[bank][:, 0:36], lhsT=XT[:, c, tcols(t)], rhs=W_R[:, c, :], start=(c == 0), stop=(c == 7)),
                 reads=[("XT", t), "W_R"], writes=[PS(bank)])
        P.op("dve", lambda e: e.tensor_tensor(out=RT[:, t, 84:88], in0=psf[bank][:, 0:4], in1=RB[:, 0:4], op=ALU.add),
             reads=[PS(bank), "RB"], writes=["LG"])
        P.op("dve", lambda e: e.tensor_tensor(out=GATE[:, t, :], in0=psf[bank][:, 4:36], in1=RB[:, 4:36], op=ALU.add),
             reads=[PS(bank), "RB"], writes=["LG"])

    def p5_load(t):
        s = t % 3
        P.dma("sp", lambda e: e.dma_start(out=XIN5[s], in_=XNS[t * 128:(t + 1) * 128, :]), ("XIN5", s), reads=[("XNS", t)], writes=[("XIN5", s)])

    def p5_mm(t):
        s = t % 3
        s2 = t % 2
        for half in range(2):
            bank = 2 * s2 + half
            for c in range(8):
                P.op("pe", lambda e, c=c, half=half, bank=bank: e.matmul(psf[bank], lhsT=MIXT[:, c, tcols(t)], rhs=W_OUT[:, c, half * 512:(half + 1) * 512],
                                                                         start=(c == 0), stop=(c == 7)),
                     reads=mix_res(t) + ["W_OUT"], writes=[PS(bank)])
            P.op("dve", lambda e, half=half, bank=bank: e.scalar_tensor_tensor(
                out=H5[s][:, half * 512:(half + 1) * 512], in0=XIN5[s][:, half * 512:(half + 1) * 512], scalar=ALPHA, in1=psf[bank],
                op0=ALU.mult, op1=ALU.add), reads=[("XIN5", s), PS(bank)], writes=[("XIN5", s)])

    def p5_ln(t):
        s = t % 3
        s2 = t % 2
        layernorm("ln1", H5[s], ("XIN5", s), H5[s], ("XIN5", s), LNG1, LNB1, LN1R)
        P.op("act", lambda e: e.mul(out=R[:, t, :], in_=H5[s], mul=ALPHA), reads=[("XIN5", s)], writes=[("R", t)])
        P.op("act", lambda e: e.copy(out=X1B[s2], in_=H5[s]), reads=[("XIN5", s)], writes=[("X1B", s2)])

    def p5_T(t):
        s2 = t % 2
        transpose_to_XT(t, X1B[s2], ("X1B", s2), 6 + s2)

    p5_load(0)
    p5_load(1)
    for t in range(20):
        if t < 18:
            p5_mm(t)
        if 1 <= t < 19:
            p5_T(t - 1)
        if t < 18:
            p5_ln(t)
            if t + 2 < 18:
                p5_load(t + 2)
        if t >= 2:
            router_mm(t - 2)
    P.barrier()
    aF2 = Arena(aFREE.base + off_rt, aFREE.size - off_rt)
    X_ = mybir.AxisListType.X
    NS = 50
    TS = 256
    SELB = aF2.t("SELB", [128, 18, 32], BF16)
    RANK = aF2.t("RANK", [128, 18, 32], F32)
    TMP = aF2.t("TMP", [128, 18, 32], F32)
    CNT = aF2.t("CNT", [128, 32], F32)
    NTL = aF2.t("NTL", [128, 32], F32)
    SCA = aF2.t("SCA", [128, 32], F32)
    SCB = aF2.t("SCB", [128, 32], F32)
    CMP9 = aF2.t("CMP9", [128, 32, 9], F32)
    THR = aF2.t("THR", [128, 32, 9], F32)
    S1F = aF2.t("S1F", [128, 18], F32)
    S2F = aF2.t("S2F", [128, 18], F32)
    S1I = aF2.t("S1I", [128, 18], I32)
    S2I = aF2.t("S2I", [128, 18], I32)
    DV1 = aF2.t("DV1", [128, 18], I32)
    DV2 = aF2.t("DV2", [128, 18], I32)
    JCI = aF2.t("JCI", [128, NS, 32], I32)
    JC = aF2.t("JC", [128, NS, 32], F32)
    CMPJ = aF2.t("CMPJ", [128, NS, 32], F32)
    EJF = aF2.t("EJF", [128, NS], F32)
    PIDI = aF2.t("PIDI", [128, 1], I32)
    PIDF = aF2.t("PIDF", [128, 1], F32)
    XB = [aF2.t("XB%d" % i, [128, 1028], BF16) for i in range(8)]
    TRIB = aF2.t("TRIB", [128, 128], BF16)

    gm = RT[:, :, 0]
    ngs = RT[:, :, 1]
    gw = RT[:, :, 2]
    m1 = RT[:, :, 3]
    m2 = RT[:, :, 4]
    d21 = RT[:, :, 5]
    w1 = RT[:, :, 6]
    w2 = RT[:, :, 7]
    gmask = RT[:, :, 8:12]
    pen = RT[:, :, 12:16]
    ge = RT[:, :, 16:20]
    em = GATE[:, :, :]
    mk1 = RT[:, :, 20:52]
    mk2 = RT[:, :, 52:84]
    lg4 = RT[:, :, 84:88]

    def bc(a_, n):
        return a_.unsqueeze(2).to_broadcast([128, 18, n])

    def D(fn):
        P.op("dve", fn, reads=["RT", "LG"], writes=["RT"])
    D(lambda e: e.tensor_reduce(out=gm, in_=lg4, axis=X_, op=ALU.max))
    D(lambda e: e.tensor_tensor(out=gmask, in0=lg4, in1=bc(gm, 4), op=ALU.is_ge))
    D(lambda e: e.tensor_scalar(out=pen, in0=gmask, scalar1=-1.0, scalar2=BIG, op0=ALU.add, op1=ALU.mult))
    D(lambda e: e.tensor_tensor(out=ge, in0=lg4, in1=bc(gm, 4), op=ALU.subtract))
    P.op("act", lambda e: e.activation(out=ge, in_=ge, func=AF.Exp), reads=["RT"], writes=["RT"])
    D(lambda e: e.tensor_reduce(out=ngs, in_=ge, axis=X_, op=ALU.add))
    D(lambda e: e.reciprocal(out=gw, in_=ngs))
    D(lambda e: e.tensor_tensor(out=em.rearrange("p t (g x) -> p t g x", x=8), in0=em.rearrange("p t (g x) -> p t g x", x=8),
                                in1=pen.unsqueeze(3).to_broadcast([128, 18, 4, 8]), op=ALU.add))
    D(lambda e: e.tensor_reduce(out=m1, in_=em, axis=X_, op=ALU.max))
    D(lambda e: e.tensor_tensor(out=mk1, in0=em, in1=bc(m1, 32), op=ALU.is_ge))
    D(lambda e: e.scalar_tensor_tensor(out=em, in0=mk1, scalar=-BIG, in1=em, op0=ALU.mult, op1=ALU.add))
    D(lambda e: e.tensor_reduce(out=m2, in_=em, axis=X_, op=ALU.max))
    D(lambda e: e.tensor_tensor(out=mk2, in0=em, in1=bc(m2, 32), op=ALU.is_ge))
    D(lambda e: e.tensor_tensor(out=d21, in0=m2, in1=m1, op=ALU.subtract))
    P.op("act", lambda e: e.activation(out=d21, in_=d21, func=AF.Exp), reads=["RT"], writes=["RT"])
    D(lambda e: e.tensor_scalar(out=w1, in0=d21, scalar1=1.0, scalar2=None, op0=ALU.add))
    D(lambda e: e.reciprocal(out=w1, in_=w1))
    D(lambda e: e.tensor_tensor(out=w2, in0=d21, in1=w1, op=ALU.mult))
    D(lambda e: e.tensor_tensor(out=w1, in0=w1, in1=gw, op=ALU.mult))
    D(lambda e: e.tensor_tensor(out=w2, in0=w2, in1=gw, op=ALU.mult))
    D(lambda e: e.tensor_tensor(out=SELB, in0=mk1, in1=mk2, op=ALU.add))
    P.op("act", lambda e: e.copy(out=TRIB, in_=tri), reads=["tri"], writes=["TRIB"])
    for t in range(18):
        bank = 0 if t < 9 else 1
        cs = slice((t % 9) * 32, (t % 9) * 32 + 32)
        for t2 in range(t + 1):
            P.op("pe", lambda e, t2=t2, t=t, bank=bank, cs=cs: e.matmul(psf[bank][:, cs], lhsT=(TRIB if t2 == t else ones), rhs=SELB[:, t2, :],
                                                                      start=(t2 == 0), stop=(t2 == t)),
                 reads=["RT", "TRIB", "ones"], writes=[PS(bank)])
    for t2 in range(18):
        P.op("pe", lambda e, t2=t2: e.matmul(psf[2][:, 0:32], lhsT=ones, rhs=SELB[:, t2, :], start=(t2 == 0), stop=(t2 == 17)),
             reads=["RT", "ones"], writes=[PS(2)])
    P.op("dve", lambda e: e.tensor_copy(out=RANK[:, 0:9, :], in_=psf[0][:, 0:288].rearrange("p (t x) -> p t x", x=32)), reads=[PS(0)], writes=["RANK"])
    P.op("dve", lambda e: e.tensor_copy(out=RANK[:, 9:18, :], in_=psf[1][:, 0:288].rearrange("p (t x) -> p t x", x=32)), reads=[PS(1), "RANK"], writes=["RANK"])
    P.op("dve", lambda e: e.tensor_copy(out=CNT, in_=psf[2][:, 0:32]), reads=[PS(2)], writes=["CNT"])
    for k in range(9):
        P.op("pool", lambda e, k=k: e.memset(THR[:, :, k:k + 1], float(TS * k)), writes=["THR"])
    P.op("dve", lambda e: e.tensor_tensor(out=CMP9, in0=CNT.unsqueeze(2).to_broadcast([128, 32, 9]), in1=THR, op=ALU.is_gt),
         reads=["CNT", "THR"], writes=["CMP9"])
    P.op("dve", lambda e: e.tensor_reduce(out=NTL, in_=CMP9, axis=X_, op=ALU.add), reads=["CMP9"], writes=["NTL"])
    src_, dst_ = NTL, SCA
    for sh in (1, 2, 4, 8, 16):
        P.op("dve", lambda e, src_=src_, dst_=dst_, sh=sh: e.tensor_copy(out=dst_[:, 0:sh], in_=src_[:, 0:sh]), reads=["NTL", "SC"], writes=["SC"])
        P.op("dve", lambda e, src_=src_, dst_=dst_, sh=sh: e.tensor_tensor(out=dst_[:, sh:32], in0=src_[:, sh:32], in1=src_[:, 0:32 - sh], op=ALU.add),
             reads=["NTL", "SC"], writes=["SC"])
        src_, dst_ = dst_, (SCB if dst_ is SCA else SCA)
    INCL = src_
    EXCL = dst_
    P.op("dve", lambda e: e.tensor_tensor(out=EXCL, in0=INCL, in1=NTL, op=ALU.subtract), reads=["SC", "NTL"], writes=["SC"])
    P.op("dve", lambda e: e.tensor_scalar(out=EXCL, in0=EXCL, scalar1=float(TS), scalar2=None, op0=ALU.mult), reads=["SC"], writes=["SC"])
    P.op("dve", lambda e: e.tensor_tensor(out=RANK, in0=RANK, in1=EXCL.unsqueeze(1).to_broadcast([128, 18, 32]), op=ALU.add),
         reads=["RANK", "SC"], writes=["RANK"])
    for (mk_, SF_, SI_) in ((mk1, S1F, S1I), (mk2, S2F, S2I)):
        P.op("dve", lambda e, mk_=mk_: e.tensor_tensor(out=TMP, in0=RANK, in1=mk_, op=ALU.mult), reads=["RANK", "RT"], writes=["TMP"])
        P.op("dve", lambda e, SF_=SF_: e.tensor_reduce(out=SF_, in_=TMP, axis=X_, op=ALU.add), reads=["TMP"], writes=["SF"])
        P.op("dve", lambda e, SF_=SF_, SI_=SI_: e.tensor_copy(out=SI_, in_=SF_), reads=["SF"], writes=["SI"])
    P.op("pool", lambda e: e.iota(DV1, pattern=[[128, 18]], base=0, channel_multiplier=1), writes=["DV"])
    P.op("pool", lambda e: e.iota(DV2, pattern=[[128, 18]], base=NT, channel_multiplier=1), writes=["DV"])
    P.op("pool", lambda e: e.iota(JCI, pattern=[[1, NS], [0, 32]], base=0, channel_multiplier=0), writes=["JCI"])
    P.op("dve", lambda e: e.tensor_copy(out=JC, in_=JCI), reads=["JCI"], writes=["JC"])
    P.op("dve", lambda e: e.tensor_tensor(out=CMPJ, in0=INCL.unsqueeze(1).to_broadcast([128, NS, 32]), in1=JC, op=ALU.is_le),
         reads=["SC", "JC"], writes=["CMPJ"])
    P.op("dve", lambda e: e.tensor_reduce(out=EJF, in_=CMPJ, axis=X_, op=ALU.add), reads=["CMPJ"], writes=["EJF"])
    P.op("pool", lambda e: e.iota(PIDI, pattern=[[0, 1]], base=0, channel_multiplier=1), writes=["PIDI"])
    P.op("dve", lambda e: e.tensor_copy(out=PIDF, in_=PIDI), reads=["PIDI"], writes=["PIDF"])
    P.op("dve", lambda e: e.tensor_scalar(out=EJF, in0=EJF, scalar1=128.0, scalar2=None, op0=ALU.mult), reads=["EJF"], writes=["EJF"])
    P.op("dve", lambda e: e.tensor_scalar(out=EJF, in0=EJF, scalar1=PIDF[:, 0:1], scalar2=None, op0=ALU.add), reads=["EJF", "PIDF"], writes=["EJF"])
    P.op("dve", lambda e: e.tensor_copy(out=IDXW, in_=EJF), reads=["EJF"], writes=["IDXW"])
    for t in range(18):
        for k in range(2):
            xb = XB[(t % 4) * 2 + k]
            xr = ("XB", (t % 4) * 2 + k)
            DV_, SI_, wk = (DV1, S1I, w1) if k == 0 else (DV2, S2I, w2)
            P.op("act", lambda e, xb=xb, t=t: e.mul(out=xb[:, 0:1024], in_=R[:, t, :], mul=1.0 / ALPHA), reads=[("R", t)], writes=[xr])
            P.op("dve", lambda e, xb=xb, t=t, DV_=DV_: e.tensor_copy(out=xb[:, 1024:1026].bitcast(I32), in_=DV_[:, t:t + 1]),
                 reads=["DV", xr], writes=[xr])
            P.op("dve", lambda e, xb=xb, t=t, wk=wk: e.tensor_copy(out=xb[:, 1026:1028].bitcast(F32), in_=wk[:, t:t + 1]),
                 reads=["RT", xr], writes=[xr])
            P.dma("pool", lambda e, xb=xb, t=t, SI_=SI_: e.indirect_dma_start(
                out=XS, out_offset=bass.IndirectOffsetOnAxis(ap=SI_[:, t:t + 1], axis=0), in_=xb, in_offset=None), "XSsc", reads=[xr, "SI", "XSfill"], writes=[("XSs", t, k)])
    XS_ALL = [("XSs", t, k) for t in range(18) for k in range(2)]
    if stop_after <= 6:
        P.emit(); return nc

    P.barrier()
    aMIX.reset()
    NW = 3
    WG = [aMIX.t("WG%d" % i, [128, 8, 256], BF16) for i in range(NW)]
    WU = [aMIX.t("WU%d" % i, [128, 8, 256], BF16) for i in range(NW)]
    WD = [aMIX.t("WD%d" % i, [128, 2, 1024], BF16) for i in range(NW)]
    aFREE.reset()
    XSB = [aFREE.t("XSB%d" % i, [128, 1028], BF16) for i in range(6)]
    XST = [aFREE.t("XST%d" % i, [128, 8, 256], BF16) for i in range(2)]
    SLs = [aFREE.t("SLs%d" % i, [128, 512], BF16) for i in range(2)]
    HIDT = [aFREE.t("HIDT%d" % i, [128, 512], BF16) for i in range(2)]
    YSB = [aFREE.t("YSB%d" % i, [128, 1024], F32) for i in range(4)]
    LNG2 = aFREE.t("LNG2", [128, 1024], F32)
    LNB2 = aFREE.t("LNB2", [128, 1024], F32)
    P.dma("sp", lambda e: e.dma_start(out=LNG2, in_=ln2_g.partition_broadcast(128)), "LNG2", writes=["LNG2"])
    P.dma("sp", lambda e: e.dma_start(out=LNB2, in_=ln2_b.partition_broadcast(128)), "LNB2", writes=["LNB2"])
    assert not cast_jobs
    wgv, wuv, wdv = WGB, WUB, WDB

    regs = {}

    def bc_reg(e):
        if "bc" not in regs:
            regs["bc"] = e.alloc_register("bcreg")
            e.reg_mov(regs["bc"], 4095)
        return regs["bc"]

    def bc_reg2(e):
        if "bc2" not in regs:
            regs["bc2"] = e.alloc_register("bcreg2")
            e.reg_mov(regs["bc2"], 2 * NT - 1)
        return regs["bc2"]

    def sp_L(j):
        w = j % NW
        for h in range(2):
            xi = (j % 3) * 2 + h
            P.dma("sp", lambda e, xi=xi, h=h: e.dma_start(out=XSB[xi], in_=XS[(2 * j + h) * 128:(2 * j + h + 1) * 128, :]), ("XSB", xi),
                  reads=XS_ALL, writes=[("XSB", xi)])
        for (Wt, wres, wv) in ((WG[w], ("WG", w), wgv), (WU[w], ("WU", w), wuv), (WD[w], ("WD", w), wdv)):
            P.dma("pool", lambda e, Wt=Wt, wv=wv: e.indirect_dma_start(
                out=(Wt.rearrange("p c f -> p (c f)")), out_offset=None, in_=wv,
                in_offset=bass.IndirectOffsetOnAxis(ap=IDXW[:, j:j + 1], axis=0), bounds_check=bc_reg(e), oob_is_err=False),
                wres, reads=["IDXW", "WCAST"], writes=[wres])

    def sp_T(j):
        x = j % 2
        for h in range(2):
            xi = (j % 3) * 2 + h
            bank = 6 + h
            for c in range(8):
                P.op("pe", lambda e, c=c, xi=xi, bank=bank: e.transpose(psb[bank][:, c * 128:(c + 1) * 128], XSB[xi][:, c:1024:8], ident),
                     reads=[("XSB", xi), "ident"], writes=[PS(bank)])
            if h == 0:
                P.op("act", lambda e, bank=bank, h=h: e.copy(out=XST[x][:, :, h * 128:(h + 1) * 128], in_=psb[bank].rearrange("p (c n) -> p c n", n=128)),
                     reads=[PS(bank)], writes=[("XST", x, h)])
            else:
                P.op("dve", lambda e, bank=bank, h=h: e.tensor_copy(out=XST[x][:, :, h * 128:(h + 1) * 128], in_=psb[bank].rearrange("p (c n) -> p c n", n=128)),
                     reads=[PS(bank)], writes=[("XST", x, h)])

    def sp_U(j):
        w = j % NW
        x = j % 2
        bA, bU = 0, 1
        for (Wt, wres, bank) in ((WG[w], ("WG", w), bA), (WU[w], ("WU", w), bU)):
            for fc in range(2):
                for c in range(8):
                    P.op("pe", lambda e, Wt=Wt, fc=fc, c=c, bank=bank: e.matmul(psf[bank][:, fc * 256:(fc + 1) * 256], lhsT=Wt[:, c, fc:256:2], rhs=XST[x][:, c, :],
                                                                                 start=(c == 0), stop=(c == 7)),
                         reads=[wres, ("XST", x, 0), ("XST", x, 1)], writes=[PS(bank)])
        P.op("act", lambda e: e.activation(out=SLs[x], in_=psf[bA], func=AF.Silu), reads=[PS(bA)], writes=[("SLs", x)])
        P.op("dve", lambda e: e.tensor_tensor(out=HIDT[x], in0=psf[bU], in1=SLs[x], op=ALU.mult), reads=[PS(bU), ("SLs", x)], writes=[("HIDT", x)])

    def sp_D(j):
        w = j % NW
        x = j % 2
        for h in range(2):
            xi = (j % 3) * 2 + h
            yi = x * 2 + h
            gate = XSB[xi][:, 1026:1028].bitcast(F32)
            for half in range(2):
                bank = 2 + 2 * h + half
                for fc in range(2):
                    P.op("pe", lambda e, fc=fc, h=h, half=half, bank=bank: e.matmul(
                        psf[bank], lhsT=HIDT[x][:, fc * 256 + h * 128:fc * 256 + (h + 1) * 128], rhs=WD[w][:, fc, half * 512:(half + 1) * 512],
                        start=(fc == 0), stop=(fc == 1)), reads=[("HIDT", x), ("WD", w)], writes=[PS(bank)])
                if half == 0:
                    P.op("act", lambda e, yi=yi, bank=bank, gate=gate: e.activation(out=YSB[yi][:, 0:512], in_=psf[bank], func=AF.Copy, scale=gate),
                         reads=[PS(bank), ("XSB", xi)], writes=[("YSB", yi)])
                else:
                    P.op("dve", lambda e, yi=yi, bank=bank, gate=gate: e.tensor_scalar(out=YSB[yi][:, 512:1024], in0=psf[bank], scalar1=gate, scalar2=None, op0=ALU.mult),
                         reads=[PS(bank), ("XSB", xi)], writes=[("YSB", yi)])
            P.dma("pool", lambda e, xi=xi, yi=yi: e.indirect_dma_start(
                out=YK, out_offset=bass.IndirectOffsetOnAxis(ap=XSB[xi][:, 1024:1026].bitcast(I32), axis=0), in_=YSB[yi], in_offset=None,
                bounds_check=bc_reg2(e), oob_is_err=False), ("st", "YSB", yi), reads=[("YSB", yi), ("XSB", xi)], writes=[("YKs", j, h)])

    sp_L(0)
    sp_L(1)
    for st_ in range(NS + 1):
        if st_ < NS:
            sp_T(st_)
        if st_ >= 1:
            sp_D(st_ - 1)
        if st_ < NS:
            sp_U(st_)
        if st_ + 2 < NS:
            sp_L(st_ + 2)
    YK_ALL = [("YKs", j, h) for j in range(NS) for h in range(2)]

    P.barrier()
    YO = [XST[0].rearrange("p c n -> p (c n)").bitcast(F32), XST[1].rearrange("p c n -> p (c n)").bitcast(F32)]
    def racc(t, k):
        P.dma("pool", lambda e: e.dma_start(out=R[:, t, :], in_=YK[k * NT + t * 128:k * NT + (t + 1) * 128, :], accum_op=ALU.add), ("RACC", t),
              reads=YK_ALL + [("R", t)], writes=[("R", t)])

    racc(0, 0)
    for t in range(18):
        if t + 1 < 18:
            racc(t + 1, 0)
        racc(t, 1)
    for t in range(18):
        s = t % 2
        layernorm("ln2", R[:, t, :], ("R", t), YO[s], ("YO", s), LNG2, LNB2, ["LNG2", "LNB2"])
        dst = yp[t * 128:(t + 1) * 128, :] if t < 16 else ys[(t - 16) * 128:(t - 15) * 128, :]
        P.dma("sp", lambda e, s=s, dst=dst: e.dma_start(out=dst, in_=YO[s]), ("st", "YO", s), reads=[("YO", s)])

    P.emit()
    return nc


_CACHE = {}
STOP = 99


def kernel(**inp):
    f = lambda a: np.ascontiguousarray(np.asarray(a, dtype=np.float32))
    if "nc" not in _CACHE:
        nc = bass.Bass("TRN2", target_bir_lowering=False)
        build(nc, STOP)
        _CACHE["nc"] = nc
    nc = _CACHE["nc"]
    rep = {}
    for k in ["ln0_g", "ln0_b"]:
        rep[k] = f(inp[k])
    for k in ["w_in", "w_dw", "b_dw", "lnc_g", "lnc_b", "w_cpw", "w_mk", "w_mv", "w_out", "ln1_g", "ln1_b", "w_rg", "b_rg",
              "w_re", "b_re", "w_eg", "w_eu", "w_ed", "ln2_g", "ln2_b"]:
        rep[k] = f(inp[k][0])
    x_prompt = f(inp["x_prompt"]); x_sample = f(inp["x_sample"]); mem_prompt = f(inp["mem_prompt"])
    cK = inp["cache_sba_k"]; cV = inp["cache_sba_v"]
    in_maps = []
    for c in range(NCORES):
        m = dict(rep)
        m["xp"] = x_prompt[c]
        m["xs"] = x_sample[4 * c:4 * c + 4].reshape(256, 1024)
        m["memp"] = mem_prompt[c]
        m["ck"] = f(cK[0, 4 * c:4 * c + 4]).reshape(4, 4096, 512)
        m["cv"] = f(cV[0, 4 * c:4 * c + 4]).reshape(4, 4096, 512)
        m["cconv"] = f(inp["cache_conv"][0, 4 * c:4 * c + 4])
        m["cmk"] = f(inp["cache_mem_k"][0, 4 * c:4 * c + 4]).reshape(4, 256, 256)
        m["cmv"] = f(inp["cache_mem_v"][0, 4 * c:4 * c + 4]).reshape(4, 256, 256)
        in_maps.append(m)
    res = run_bass_kernel_spmd(nc, in_maps, core_ids=list(range(NCORES)))
    rs = res.results
    cat = lambda k: np.stack([np.asarray(r[k], dtype=np.float32) for r in rs], 0)
    y_prompt = cat("yp")
    y_sample = cat("ys").reshape(32, 64, 1024)
    kpo = cat("kp").reshape(1, 8, 2048, 8, 64)
    vpo = cat("vp").reshape(1, 8, 2048, 8, 64)
    cpo = cat("convp").reshape(1, 8, 30, 256)
    mko = cat("mkp").reshape(1, 8, 256, 4, 64)
    mvo = cat("mvp").reshape(1, 8, 256, 4, 64)
    kso = cat("ksn").reshape(1, 32, 64, 8, 64)
    vso = cat("vsn").reshape(1, 32, 64, 8, 64)
    cso = cat("convs").reshape(1, 32, 30, 256)
    return (y_prompt, y_sample, kpo, vpo, cpo, mko, mvo, kso, vso, cso)
```

```python
import numpy as np
from contextlib import ExitStack
import concourse.bass as bass
import concourse.mybir as mybir
from concourse.bass_utils import run_bass_kernel_spmd

F32 = mybir.dt.float32
BF16 = mybir.dt.bfloat16
I32 = mybir.dt.int32
AF = mybir.ActivationFunctionType
ALU = mybir.AluOpType

ENGS = ["pe", "act", "dve", "pool", "sp"]
NCORES = 8
ALPHA = 2.0 ** 0.25
EPS = 1e-5
SCALE = 0.125
NT = 2304
NP = 2048
BIG = 1.0e30


class Prog:
    def __init__(self, nc):
        self.nc = nc
        self.ins = {e: [] for e in ENGS}
        self.last_w = {}
        self.readers = {}
        self.dma_cnt = {}

    def _tok_deps(self, reads, writes):
        deps = []
        for r in reads:
            t = self.last_w.get(r)
            if t is not None:
                deps.append(t)
        for w in writes:
            t = self.last_w.get(w)
            if t is not None:
                deps.append(t)
            deps.extend(self.readers.get(w, ()))
        return deps

    def _add_waits(self, eng, deps):
        waits = []
        for t in deps:
            if t[0] == "dma":
                waits.append((("dma", t[1]), t[2] * 16))
            else:
                _, e2, idx = t
                if e2 == eng and eng == "pe":
                    continue
                self.ins[e2][idx]["flag"] = True
                waits.append((("eng", e2), idx))
        return waits

    def op(self, eng, fn, reads=(), writes=()):
        deps = self._tok_deps(reads, writes)
        waits = self._add_waits(eng, deps)
        idx = len(self.ins[eng])
        self.ins[eng].append(dict(fn=fn, waits=waits, flag=False, dma=None))
        tok = ("eng", eng, idx)
        for r in reads:
            lst = self.readers.setdefault(r, [])
            lst[:] = [t for t in lst if not (t[0] == "eng" and t[1] == eng)]
            lst.append(tok)
        for w in writes:
            self.last_w[w] = tok
            self.readers[w] = []
        return tok

    def dma(self, eng, fn, semkey, reads=(), writes=()):
        deps = self._tok_deps(reads, writes)
        waits = self._add_waits(eng, deps)
        cnt = self.dma_cnt.get(semkey, 0) + 1
        self.dma_cnt[semkey] = cnt
        self.ins[eng].append(dict(fn=fn, waits=waits, flag=False, dma=semkey))
        tok = ("dma", semkey, cnt)
        for r in reads:
            self.readers.setdefault(r, []).append(tok)
        for w in writes:
            self.last_w[w] = tok
            self.readers[w] = []
        return tok

    def barrier(self):
        lasts = {}
        for e in ENGS:
            for idx in range(len(self.ins[e]) - 1, -1, -1):
                ins = self.ins[e][idx]
                if ins["fn"] is not None and ins["dma"] is None:
                    lasts[e] = idx
                    break
        dmas = dict(self.dma_cnt)
        for e in ENGS:
            waits = []
            for e2, idx in lasts.items():
                if e2 != e:
                    self.ins[e2][idx]["flag"] = True
                    waits.append((("eng", e2), idx))
            for k, c in dmas.items():
                waits.append((("dma", k), c * 16))
            self.ins[e].append(dict(fn=None, waits=waits, flag=False, dma=None))

    def emit(self):
        nc = self.nc
        with ExitStack() as st:
            sems = {}
            for e in ENGS:
                sems[("eng", e)] = st.enter_context(nc.semaphore("s_" + e))
            for i, k in enumerate(self.dma_cnt):
                sems[("dma", k)] = st.enter_context(nc.semaphore("d%d" % i))
            cnts = {}
            for e in ENGS:
                c = 0
                arr = []
                for ins in self.ins[e]:
                    if ins["flag"]:
                        c += 1
                    arr.append(c)
                cnts[e] = arr
            block = st.enter_context(nc.Block())
            final_dma = [(sems[("dma", k)], c * 16) for k, c in self.dma_cnt.items()]

            def run(e, engobj):
                waited = {}
                for ins in self.ins[e]:
                    for key, v in ins["waits"]:
                        if key[0] == "eng":
                            v = cnts[key[1]][v]
                        if waited.get(key, 0) >= v:
                            continue
                        waited[key] = v
                        engobj.wait_ge(sems[key], v)
                    if ins["fn"] is None:
                        continue
                    r = ins["fn"](engobj)
                    if ins["dma"] is not None:
                        r.then_inc(sems[("dma", ins["dma"])], 16)
                    elif ins["flag"]:
                        r.then_inc(sems[("eng", e)], 1)
                if e == "sp":
                    for s, v in final_dma:
                        engobj.wait_ge(s, v)

            @block.tensor
            def _(eng):
                run("pe", eng)

            @block.scalar
            def _(eng):
                run("act", eng)

            @block.vector
            def _(eng):
                run("dve", eng)

            @block.gpsimd
            def _(eng):
                run("pool", eng)

            @block.sync
            def _(eng):
                run("sp", eng)


def build(nc, stop_after=99):
    P = Prog(nc)

    def din(name, shape):
        return nc.dram_tensor(name, list(shape), F32, kind="ExternalInput").ap()

    def dout(name, shape):
        return nc.dram_tensor(name, list(shape), F32, kind="ExternalOutput").ap()

    xp = din("xp", [NP, 1024]); xs = din("xs", [256, 1024]); memp = din("memp", [256, 1024])
    ck = din("ck", [4, 4096, 512]); cv = din("cv", [4, 4096, 512])
    cconv = din("cconv", [4, 30, 256]); cmk = din("cmk", [4, 256, 256]); cmv = din("cmv", [4, 256, 256])
    ln0_g = din("ln0_g", [1024]); ln0_b = din("ln0_b", [1024])
    w_in = din("w_in", [1024, 2304]); w_dw = din("w_dw", [31, 256]); b_dw = din("b_dw", [256])
    lnc_g = din("lnc_g", [256]); lnc_b = din("lnc_b", [256]); w_cpw = din("w_cpw", [256, 256])
    w_mk = din("w_mk", [1024, 256]); w_mv = din("w_mv", [1024, 256]); w_out = din("w_out", [1024, 1024])
    ln1_g = din("ln1_g", [1024]); ln1_b = din("ln1_b", [1024])
    w_rg = din("w_rg", [1024, 4]); b_rg = din("b_rg", [4]); w_re = din("w_re", [1024, 32]); b_re = din("b_re", [32])
    w_eg = din("w_eg", [32, 1024, 256]); w_eu = din("w_eu", [32, 1024, 256]); w_ed = din("w_ed", [32, 256, 1024])
    ln2_g = din("ln2_g", [1024]); ln2_b = din("ln2_b", [1024])

    yp = dout("yp", [NP, 1024]); ys = dout("ys", [256, 1024])
    kp = dout("kp", [NP, 512]); vp = dout("vp", [NP, 512]); convp = dout("convp", [30, 256])
    mkp = dout("mkp", [256, 256]); mvp = dout("mvp", [256, 256])
    ksn = dout("ksn", [256, 512]); vsn = dout("vsn", [256, 512]); convs = dout("convs", [4, 30, 256])
    XS = nc.dram_tensor("XS_scr", [50 * 256, 1028], BF16, kind="Internal").ap()
    YK = nc.dram_tensor("YK_scr", [2 * NT, 1024], F32, kind="Internal").ap()
    XNS = nc.dram_tensor("XN_scr", [NT, 1024], F32, kind="Internal").ap()
    WGB = nc.dram_tensor("WGB_scr", [4096, 2048], BF16, kind="Internal").ap()
    WUB = nc.dram_tensor("WUB_scr", [4096, 2048], BF16, kind="Internal").ap()
    WDB = nc.dram_tensor("WDB_scr", [4096, 2048], BF16, kind="Internal").ap()

    class Arena:
        def __init__(self, base, size):
            self.base, self.size, self.off = base, size, 0

        def t(self, name, shape, dtype):
            el = 2 if dtype == BF16 else 4
            n = 1
            for s in shape[1:]:
                n *= s
            sz = (n * el + 31) // 32 * 32
            assert self.off + sz <= self.size, (name, self.off, sz, self.size)
            h = nc.alloc_sbuf_tensor_at(name, list(shape), dtype, offset=self.base + self.off)
            self.off += sz
            return h.ap()

        def reset(self):
            self.off = 0

    BASE = 16640
    aXT = Arena(BASE, 36864)
    aMIX = Arena(BASE + 36864, 36864)
    CSZ = 11776
    aCONST = Arena(BASE + 73728, CSZ)
    aR = Arena(BASE + 73728 + CSZ, 73728)
    aFREE = Arena(BASE + 73728 + CSZ + 73728, 228640 - (BASE + 73728 + CSZ + 73728))

    XT = aXT.t("XT", [128, 8, NT], BF16)
    MIXT = aMIX.t("MIXT", [128, 8, NT], BF16)
    R = aR.t("R", [128, 18, 1024], F32)
    GATE = aCONST.t("GATE", [128, 18, 32], F32)
    ident = aCONST.t("ident", [128, 128], BF16)
    identF = aCONST.t("identF", [128, 128], F32)
    Uinc = aCONST.t("Uinc", [128, 128], BF16)
    ones = aCONST.t("ones", [128, 128], BF16)
    onesF = aCONST.t("onesF", [128, 128], F32)
    tri = aCONST.t("tri", [128, 128], F32)
    tri8 = aCONST.t("tri8", [128, 8, 64], F32)
    wdw = aCONST.t("wdw", [128, 2, 31], F32)
    cpar = aCONST.t("cpar", [128, 3, 2], F32)
    W_CPW = aCONST.t("W_CPW", [128, 2, 256], BF16)
    W_R = aCONST.t("W_R", [128, 8, 36], BF16)
    RB = aCONST.t("RB", [128, 36], F32)
    small = aCONST.t("small", [128, 64], F32)
    small2 = aCONST.t("small2", [128, 64], F32)
    IDXW = aCONST.t("IDXW", [128, 50], I32)

    ps = [nc.alloc_psum_tensor("ps%d" % i, [128, 512], F32) for i in range(8)]
    psf = [p.ap() for p in ps]
    psb = [p.bitcast(BF16).ap() for p in ps]

    def PS(i):
        return ("ps", i)

    def pool(fn, reads=(), writes=()):
        return P.op("pool", fn, reads, writes)

    pool(lambda e: e.memset(ident, 0.0), writes=["ident"])
    pool(lambda e: e.affine_select(out=ident, in_=ident, pattern=[[-1, 128]], compare_op=ALU.not_equal,
                                   fill=1.0, base=0, channel_multiplier=1), reads=["ident"], writes=["ident"])
    pool(lambda e: e.memset(identF, 0.0), writes=["identF"])
    pool(lambda e: e.affine_select(out=identF, in_=identF, pattern=[[-1, 128]], compare_op=ALU.not_equal,
                                   fill=1.0, base=0, channel_multiplier=1), reads=["identF"], writes=["identF"])
    aR.reset()
    W_IN = aR.t("W_IN", [128, 8, 2304], BF16)
    FILLR = aR.t("FILLR", [128, 1028], BF16)
    for c in range(8):
        P.dma("pool", lambda e, c=c: e.dma_start(out=W_IN[:, c, :], in_=w_in[c * 128:(c + 1) * 128, :]), "W_IN", writes=["W_IN"])
    WIN_ALL = [("W_IN", c) for c in range(8)]

    pool(lambda e: e.memset(ones, 1.0), writes=["ones"])
    pool(lambda e: e.memset(onesF, 1.0), writes=["onesF"])
    pool(lambda e: e.memset(Uinc, 1.0), writes=["Uinc"])
    pool(lambda e: e.affine_select(out=Uinc, in_=Uinc, pattern=[[-1, 128]], compare_op=ALU.is_ge,
                                   fill=0.0, base=0, channel_multiplier=1), reads=["Uinc"], writes=["Uinc"])
    pool(lambda e: e.memset(tri, 1.0), writes=["tri"])
    pool(lambda e: e.affine_select(out=tri, in_=tri, pattern=[[1, 128]], compare_op=ALU.is_gt,
                                   fill=0.0, base=0, channel_multiplier=-1), reads=["tri"], writes=["tri"])
    pool(lambda e: e.memset(tri8, 1.0), writes=["tri8"])
    pool(lambda e: e.affine_select(out=tri8[0:64], in_=tri8[0:64], pattern=[[0, 8], [1, 64]], compare_op=ALU.is_gt,
                                   fill=0.0, base=0, channel_multiplier=-1), reads=["tri8"], writes=["tri8"])
    P.dma("pool", lambda e: e.dma_start(out=tri8[64:128], in_=tri8[0:64]), "tri8", reads=["tri8"], writes=["tri8"])

    def xrows(t):
        return xp[t * 128:(t + 1) * 128, :] if t < 16 else xs[(t - 16) * 128:(t - 15) * 128, :]

    def tcols(t):
        return slice(t * 128, (t + 1) * 128)

    ln_ctr = [0]

    def ln_A(src, src_res):
        k = ln_ctr[0] % 2
        ln_ctr[0] += 1
        sm = small if k == 0 else small2
        smr = ("small", k)
        st6 = sm[:, 0:12].rearrange("p (a b) -> p a b", b=6)
        mv = sm[:, 12:14]
        rs = sm[:, 14:15]
        P.op("dve", lambda e: e.bn_stats(out=st6[:, 0, :], in_=src[:, 0:512]), reads=[src_res], writes=[smr])
        P.op("dve", lambda e: e.bn_stats(out=st6[:, 1, :], in_=src[:, 512:1024]), reads=[src_res], writes=[smr])
        P.op("dve", lambda e: e.bn_aggr(out=mv, in_=st6), reads=[smr], writes=[smr])
        P.op("dve", lambda e: e.tensor_scalar(out=rs, in0=mv[:, 1:2], scalar1=EPS, scalar2=None, op0=ALU.add),
             reads=[smr], writes=[smr])
        P.op("act", lambda e: e.activation(out=rs, in_=rs, func=AF.Ln), reads=[smr], writes=[smr])
        P.op("act", lambda e: e.activation(out=rs, in_=rs, func=AF.Exp, scale=-0.5), reads=[smr], writes=[smr])
        return (smr, mv, rs)

    def ln_B(ctx, src, src_res, dst, dst_res, G, B, gres):
        smr, mv, rs = ctx
        P.op("dve", lambda e: e.scalar_tensor_tensor(out=src, in0=src, scalar=mv[:, 0:1], in1=G, op0=ALU.subtract, op1=ALU.mult),
             reads=[smr, src_res] + gres, writes=[src_res])
        P.op("dve", lambda e: e.scalar_tensor_tensor(out=dst, in0=src, scalar=rs, in1=B, op0=ALU.mult, op1=ALU.add),
             reads=[smr, src_res] + gres, writes=[dst_res])

    def layernorm(tag, src, src_res, dst, dst_res, G, B, gres, out_scale=None):
        ln_B(ln_A(src, src_res), src, src_res, dst, dst_res, G, B, gres)

    aFREE.reset()
    aMIX.reset()
    XIN = [aMIX.t("XIN%d" % i, [128, 1024], F32) for i in range(3)]
    XNB = [aMIX.t("XNB%d" % i, [128, 1024], BF16) for i in range(2)]
    LNG = aMIX.t("LNG", [128, 1024], F32)
    LNB = aMIX.t("LNB", [128, 1024], F32)
    P.dma("sp", lambda e: e.dma_start(out=LNG, in_=ln0_g.partition_broadcast(128)), "LNG", writes=["LNG"])
    P.dma("sp", lambda e: e.dma_start(out=LNB, in_=ln0_b.partition_broadcast(128)), "LNB", writes=["LNB"])
    def transpose_to_XT(t, srcb, src_res, bank):
        for c in range(8):
            P.op("pe", lambda e, c=c: e.transpose(psb[bank][:, c * 128:(c + 1) * 128], srcb[:, c * 128:(c + 1) * 128], ident),
                 reads=[src_res, "ident"], writes=[PS(bank)])
        P.op("act", lambda e: e.copy(out=XT[:, :, tcols(t)], in_=psb[bank].rearrange("p (c n) -> p c n", n=128)),
             reads=[PS(bank)], writes=[("XT", t)])

    KST = [aMIX.t("KST%d" % i, [128, 512], F32) for i in range(2)]
    VST = [aMIX.t("VST%d" % i, [128, 512], F32) for i in range(2)]
    aFREE.reset()
    V_ALL = aFREE.t("V_ALL", [128, 18, 512], BF16)
    aA_base = aFREE.off

    def p1_load(t):
        s3 = t % 3
        P.dma("sp", lambda e: e.dma_start(out=XIN[s3], in_=xrows(t)), ("XIN", s3), writes=[("XIN", s3)])

    def p2a_mm(t):
        s = t % 2
        for (which, col0, bank) in (("k", 512, 0 + s), ("v", 1024, 2 + s)):
            for c in range(8):
                P.op("pe", lambda e, c=c, col0=col0, bank=bank: e.matmul(
                    psf[bank], lhsT=XT[:, c, tcols(t)], rhs=W_IN[:, c, col0:col0 + 512], start=(c == 0), stop=(c == 7)),
                    reads=[("XT", t), "W_IN"], writes=[PS(bank)])

    def p2a_evac(t):
        s = t % 2
        P.op("act", lambda e: e.copy(out=KST[s], in_=psf[0 + s]), reads=[PS(0 + s)], writes=[("KST", s)])
        P.op("act", lambda e: e.copy(out=VST[s], in_=psf[2 + s]), reads=[PS(2 + s)], writes=[("VST", s)])
        P.op("act", lambda e: e.copy(out=V_ALL[:, t, :], in_=psf[2 + s]), reads=[PS(2 + s)], writes=[("V_ALL", t)])
        kdst = kp[t * 128:(t + 1) * 128, :] if t < 16 else ksn[(t - 16) * 128:(t - 15) * 128, :]
        vdst = vp[t * 128:(t + 1) * 128, :] if t < 16 else vsn[(t - 16) * 128:(t - 15) * 128, :]
        P.dma("pool", lambda e: e.dma_start(out=kdst, in_=KST[s]), ("st", "KST", s), reads=[("KST", s)])
        P.dma("pool", lambda e: e.dma_start(out=vdst, in_=VST[s]), ("st", "VST", s), reads=[("VST", s)])

    p1_load(0)
    p1_load(1)
    WDWT = aCONST.t("WDWT", [31, 256], F32)
    P.dma("sp", lambda e: e.dma_start(out=WDWT, in_=w_dw), "WDWT", writes=["WDWT"])
    for cc in range(2):
        P.op("pe", lambda e, cc=cc: e.transpose(psf[0][:, cc * 32:cc * 32 + 31], WDWT[:, cc * 128:(cc + 1) * 128], identF[0:31, 0:31]),
             reads=["WDWT", "identF"], writes=[PS(0)])
    P.op("act", lambda e: e.copy(out=wdw, in_=psf[0][:, 0:64].rearrange("p (c n) -> p c n", n=32)[:, :, 0:31]),
         reads=[PS(0)], writes=["wdw"])
    for i, v in enumerate([b_dw, lnc_g, lnc_b]):
        P.dma("sp", lambda e, i=i, v=v: e.dma_start(out=cpar[:, i, :], in_=v.rearrange("(c p) -> p c", p=128),
                                                     allow_slow_non_contiguous=True), "cpar", writes=["cpar"])
    P.dma("pool", lambda e: e.dma_start(out=W_CPW, in_=w_cpw.rearrange("(c p) f -> p c f", p=128)), "W_CPW", writes=["W_CPW"])
    P.dma("pool", lambda e: e.dma_start(out=W_R[:, :, 0:4], in_=w_rg.rearrange("(c p) f -> p c f", p=128)), "W_R", writes=["W_R"])
    P.dma("pool", lambda e: e.dma_start(out=W_R[:, :, 4:36], in_=w_re.rearrange("(c p) f -> p c f", p=128)), "W_R", writes=["W_R"])
    P.dma("sp", lambda e: e.dma_start(out=RB[:, 0:4], in_=b_rg.partition_broadcast(128)), "RB", writes=["RB"])
    P.dma("sp", lambda e: e.dma_start(out=RB[:, 4:36], in_=b_re.partition_broadcast(128)), "RB", writes=["RB"])

    for t in range(20):
        if t < 18:
            s = t % 2
            s3 = t % 3
            layernorm("ln0", XIN[s3], ("XIN", s3), XIN[s3], ("XIN", s3), LNG, LNB, ["LNG", "LNB"])
        if t >= 2:
            p2a_evac(t - 2)
        if t < 18:
            P.op("act", lambda e, s=s, s3=s3: e.copy(out=XNB[s], in_=XIN[s3]), reads=[("XIN", s3)], writes=[("XNB", s)])
            P.dma("pool", lambda e, t=t, s3=s3: e.dma_start(out=XNS[t * 128:(t + 1) * 128, :], in_=XIN[s3]), ("st", "XIN", s3), reads=[("XIN", s3)], writes=[("XNS", t)])
            transpose_to_XT(t, XNB[s], ("XNB", s), 6 + s)
            if t + 2 < 18:
                p1_load(t + 2)
        if 1 <= t < 19:
            p2a_mm(t - 1)
    P.op("pool", lambda e: e.memset(FILLR, 0.0), writes=["FILLR"])
    P.op("pool", lambda e: e.memset(FILLR[:, 1024:1026].bitcast(I32), 1 << 24), reads=["FILLR"], writes=["FILLR"])
    for a_ in range(10):
        P.dma("sp", lambda e, a_=a_: e.dma_start(out=XS[a_ * 1280:(a_ + 1) * 1280, :].rearrange("(a p) f -> p a f", p=128),
                                                 in_=FILLR.unsqueeze(1).to_broadcast([128, 10, 1028])), "XSfill", reads=["FILLR"], writes=["XSfill"])
    XT_ALL = [("XT", t) for t in range(18)]
    if stop_after <= 2:
        P.emit(); return nc

    QT = aR.t("QT", [128, 4, NP], BF16)
    KT = aR.t("KT", [128, 4, NP], BF16)
    QS = aFREE.t("QS", [128, 4, 256], BF16)
    KS = aFREE.t("KS", [128, 4, 256], BF16)
    aA_base = aFREE.off
    GW = 2078 + 4 * 94
    GLU = aFREE.t("GLU", [128, 2, GW], F32)
    MQT = aFREE.t("MQT", [128, 2, NT], BF16)
    SIG = [aMIX.t("SIG%d" % i, [128, 512], F32) for i in range(2)]
    blocks = [(0, 512), (512, 1024), (1024, 1536), (1536, 2048), (2048, 2304)]
    bankrr = [0]

    def inproj_fm(j, n0, n1, bank):
        N = n1 - n0
        for c in range(8):
            P.op("pe", lambda e, c=c: e.matmul(psf[bank][:, 0:N], lhsT=W_IN[:, c, j * 128:(j + 1) * 128], rhs=XT[:, c, n0:n1],
                                               start=(c == 0), stop=(c == 7)),
                 reads=["W_IN"] + [("XT", t) for t in range(n0 // 128, n1 // 128)], writes=[PS(bank)])

    def sample_glu_view(cc):
        return GLU[:, cc, 2078:2078 + 376].rearrange("p (s w) -> p s w", w=94)[:, :, 30:94]

    for bi, (n0, n1) in enumerate(blocks):
        N = n1 - n0
        for j in list(range(8)) + [16, 17]:
            bank = bankrr[0] % 4
            bankrr[0] += 1
            inproj_fm(j, n0, n1, bank)
            if j < 4:
                dst, dres = (QT[:, j, n0:n1], ("QT", j, bi)) if bi < 4 else (QS[:, j, :], ("QS", j))
            elif j < 8:
                dst, dres = (KT[:, j - 4, n0:n1], ("KT", j - 4, bi)) if bi < 4 else (KS[:, j - 4, :], ("KS", j - 4))
            else:
                dst, dres = MQT[:, j - 16, n0:n1], ("MQT", j - 16, bi)
            if (bankrr[0] % 2) == 0:
                P.op("act", lambda e, dst=dst, bank=bank, N=N: e.copy(out=dst, in_=psf[bank][:, 0:N]), reads=[PS(bank)], writes=[dres])
            else:
                P.op("dve", lambda e, dst=dst, bank=bank, N=N: e.tensor_copy(out=dst, in_=psf[bank][:, 0:N]), reads=[PS(bank)], writes=[dres])
        for cc in range(2):
            b1, b2 = 4 + 2 * (cc % 2), 5 + 2 * (cc % 2)
            inproj_fm(12 + cc, n0, n1, b1)
            inproj_fm(14 + cc, n0, n1, b2)
            sg = SIG[cc]
            P.op("act", lambda e, sg=sg, b2=b2, N=N: e.activation(out=sg[:, 0:N], in_=psf[b2][:, 0:N], func=AF.Sigmoid),
                 reads=[PS(b2)], writes=[("SIG", cc)])
            if bi < 4:
                P.op("dve", lambda e, sg=sg, b1=b1, cc=cc, n0=n0, N=N: e.tensor_tensor(
                    out=GLU[:, cc, 30 + n0:30 + n0 + N], in0=psf[b1][:, 0:N], in1=sg[:, 0:N], op=ALU.mult),
                    reads=[PS(b1), ("SIG", cc)], writes=[("GLU", cc)])
            else:
                P.op("dve", lambda e, sg=sg, b1=b1, cc=cc: e.tensor_tensor(
                    out=sample_glu_view(cc), in0=psf[b1][:, 0:256].rearrange("p (s w) -> p s w", w=64),
                    in1=sg[:, 0:256].rearrange("p (s w) -> p s w", w=64), op=ALU.mult),
                    reads=[PS(b1), ("SIG", cc)], writes=[("GLU", cc)])
    if stop_after <= 3:
        P.emit(); return nc

    P.barrier()
    aMIX.reset()
    DIAG = aMIX.t("DIAG", [128, 62, 128], BF16)
    assert aMIX.off <= 18432, aMIX.off
    for cc in range(2):
        for j in range(31):
            P.op("dve", lambda e, cc=cc, j=j: e.tensor_scalar(out=DIAG[:, cc * 31 + j, :], in0=identF, scalar1=wdw[:, cc, j:j + 1],
                                                              scalar2=None, op0=ALU.mult), reads=["identF", "wdw"], writes=["DIAG"])
    aW = Arena(aR.base, 36864)
    MEMB = aW.t("MEMB", [128, 2, 1024], BF16)
    MEMT = aW.t("MEMT", [128, 8, 256], BF16)
    W_MK = aW.t("W_MK", [128, 8, 256], BF16)
    W_MV = aW.t("W_MV", [128, 8, 256], BF16)
    MKT = aW.t("MKT", [128, 2, 256], BF16)
    MV = aW.t("MV", [128, 2, 256], BF16)
    MST = [aW.t("MST%d" % i, [128, 256], F32) for i in range(2)]
    PT = [aW.t("PT%d" % i, [128, 512], BF16) for i in range(2)]
    RDEN = aW.t("RDEN", [128, 512], F32)
    P.dma("pool", lambda e: e.dma_start(out=MEMB, in_=memp.rearrange("(m p) d -> p m d", p=128)), "MEMB", writes=["MEMB"])
    P.dma("pool", lambda e: e.dma_start(out=W_MK, in_=w_mk.rearrange("(c p) f -> p c f", p=128)), "W_MK", writes=["W_MK"])
    P.dma("pool", lambda e: e.dma_start(out=W_MV, in_=w_mv.rearrange("(c p) f -> p c f", p=128)), "W_MV", writes=["W_MV"])
    for mt in range(2):
        for c in range(8):
            P.op("pe", lambda e, mt=mt, c=c: e.transpose(psb[6][:, c * 128:(c + 1) * 128], MEMB[:, mt, c * 128:(c + 1) * 128], ident),
                 reads=["MEMB", "ident"], writes=[PS(6)])
        P.op("act", lambda e, mt=mt: e.copy(out=MEMT[:, :, mt * 128:(mt + 1) * 128], in_=psb[6].rearrange("p (c n) -> p c n", n=128)),
             reads=[PS(6)], writes=["MEMT"])
    for wi, (W, wres, dstout) in enumerate(((W_MK, "W_MK", mkp), (W_MV, "W_MV", mvp))):
        for mt in range(2):
            bank = (wi * 2 + mt) % 4
            for c in range(8):
                P.op("pe", lambda e, W=W, mt=mt, c=c, bank=bank: e.matmul(psf[bank][:, 0:256], lhsT=MEMT[:, c, mt * 128:(mt + 1) * 128],
                                                                           rhs=W[:, c, :], start=(c == 0), stop=(c == 7)),
                     reads=["MEMT", wres], writes=[PS(bank)])
            s = mt
            P.op("act", lambda e, s=s, bank=bank: e.copy(out=MST[s], in_=psf[bank][:, 0:256]), reads=[PS(bank)], writes=[("MST", s)])
            if wi == 1:
                P.op("pool", lambda e, mt=mt, s=s: e.tensor_copy(out=MV[:, mt, :], in_=MST[s]), reads=[("MST", s)], writes=["MV"])
            P.dma("sp", lambda e, s=s, dstout=dstout, mt=mt: e.dma_start(out=dstout[mt * 128:(mt + 1) * 128, :], in_=MST[s]),
                  ("st", "MST", s), reads=[("MST", s)])
    for fc in range(2):
        bank = 4 + fc
        for c in range(8):
            P.op("pe", lambda e, fc=fc, c=c, bank=bank: e.matmul(psf[bank][:, 0:256], lhsT=W_MK[:, c, fc * 128:(fc + 1) * 128],
                                                                  rhs=MEMT[:, c, :], start=(c == 0), stop=(c == 7)),
                 reads=["MEMT", "W_MK"], writes=[PS(bank)])
        P.op("act", lambda e, fc=fc, bank=bank: e.copy(out=MKT[:, fc, :], in_=psf[bank][:, 0:256]), reads=[PS(bank)], writes=["MKT"])

    def mem_attn(mkt, mkt_res, mvv, mv_res, n0, N, mq_res):
        for fc in range(2):
            bO, bD = 4 + fc, 6 + fc
            for hh in range(2):
                h = 2 * fc + hh
                pr = slice(64 * hh, 64 * hh + 64)
                for mt in range(2):
                    bS = mt
                    P.op("pe", lambda e, mt=mt, bS=bS, pr=pr, fc=fc: e.matmul(psf[bS][:, 0:N], lhsT=mkt[pr, fc, mt * 128:(mt + 1) * 128],
                                                                       rhs=MQT[pr, fc, n0:n0 + N], start=True, stop=True),
                         reads=[mkt_res] + mq_res, writes=[PS(bS)])
                    P.op("act", lambda e, mt=mt, bS=bS: e.activation(out=PT[mt][:, 0:N], in_=psf[bS][:, 0:N], func=AF.Exp, scale=SCALE),
                         reads=[PS(bS)], writes=[("PT", mt)])
                for mt in range(2):
                    P.op("pe", lambda e, mt=mt, pr=pr, h=h, bO=bO: e.matmul(psf[bO][pr, 0:N], lhsT=mvv[:, mt, h * 64:(h + 1) * 64], rhs=PT[mt][:, 0:N],
                                                                     start=(mt == 0), stop=(mt == 1)),
                         reads=[mv_res, ("PT", mt)], writes=[PS(bO)])
                for mt in range(2):
                    P.op("pe", lambda e, mt=mt, pr=pr, bD=bD: e.matmul(psf[bD][pr, 0:N], lhsT=ones[:, 0:64], rhs=PT[mt][:, 0:N],
                                                                start=(mt == 0), stop=(mt == 1)),
                         reads=["ones", ("PT", mt)], writes=[PS(bD)])
            P.op("act", lambda e, bD=bD: e.activation(out=RDEN[:, 0:N], in_=psf[bD][:, 0:N], func=AF.Ln), reads=[PS(bD)], writes=["RDEN"])
            P.op("act", lambda e: e.activation(out=RDEN[:, 0:N], in_=RDEN[:, 0:N], func=AF.Exp, scale=-1.0), reads=["RDEN"], writes=["RDEN"])
            P.op("dve", lambda e, bO=bO, fc=fc: e.tensor_tensor(out=MIXT[:, 6 + fc, n0:n0 + N], in0=psf[bO][:, 0:N], in1=RDEN[:, 0:N], op=ALU.mult),
                 reads=[PS(bO), "RDEN"], writes=[("MIXT", 6 + fc, n0)])

    for bi in range(4):
        mem_attn(MKT, "MKT", MV, "MV", bi * 512, 512, [("MQT", 0, bi), ("MQT", 1, bi)])
    if stop_after <= 3.2:
        P.emit(); return nc
    CMK = [aW.t("CMK%d" % i, [128, 2, 256], BF16) for i in range(2)]
    CMV = [aW.t("CMV%d" % i, [128, 2, 256], BF16) for i in range(2)]
    CMKT = [aW.t("CMKT%d" % i, [128, 2, 256], BF16) for i in range(2)]
    for s in range(4):
        sl = s % 2
        P.dma("pool", lambda e, s=s, sl=sl: e.dma_start(out=CMK[sl], in_=cmk[s].rearrange("(m p) f -> p m f", p=128)), ("CMK", sl), writes=[("CMK", sl)])
        P.dma("pool", lambda e, s=s, sl=sl: e.dma_start(out=CMV[sl], in_=cmv[s].rearrange("(m p) f -> p m f", p=128)), ("CMV", sl), writes=[("CMV", sl)])
        for mt in range(2):
            for fc in range(2):
                P.op("pe", lambda e, mt=mt, fc=fc, sl=sl: e.transpose(psb[3][:, (mt * 2 + fc) * 128:(mt * 2 + fc + 1) * 128],
                                                                      CMK[sl][:, mt, fc * 128:(fc + 1) * 128], ident),
                     reads=[("CMK", sl), "ident"], writes=[PS(3)])
        P.op("act", lambda e, sl=sl: e.copy(out=CMKT[sl].rearrange("p f (m n) -> p m f n", n=128),
                                            in_=psb[3][:, 0:512].rearrange("p (m f n) -> p m f n", f=2, n=128)),
             reads=[PS(3)], writes=[("CMKT", sl)])
        mem_attn(CMKT[sl], ("CMKT", sl), CMV[sl], ("CMV", sl), 2048 + 64 * s, 64, [("MQT", 0, 4), ("MQT", 1, 4)])

    if stop_after <= 3.4:
        P.emit(); return nc
    P.barrier()
    aW.reset()
    GLUB = aW.t("GLUB", [128, 2, GW], BF16)
    CT = aW.t("CT", [30, 256], F32)
    CST = [aW.t("CST%d" % i, [30, 256], F32) for i in range(2)]
    DW = [aW.t("DW%d" % i, [128, 512], F32) for i in range(4)]
    SQ = [aW.t("SQ%d" % i, [128, 512], F32) for i in range(2)]
    MEANT = aW.t("MEANT", [128, 512], F32)
    RSTD = aW.t("RSTD", [128, 512], F32)
    SU = [aW.t("SU%d" % i, [128, 512], BF16) for i in range(4)]
    cblk = [0]
    segs = [(0, 2048)] + [(2078 + 94 * s, 64) for s in range(4)]
    P.op("pool", lambda e: e.memset(GLU[:, :, 0:30], 0.0), writes=[("GLU", 0), ("GLU", 1)])
    for s in range(4):
        P.dma("sp", lambda e, s=s: e.dma_start(out=CT, in_=cconv[s]), "CT", writes=["CT"])
        for cc in range(2):
            P.op("pe", lambda e, cc=cc: e.transpose(psf[0][:, cc * 32:cc * 32 + 30], CT[:, cc * 128:(cc + 1) * 128], identF[0:30, 0:30]),
                 reads=["CT", "identF"], writes=[PS(0)])
        P.op("act", lambda e, s=s: e.copy(out=GLU[:, :, 2078 + 94 * s:2078 + 94 * s + 30],
                                          in_=psf[0][:, 0:64].rearrange("p (c n) -> p c n", n=32)[:, :, 0:30]),
             reads=[PS(0)], writes=[("GLU", 0), ("GLU", 1)])
    P.op("act", lambda e: e.copy(out=GLUB[:, 0, :], in_=GLU[:, 0, :]), reads=[("GLU", 0)], writes=[("GLUB", 0)])
    P.op("pool", lambda e: e.tensor_copy(out=GLUB[:, 1, :], in_=GLU[:, 1, :]), reads=[("GLU", 1)], writes=[("GLUB", 1)])
    for si, (gb, ln) in enumerate(segs):
        sl = si % 2
        for cc in range(2):
            P.op("pe", lambda e, cc=cc, gb=gb, ln=ln: e.transpose(psf[1][0:30, cc * 128:(cc + 1) * 128], GLU[:, cc, gb + ln:gb + ln + 30], identF),
                 reads=[("GLU", cc), "identF"], writes=[PS(1)])
        P.op("act", lambda e, sl=sl: e.copy(out=CST[sl], in_=psf[1][0:30, 0:256]), reads=[PS(1)], writes=[("CST", sl)])
        dst = convp if si == 0 else convs[si - 1]
        P.dma("sp", lambda e, sl=sl, dst=dst: e.dma_start(out=dst, in_=CST[sl]), ("st", "CST", sl), reads=[("CST", sl)])

    if stop_after <= 3.6:
        P.emit(); return nc

    def conv_s1(gb, t0, N, tok0):
        pb = cblk[0] % 2
        cblk[0] += 1
        DWl = [DW[2 * pb], DW[2 * pb + 1]]
        SUl = [SU[2 * pb], SU[2 * pb + 1]]
        for cc in range(2):
            dw = DWl[cc]
            for j in range(31):
                P.op("pe", lambda e, cc=cc, j=j: e.matmul(psf[cc][:, 0:N], lhsT=DIAG[:, cc * 31 + j, :], rhs=GLUB[:, cc, gb + t0 + j:gb + t0 + j + N],
                                                          start=(j == 0), stop=(j == 30)), reads=["DIAG", ("GLUB", cc)], writes=[PS(cc)])
            P.op("act", lambda e, cc=cc, dw=dw: e.activation(out=dw[:, 0:N], in_=psf[cc][:, 0:N], func=AF.Identity, bias=cpar[:, 0, cc:cc + 1]),
                 reads=[PS(cc), "cpar"], writes=[("DW", cc, pb)])
            P.op("act", lambda e, cc=cc, dw=dw: e.activation(out=SQ[cc][:, 0:N], in_=dw[:, 0:N], func=AF.Square),
                 reads=[("DW", cc, pb)], writes=[("SQ", cc)])
        return (pb, DWl, SUl)

    def conv_s2(ctx, gb, t0, N, tok0):
        pb, DWl, SUl = ctx
        for cc in range(2):
            P.op("pe", lambda e, cc=cc: e.matmul(psf[2][:, 0:N], lhsT=onesF, rhs=DWl[cc][:, 0:N], start=(cc == 0), stop=(cc == 1)),
                 reads=["onesF", ("DW", cc, pb)], writes=[PS(2)])
        for cc in range(2):
            P.op("pe", lambda e, cc=cc: e.matmul(psf[3][:, 0:N], lhsT=onesF, rhs=SQ[cc][:, 0:N], start=(cc == 0), stop=(cc == 1)),
                 reads=["onesF", ("SQ", cc)], writes=[PS(3)])
        P.op("dve", lambda e: e.tensor_scalar(out=MEANT[:, 0:N], in0=psf[2][:, 0:N], scalar1=1.0 / 256, scalar2=None, op0=ALU.mult),
             reads=[PS(2)], writes=["MEANT"])
        P.op("dve", lambda e: e.tensor_tensor(out=RSTD[:, 0:N], in0=MEANT[:, 0:N], in1=MEANT[:, 0:N], op=ALU.mult),
             reads=["MEANT"], writes=["RSTD"])
        P.op("dve", lambda e: e.scalar_tensor_tensor(out=RSTD[:, 0:N], in0=psf[3][:, 0:N], scalar=1.0 / 256, in1=RSTD[:, 0:N],
                                                     op0=ALU.mult, op1=ALU.subtract), reads=[PS(3), "RSTD"], writes=["RSTD"])
        P.op("dve", lambda e: e.tensor_scalar(out=RSTD[:, 0:N], in0=RSTD[:, 0:N], scalar1=EPS, scalar2=None, op0=ALU.add),
             reads=["RSTD"], writes=["RSTD"])
        P.op("act", lambda e: e.activation(out=RSTD[:, 0:N], in_=RSTD[:, 0:N], func=AF.Ln), reads=["RSTD"], writes=["RSTD"])
        P.op("act", lambda e: e.activation(out=RSTD[:, 0:N], in_=RSTD[:, 0:N], func=AF.Exp, scale=-0.5), reads=["RSTD"], writes=["RSTD"])
        for cc in range(2):
            dw = DWl[cc]
            P.op("dve", lambda e, dw=dw: e.tensor_tensor(out=dw[:, 0:N], in0=dw[:, 0:N], in1=MEANT[:, 0:N], op=ALU.subtract),
                 reads=[("DW", cc, pb), "MEANT"], writes=[("DW", cc, pb)])
            P.op("pool", lambda e, dw=dw: e.tensor_tensor(out=dw[:, 0:N], in0=dw[:, 0:N], in1=RSTD[:, 0:N], op=ALU.mult),
                 reads=[("DW", cc, pb), "RSTD"], writes=[("DW", cc, pb)])
            P.op("act", lambda e, dw=dw, cc=cc: e.activation(out=SUl[cc][:, 0:N], in_=dw[:, 0:N], func=AF.Silu,
                                                             scale=cpar[:, 1, cc:cc + 1], bias=cpar[:, 2, cc:cc + 1]),
                 reads=[("DW", cc, pb), "cpar"], writes=[("SU", cc, pb)])

    def conv_s3(ctx, gb, t0, N, tok0):
        pb, DWl, SUl = ctx
        for oc in range(2):
            bank = 4 + oc
            for cc in range(2):
                P.op("pe", lambda e, oc=oc, cc=cc, bank=bank: e.matmul(psf[bank][:, 0:N], lhsT=W_CPW[:, cc, oc * 128:(oc + 1) * 128],
                                                                        rhs=SUl[cc][:, 0:N], start=(cc == 0), stop=(cc == 1)),
                     reads=["W_CPW", ("SU", cc, pb)], writes=[PS(bank)])
            P.op("act", lambda e, oc=oc, bank=bank: e.copy(out=MIXT[:, 4 + oc, tok0:tok0 + N], in_=psf[bank][:, 0:N]),
                 reads=[PS(bank)], writes=[("MIXT", 4 + oc, tok0)])

    cblocks = [(0, b_ * 512, 512, b_ * 512) for b_ in range(4)] + [(2078 + 94 * s_, 0, 64, 2048 + 64 * s_) for s_ in range(4)]
    cctx = {}
    for k_ in range(len(cblocks) + 2):
        if 0 <= k_ - 1 < len(cblocks):
            conv_s2(cctx[k_ - 1], *cblocks[k_ - 1])
        if k_ < len(cblocks):
            cctx[k_] = conv_s1(*cblocks[k_])
        if 0 <= k_ - 2 < len(cblocks):
            conv_s3(cctx[k_ - 2], *cblocks[k_ - 2])
    if stop_after <= 4:
        P.emit(); return nc

    P.barrier()
    WOUT_OFF = 24576
    assert WOUT_OFF >= aA_base and WOUT_OFF + 16384 <= aFREE.off
    W_OUT = nc.alloc_sbuf_tensor_at("W_OUT", [128, 8, 1024], BF16, offset=aFREE.base + WOUT_OFF).ap()
    for c in range(8):
        P.dma("pool", lambda e, c=c: e.dma_start(out=W_OUT[:, c, :], in_=w_out[c * 128:(c + 1) * 128, :]), "W_OUT", writes=["W_OUT"])
    aA = Arena(aR.base, 36864)
    NSL = 4
    E = [aA.t("E%d" % i, [128, 512], BF16) for i in range(NSL)]
    SPb = [aA.t("SP%d" % i, [128, 512], BF16) for i in range(NSL)]
    Xb = [aA.t("X%d" % i, [128, 512], BF16) for i in range(NSL)]
    Wb = Xb
    SACC = [aA.t("SACC%d" % i, [128, 512], BF16) for i in range(4)]
    KC = [aA.t("KC%d" % i, [128, 512], BF16) for i in range(5)]
    VC = [aA.t("VC%d" % i, [128, 512], BF16) for i in range(10)]
    KCT = [aA.t("KCT%d" % i, [128, 4, 128], BF16) for i in range(2)]
    QBD = [aA.t("QBD%d" % i, [128, 4, 128], BF16) for i in range(2)]
    FSRC = aA.t("FSRC", [128, 512], BF16)
    P.op("dve", lambda e: e.memset(FSRC, 1.0), writes=["FSRC"])

    items = []
    gi = 0
    for hp in range(4):
        for Q in range(4):
            for hh in range(2):
                nkb = 4 * Q + 4
                for kb in range(nkb - 1, -1, -1):
                    items.append(dict(kind="p", hp=hp, Q=Q, hh=hh, kb=kb, first=(kb == nkb - 1), last=(kb == 0),
                                      grp=gi, sacc=(gi * 2 + hh) % 2, obank=4 + (gi % 2), endgrp=(hh == 1 and kb == 0)))
            gi += 1
    for s in range(4):
        for kb in range(32, -1, -1):
            items.append(dict(kind="s", s=s, kb=kb, first=(kb == 32), last=(kb == 0), grp=gi, sacc=gi % 2,
                              obank=4 + (gi % 2), endgrp=(kb == 0)))
        gi += 1
    gcount = {}
    for it in items:
        n = gcount.get((it["grp"], it.get("hh", 0)), 0)
        gcount[(it["grp"], it.get("hh", 0))] = n + 1
        gpar = it["hh"] if it["kind"] == "p" else it["grp"] % 2
        it["sa_cur"] = gpar * 2 + (n % 2)
        it["sa_nxt"] = gpar * 2 + ((n + 1) % 2)
        it["gpar"] = gpar
    for i, it in enumerate(items):
        it["i"] = i
        it["sl"] = i % NSL
        it["zb"] = i % 2
        it["sb"] = 2 + (i % 2)

    def build_qbd(s_):
        q = QBD[s_ % 2]
        P.op("pool", lambda e: e.memset(q, 0.0), writes=[("QBD", s_ % 2)])
        for hh in range(2):
            pr = slice(64 * hh, 64 * hh + 64)
            P.op("pool", lambda e, pr=pr, hh=hh: e.tensor_copy(out=q[pr, :, 64 * hh:64 * hh + 64], in_=QS[pr, :, 64 * s_:64 * s_ + 64]),
                 reads=[("QS", j) for j in range(4)] + [("QBD", s_ % 2)], writes=[("QBD", s_ % 2)])

    def rng(it):
        if it["kind"] == "p":
            c0 = max(0, it["kb"] - 4 * it["Q"]) * 128
            return slice(0, 128), c0, 512
        if it["kb"] == 32:
            o = 64 * (it["s"] % 2)
            return slice(o, o + 64), 0, 512
        return slice(0, 128), 0, 512

    wgv0 = w_eg.rearrange("e (p c) f -> (e p) (c f)", c=8)
    wuv0 = w_eu.rearrange("e (p c) f -> (e p) (c f)", c=8)
    wdv0 = w_ed.rearrange("e (p c) d -> (e p) (c d)", c=2)
    cast_jobs = [(src, dst, g) for g in range(8) for (src, dst) in ((wgv0, WGB), (wuv0, WUB), (wdv0, WDB))]

    def st_load(it):
        if it["kind"] == "p" and it["i"] % 12 == 0 and cast_jobs:
            src, dst, g = cast_jobs.pop(0)
            P.dma("pool", lambda e: e.dma_start(out=dst[g * 512:(g + 1) * 512, :], in_=src[g * 512:(g + 1) * 512, :]), "WCAST", writes=["WCAST"])
        if it["kind"] != "s" or it["kb"] == 32:
            return
        s, kb = it["s"], it["kb"]
        sl3 = it["i"] % 5
        P.dma("pool", lambda e: e.dma_start(out=KC[sl3], in_=ck[s, kb * 128:(kb + 1) * 128, :]), ("KC", sl3), writes=[("KC", sl3)])
        sl8 = it["i"] % 10
        P.dma("pool", lambda e: e.dma_start(out=VC[sl8], in_=cv[s, kb * 128:(kb + 1) * 128, :]), ("VC", sl8), writes=[("VC", sl8)])

    def st_T(it):
        if it["kind"] != "s" or it["kb"] == 32:
            return
        sl3 = it["i"] % 5
        s2 = it["i"] % 2
        for hp in range(4):
            P.op("pe", lambda e, hp=hp: e.transpose(psb[6][:, hp * 128:(hp + 1) * 128], KC[sl3][:, hp * 128:(hp + 1) * 128], ident),
                 reads=[("KC", sl3), "ident"], writes=[PS(6)])
        P.op("dve", lambda e: e.tensor_copy(out=KCT[s2], in_=psb[6][:, 0:512].rearrange("p (h n) -> p h n", n=128)),
             reads=[PS(6)], writes=[("KCT", s2)])

    def st_A(it):
        pr, c0, ce = rng(it)
        zb = it["zb"]
        if it["kind"] == "p":
            hp, Q, hh, kb = it["hp"], it["Q"], it["hh"], it["kb"]
            hr = slice(64 * hh, 64 * hh + 64)
            P.op("pe", lambda e: e.matmul(psf[zb][:, c0:512], lhsT=KT[hr, hp, kb * 128:(kb + 1) * 128], rhs=QT[hr, hp, Q * 512 + c0:Q * 512 + 512],
                                          start=True, stop=True),
                 reads=[("KT", hp, kb // 4), ("QT", hp, Q)], writes=[PS(zb)])
        else:
            s, kb = it["s"], it["kb"]
            if kb == 32:
                build_qbd(s)
            for hp in range(4):
                if kb == 32:
                    P.op("pe", lambda e, hp=hp: e.matmul(psf[zb][pr, hp * 128:(hp + 1) * 128], lhsT=KS[:, hp, 64 * s:64 * s + 64], rhs=QBD[s % 2][:, hp, :],
                                                         start=True, stop=True),
                         reads=[("KS", hp), ("QBD", s % 2)], writes=[PS(zb)])
                else:
                    s2 = it["i"] % 2
                    P.op("pe", lambda e, hp=hp: e.matmul(psf[zb][:, hp * 128:(hp + 1) * 128], lhsT=KCT[s2][:, hp, :], rhs=QBD[s % 2][:, hp, :],
                                                         start=True, stop=True),
                         reads=[("KCT", s2), ("QBD", s % 2)], writes=[PS(zb)])

    def st_B(it):
        pr, c0, ce = rng(it)
        zb, sl = it["zb"], it["sl"]
        P.op("act", lambda e: e.activation(out=E[sl][pr, c0:512], in_=psf[zb][pr, c0:512], func=AF.Exp, scale=SCALE),
             reads=[PS(zb)], writes=[("E", sl)])
        if it["kind"] == "p" and it["kb"] >= 4 * it["Q"]:
            P.op("dve", lambda e: e.tensor_tensor(out=E[sl][:, c0:c0 + 128], in0=E[sl][:, c0:c0 + 128], in1=tri, op=ALU.mult),
                 reads=[("E", sl), "tri"], writes=[("E", sl)])
        if it["kind"] == "s" and it["kb"] == 32:
            P.op("dve", lambda e: e.tensor_tensor(out=E[sl][pr, :], in0=E[sl][pr, :], in1=tri8[pr].rearrange("p h c -> p (h c)"), op=ALU.mult),
                 reads=[("E", sl), "tri8"], writes=[("E", sl)])
        P.op("act", lambda e: e.activation(out=SPb[sl][pr, c0:512], in_=E[sl][pr, c0:512], func=AF.Ln, bias=1.0),
             reads=[("E", sl)], writes=[("SP", sl)])

    def st_C(it):
        pr, c0, ce = rng(it)
        sb, sl, sa, sn = it["sb"], it["sl"], it["sa_cur"], it["sa_nxt"]
        if it["first"]:
            P.op("dve", lambda e: e.memset(SACC[sa], 0.0), writes=[("SACC", sa)])
            P.op("dve", lambda e: e.memset(SACC[sn], 0.0), writes=[("SACC", sn)])
        P.op("pe", lambda e: e.matmul(psf[sb][pr, c0:512], lhsT=Uinc[pr, pr], rhs=SPb[sl][pr, c0:512], start=True, stop=it["first"]),
             reads=["Uinc", ("SP", sl)], writes=[PS(sb)])
        if not it["first"]:
            P.op("pe", lambda e: e.matmul(psf[sb][pr, c0:512], lhsT=ones[:, pr], rhs=SACC[sa][:, c0:512], start=False, stop=True),
                 reads=["ones", ("SACC", sa)], writes=[PS(sb)])
        if not it["last"]:
            P.op("dve", lambda e: e.tensor_tensor(out=SACC[sn][pr, c0:512], in0=SACC[sa][pr, c0:512], in1=SPb[sl][pr, c0:512], op=ALU.add),
                 reads=[("SACC", sa), ("SP", sl)], writes=[("SACC", sn)])

    def st_D(it):
        pr, c0, ce = rng(it)
        sb, sl, sa = it["sb"], it["sl"], it["sacc"]
        P.op("act", lambda e: e.activation(out=Xb[sl][pr, c0:512], in_=psf[sb][pr, c0:512], func=AF.Exp, scale=-1.0),
             reads=[PS(sb)], writes=[("X", sl), ("W", sl)])
        P.op("dve", lambda e: e.tensor_tensor(out=Wb[sl][pr, c0:512], in0=E[sl][pr, c0:512], in1=Xb[sl][pr, c0:512], op=ALU.mult),
             reads=[("E", sl), ("X", sl)], writes=[("W", sl)])

    def st_E(it):
        pr, c0, ce = rng(it)
        sl, ob = it["sl"], it["obank"]
        if it["kind"] == "p":
            hp, Q, hh, kb = it["hp"], it["Q"], it["hh"], it["kb"]
            h = 2 * hp + hh
            hr = slice(64 * hh, 64 * hh + 64)
            P.op("pe", lambda e: e.matmul(psf[ob][hr, c0:512], lhsT=V_ALL[:, kb, h * 64:(h + 1) * 64], rhs=Wb[sl][:, c0:512],
                                          start=it["first"], stop=it["last"], skip_group_check=True),
                 reads=[("V_ALL", kb), ("W", sl)], writes=[PS(ob)])
            if it["endgrp"]:
                P.op("act", lambda e: e.copy(out=MIXT[:, hp, Q * 512:(Q + 1) * 512], in_=psf[ob]), reads=[PS(ob)], writes=[("MIXT", hp, Q * 512)])
        else:
            s, kb = it["s"], it["kb"]
            sl3 = it["i"] % 5
            for h in range(8):
                hp, hh = h // 2, h % 2
                hr = slice(64 * hh, 64 * hh + 64)
                if kb == 32:
                    vt = V_ALL[pr, 16 + s // 2, h * 64:(h + 1) * 64]
                    vres = ("V_ALL", 16 + s // 2)
                else:
                    vt = VC[it["i"] % 10][:, h * 64:(h + 1) * 64]
                    vres = ("VC", it["i"] % 10)
                P.op("pe", lambda e, h=h, hp=hp, hr=hr, vt=vt: e.matmul(psf[ob][hr, hp * 64:(hp + 1) * 64], lhsT=vt, rhs=Wb[sl][pr, h * 64:(h + 1) * 64],
                                                                          start=(it["first"] and hp == 0), stop=it["last"], skip_group_check=True),
                     reads=[vres, ("W", sl)], writes=[PS(ob)])
            if it["endgrp"]:
                P.op("act", lambda e: e.copy(out=MIXT[:, 0:4, 2048 + 64 * s:2048 + 64 * s + 64],
                                             in_=psf[ob][:, 0:256].rearrange("p (h n) -> p h n", n=64)),
                     reads=[PS(ob)], writes=[("MIXT", "s", s)])

    n_it = len(items)
    NFILL = 1
    for step in range(n_it + 10):
        def g(k):
            j = step - k
            return items[j] if 0 <= j < n_it else None
        for k, fnst in ((0, st_load), (4, st_T), (5, st_A), (6, st_B), (8, st_C), (8, st_D), (9, st_E)):
            it = g(k)
            if it is not None:
                fnst(it)
            if k in (5, 8) and fnst is not st_D and NFILL and it is not None and it["kind"] == "p":
                P.op("pe", lambda e: e.matmul(psf[7], lhsT=ones, rhs=FSRC, start=True, stop=True), reads=["FSRC", "ones"])
    if stop_after <= 5:
        P.emit(); return nc

    P.barrier()
    aFREE.reset()
    RT = aFREE.t("RT", [128, 18, 88], F32)
    off_rt = aFREE.off
    LNG1 = aFREE.t("LNG1", [128, 1024], F32)
    LNB1 = aFREE.t("LNB1", [128, 1024], F32)
    XIN5 = [aFREE.t("XIN5_%d" % i, [128, 1024], F32) for i in range(2)]
    assert aFREE.off <= WOUT_OFF
    aFREE.off = WOUT_OFF + 16384
    XIN5.append(aFREE.t("XIN5_2", [128, 1024], F32))
    H5 = XIN5
    X1B = [aFREE.t("X1B_%d" % i, [128, 1024], BF16) for i in range(2)]
    for (tl, src) in ((LNG1, ln1_g), (LNB1, ln1_b)):
        P.dma("sp", lambda e, tl=tl, src=src: e.dma_start(out=tl, in_=src.partition_broadcast(128)), ("LNP", id(tl)), writes=[("LNP", id(tl))])
    LN1R = [("LNP", id(LNG1)), ("LNP", id(LNB1))]

    def mix_res(t):
        n0 = t * 128
        if t < 16:
            q = (n0 // 512) * 512
            return [("MIXT", c, q) for c in range(8)]
        r = []
        for s in (2 * (t - 16), 2 * (t - 16) + 1):
            r += [("MIXT", "s", s)] + [("MIXT", c, 2048 + 64 * s) for c in range(4, 8)]
        return r

    def router_mm(t):
        bank = 4 + (t % 2)
        for c in range(8):
            P.op("pe", lambda e, c=c: e.matmul(psf[bank][:, 0:36], lhsT=XT[:, c, tcols(t)], rhs=W_R[:, c, :], start=(c == 0), stop=(c == 7)),
                 reads=[("XT", t), "W_R"], writes=[PS(bank)])
        P.op("dve", lambda e: e.tensor_tensor(out=RT[:, t, 84:88], in0=psf[bank][:, 0:4], in1=RB[:, 0:4], op=ALU.add),
             reads=[PS(bank), "RB"], writes=["LG"])
        P.op("dve", lambda e: e.tensor_tensor(out=GATE[:, t, :], in0=psf[bank][:, 4:36], in1=RB[:, 4:36], op=ALU.add),
             reads=[PS(bank), "RB"], writes=["LG"])

    def p5_load(t):
        s = t % 3
        P.dma("sp", lambda e: e.dma_start(out=XIN5[s], in_=XNS[t * 128:(t + 1) * 128, :]), ("XIN5", s), reads=[("XNS", t)], writes=[("XIN5", s)])

    def p5_mm(t):
        s = t % 3
        s2 = t % 2
        for half in range(2):
            bank = 2 * s2 + half
            for c in range(8):
                P.op("pe", lambda e, c=c, half=half, bank=bank: e.matmul(psf[bank], lhsT=MIXT[:, c, tcols(t)], rhs=W_OUT[:, c, half * 512:(half + 1) * 512],
                                                                         start=(c == 0), stop=(c == 7)),
                     reads=mix_res(t) + ["W_OUT"], writes=[PS(bank)])
            P.op("dve", lambda e, half=half, bank=bank: e.scalar_tensor_tensor(
                out=H5[s][:, half * 512:(half + 1) * 512], in0=XIN5[s][:, half * 512:(half + 1) * 512], scalar=ALPHA, in1=psf[bank],
                op0=ALU.mult, op1=ALU.add), reads=[("XIN5", s), PS(bank)], writes=[("XIN5", s)])

    p5ctx = {}

    def p5_lnA(t):
        s = t % 3
        p5ctx[t] = ln_A(H5[s], ("XIN5", s))

    def p5_ln(t):
        s = t % 3
        s2 = t % 2
        ln_B(p5ctx[t], H5[s], ("XIN5", s), H5[s], ("XIN5", s), LNG1, LNB1, LN1R)
        P.op("act", lambda e: e.mul(out=R[:, t, :], in_=H5[s], mul=ALPHA), reads=[("XIN5", s)], writes=[("R", t)])
        P.op("act", lambda e: e.copy(out=X1B[s2], in_=H5[s]), reads=[("XIN5", s)], writes=[("X1B", s2)])

    def p5_T(t):
        s2 = t % 2
        transpose_to_XT(t, X1B[s2], ("X1B", s2), 6 + s2)

    p5_load(0)
    p5_load(1)
    for t in range(20):
        if t < 18:
            p5_mm(t)
            p5_lnA(t)
        if 1 <= t < 19:
            p5_T(t - 1)
        if t < 18:
            p5_ln(t)
            if t + 2 < 18:
                p5_load(t + 2)
        if t >= 2:
            router_mm(t - 2)
    P.barrier()
    aF2 = Arena(aFREE.base + off_rt, aFREE.size - off_rt)
    X_ = mybir.AxisListType.X
    NS = 50
    TS = 256
    SELB = aF2.t("SELB", [128, 18, 32], BF16)
    RANK = aF2.t("RANK", [128, 18, 32], F32)
    TMP = aF2.t("TMP", [128, 18, 32], F32)
    CNT = aF2.t("CNT", [128, 32], F32)
    NTL = aF2.t("NTL", [128, 32], F32)
    SCA = aF2.t("SCA", [128, 32], F32)
    SCB = aF2.t("SCB", [128, 32], F32)
    CMP9 = aF2.t("CMP9", [128, 32, 9], F32)
    THR = aF2.t("THR", [128, 32, 9], F32)
    S1F = aF2.t("S1F", [128, 18], F32)
    S2F = aF2.t("S2F", [128, 18], F32)
    S1I = aF2.t("S1I", [128, 18], I32)
    S2I = aF2.t("S2I", [128, 18], I32)
    DV1 = aF2.t("DV1", [128, 18], I32)
    DV2 = aF2.t("DV2", [128, 18], I32)
    JCI = aF2.t("JCI", [128, NS, 32], I32)
    JC = aF2.t("JC", [128, NS, 32], F32)
    CMPJ = aF2.t("CMPJ", [128, NS, 32], F32)
    EJF = aF2.t("EJF", [128, NS], F32)
    PIDI = aF2.t("PIDI", [128, 1], I32)
    PIDF = aF2.t("PIDF", [128, 1], F32)
    XB = [aF2.t("XB%d" % i, [128, 1028], BF16) for i in range(8)]
    TRIB = aF2.t("TRIB", [128, 128], BF16)

    gm = RT[:, :, 0]
    ngs = RT[:, :, 1]
    gw = RT[:, :, 2]
    m1 = RT[:, :, 3]
    m2 = RT[:, :, 4]
    d21 = RT[:, :, 5]
    w1 = RT[:, :, 6]
    w2 = RT[:, :, 7]
    gmask = RT[:, :, 8:12]
    pen = RT[:, :, 12:16]
    ge = RT[:, :, 16:20]
    em = GATE[:, :, :]
    mk1 = RT[:, :, 20:52]
    mk2 = RT[:, :, 52:84]
    lg4 = RT[:, :, 84:88]

    def bc(a_, n):
        return a_.unsqueeze(2).to_broadcast([128, 18, n])

    def D(fn):
        P.op("dve", fn, reads=["RT", "LG"], writes=["RT"])
    D(lambda e: e.tensor_reduce(out=gm, in_=lg4, axis=X_, op=ALU.max))
    D(lambda e: e.tensor_tensor(out=gmask, in0=lg4, in1=bc(gm, 4), op=ALU.is_ge))
    D(lambda e: e.tensor_scalar(out=pen, in0=gmask, scalar1=-1.0, scalar2=BIG, op0=ALU.add, op1=ALU.mult))
    D(lambda e: e.tensor_tensor(out=ge, in0=lg4, in1=bc(gm, 4), op=ALU.subtract))
    P.op("act", lambda e: e.activation(out=ge, in_=ge, func=AF.Exp), reads=["RT"], writes=["RT"])
    D(lambda e: e.tensor_reduce(out=ngs, in_=ge, axis=X_, op=ALU.add))
    D(lambda e: e.reciprocal(out=gw, in_=ngs))
    D(lambda e: e.tensor_tensor(out=em.rearrange("p t (g x) -> p t g x", x=8), in0=em.rearrange("p t (g x) -> p t g x", x=8),
                                in1=pen.unsqueeze(3).to_broadcast([128, 18, 4, 8]), op=ALU.add))
    D(lambda e: e.tensor_reduce(out=m1, in_=em, axis=X_, op=ALU.max))
    D(lambda e: e.tensor_tensor(out=mk1, in0=em, in1=bc(m1, 32), op=ALU.is_ge))
    D(lambda e: e.scalar_tensor_tensor(out=em, in0=mk1, scalar=-BIG, in1=em, op0=ALU.mult, op1=ALU.add))
    D(lambda e: e.tensor_reduce(out=m2, in_=em, axis=X_, op=ALU.max))
    D(lambda e: e.tensor_tensor(out=mk2, in0=em, in1=bc(m2, 32), op=ALU.is_ge))
    D(lambda e: e.tensor_tensor(out=d21, in0=m2, in1=m1, op=ALU.subtract))
    P.op("act", lambda e: e.activation(out=d21, in_=d21, func=AF.Exp), reads=["RT"], writes=["RT"])
    D(lambda e: e.tensor_scalar(out=w1, in0=d21, scalar1=1.0, scalar2=None, op0=ALU.add))
    D(lambda e: e.reciprocal(out=w1, in_=w1))
    D(lambda e: e.tensor_tensor(out=w2, in0=d21, in1=w1, op=ALU.mult))
    D(lambda e: e.tensor_tensor(out=w1, in0=w1, in1=gw, op=ALU.mult))
    D(lambda e: e.tensor_tensor(out=w2, in0=w2, in1=gw, op=ALU.mult))
    D(lambda e: e.tensor_tensor(out=SELB, in0=mk1, in1=mk2, op=ALU.add))
    P.op("act", lambda e: e.copy(out=TRIB, in_=tri), reads=["tri"], writes=["TRIB"])
    for t in range(18):
        bank = 0 if t < 9 else 1
        cs = slice((t % 9) * 32, (t % 9) * 32 + 32)
        for t2 in range(t + 1):
            P.op("pe", lambda e, t2=t2, t=t, bank=bank, cs=cs: e.matmul(psf[bank][:, cs], lhsT=(TRIB if t2 == t else ones), rhs=SELB[:, t2, :],
                                                                      start=(t2 == 0), stop=(t2 == t)),
                 reads=["RT", "TRIB", "ones"], writes=[PS(bank)])
    for t2 in range(18):
        P.op("pe", lambda e, t2=t2: e.matmul(psf[2][:, 0:32], lhsT=ones, rhs=SELB[:, t2, :], start=(t2 == 0), stop=(t2 == 17)),
             reads=["RT", "ones"], writes=[PS(2)])
    P.op("dve", lambda e: e.tensor_copy(out=RANK[:, 0:9, :], in_=psf[0][:, 0:288].rearrange("p (t x) -> p t x", x=32)), reads=[PS(0)], writes=["RANK"])
    P.op("dve", lambda e: e.tensor_copy(out=RANK[:, 9:18, :], in_=psf[1][:, 0:288].rearrange("p (t x) -> p t x", x=32)), reads=[PS(1), "RANK"], writes=["RANK"])
    P.op("dve", lambda e: e.tensor_copy(out=CNT, in_=psf[2][:, 0:32]), reads=[PS(2)], writes=["CNT"])
    for k in range(9):
        P.op("pool", lambda e, k=k: e.memset(THR[:, :, k:k + 1], float(TS * k)), writes=["THR"])
    P.op("dve", lambda e: e.tensor_tensor(out=CMP9, in0=CNT.unsqueeze(2).to_broadcast([128, 32, 9]), in1=THR, op=ALU.is_gt),
         reads=["CNT", "THR"], writes=["CMP9"])
    P.op("dve", lambda e: e.tensor_reduce(out=NTL, in_=CMP9, axis=X_, op=ALU.add), reads=["CMP9"], writes=["NTL"])
    src_, dst_ = NTL, SCA
    for sh in (1, 2, 4, 8, 16):
        P.op("dve", lambda e, src_=src_, dst_=dst_, sh=sh: e.tensor_copy(out=dst_[:, 0:sh], in_=src_[:, 0:sh]), reads=["NTL", "SC"], writes=["SC"])
        P.op("dve", lambda e, src_=src_, dst_=dst_, sh=sh: e.tensor_tensor(out=dst_[:, sh:32], in0=src_[:, sh:32], in1=src_[:, 0:32 - sh], op=ALU.add),
             reads=["NTL", "SC"], writes=["SC"])
        src_, dst_ = dst_, (SCB if dst_ is SCA else SCA)
    INCL = src_
    EXCL = dst_
    P.op("dve", lambda e: e.tensor_tensor(out=EXCL, in0=INCL, in1=NTL, op=ALU.subtract), reads=["SC", "NTL"], writes=["SC"])
    P.op("dve", lambda e: e.tensor_scalar(out=EXCL, in0=EXCL, scalar1=float(TS), scalar2=None, op0=ALU.mult), reads=["SC"], writes=["SC"])
    P.op("dve", lambda e: e.tensor_tensor(out=RANK, in0=RANK, in1=EXCL.unsqueeze(1).to_broadcast([128, 18, 32]), op=ALU.add),
         reads=["RANK", "SC"], writes=["RANK"])
    for (mk_, SF_, SI_) in ((mk1, S1F, S1I), (mk2, S2F, S2I)):
        P.op("dve", lambda e, mk_=mk_: e.tensor_tensor(out=TMP, in0=RANK, in1=mk_, op=ALU.mult), reads=["RANK", "RT"], writes=["TMP"])
        P.op("dve", lambda e, SF_=SF_: e.tensor_reduce(out=SF_, in_=TMP, axis=X_, op=ALU.add), reads=["TMP"], writes=["SF"])
        P.op("dve", lambda e, SF_=SF_, SI_=SI_: e.tensor_copy(out=SI_, in_=SF_), reads=["SF"], writes=["SI"])
    P.op("pool", lambda e: e.iota(DV1, pattern=[[128, 18]], base=0, channel_multiplier=1), writes=["DV"])
    P.op("pool", lambda e: e.iota(DV2, pattern=[[128, 18]], base=NT, channel_multiplier=1), writes=["DV"])
    P.op("pool", lambda e: e.iota(JCI, pattern=[[1, NS], [0, 32]], base=0, channel_multiplier=0), writes=["JCI"])
    P.op("dve", lambda e: e.tensor_copy(out=JC, in_=JCI), reads=["JCI"], writes=["JC"])
    P.op("dve", lambda e: e.tensor_tensor(out=CMPJ, in0=INCL.unsqueeze(1).to_broadcast([128, NS, 32]), in1=JC, op=ALU.is_le),
         reads=["SC", "JC"], writes=["CMPJ"])
    P.op("dve", lambda e: e.tensor_reduce(out=EJF, in_=CMPJ, axis=X_, op=ALU.add), reads=["CMPJ"], writes=["EJF"])
    P.op("pool", lambda e: e.iota(PIDI, pattern=[[0, 1]], base=0, channel_multiplier=1), writes=["PIDI"])
    P.op("dve", lambda e: e.tensor_copy(out=PIDF, in_=PIDI), reads=["PIDI"], writes=["PIDF"])
    P.op("dve", lambda e: e.tensor_scalar(out=EJF, in0=EJF, scalar1=128.0, scalar2=None, op0=ALU.mult), reads=["EJF"], writes=["EJF"])
    P.op("dve", lambda e: e.tensor_scalar(out=EJF, in0=EJF, scalar1=PIDF[:, 0:1], scalar2=None, op0=ALU.add), reads=["EJF", "PIDF"], writes=["EJF"])
    P.op("dve", lambda e: e.tensor_copy(out=IDXW, in_=EJF), reads=["EJF"], writes=["IDXW"])
    for t in range(18):
        for k in range(2):
            xb = XB[(t % 4) * 2 + k]
            xr = ("XB", (t % 4) * 2 + k)
            DV_, SI_, wk = (DV1, S1I, w1) if k == 0 else (DV2, S2I, w2)
            P.op("act", lambda e, xb=xb, t=t: e.mul(out=xb[:, 0:1024], in_=R[:, t, :], mul=1.0 / ALPHA), reads=[("R", t)], writes=[xr])
            P.op("dve", lambda e, xb=xb, t=t, DV_=DV_: e.tensor_copy(out=xb[:, 1024:1026].bitcast(I32), in_=DV_[:, t:t + 1]),
                 reads=["DV", xr], writes=[xr])
            P.op("dve", lambda e, xb=xb, t=t, wk=wk: e.tensor_copy(out=xb[:, 1026:1028].bitcast(F32), in_=wk[:, t:t + 1]),
                 reads=["RT", xr], writes=[xr])
            P.dma("pool", lambda e, xb=xb, t=t, SI_=SI_: e.indirect_dma_start(
                out=XS, out_offset=bass.IndirectOffsetOnAxis(ap=SI_[:, t:t + 1], axis=0), in_=xb, in_offset=None), "XSsc", reads=[xr, "SI", "XSfill"], writes=[("XSs", t, k)])
    XS_ALL = [("XSs", t, k) for t in range(18) for k in range(2)]
    if stop_after <= 6:
        P.emit(); return nc

    P.barrier()
    aMIX.reset()
    NW = 3
    WG = [aMIX.t("WG%d" % i, [128, 8, 256], BF16) for i in range(NW)]
    WU = [aMIX.t("WU%d" % i, [128, 8, 256], BF16) for i in range(NW)]
    WD = [aMIX.t("WD%d" % i, [128, 2, 1024], BF16) for i in range(NW)]
    aFREE.reset()
    XSB = [aFREE.t("XSB%d" % i, [128, 1028], BF16) for i in range(6)]
    XST = [aFREE.t("XST%d" % i, [128, 8, 256], BF16) for i in range(2)]
    SLs = [aFREE.t("SLs%d" % i, [128, 512], BF16) for i in range(2)]
    HIDT = [aFREE.t("HIDT%d" % i, [128, 512], BF16) for i in range(2)]
    YSB = [aFREE.t("YSB%d" % i, [128, 1024], F32) for i in range(4)]
    LNG2 = aFREE.t("LNG2", [128, 1024], F32)
    LNB2 = aFREE.t("LNB2", [128, 1024], F32)
    P.dma("sp", lambda e: e.dma_start(out=LNG2, in_=ln2_g.partition_broadcast(128)), "LNG2", writes=["LNG2"])
    P.dma("sp", lambda e: e.dma_start(out=LNB2, in_=ln2_b.partition_broadcast(128)), "LNB2", writes=["LNB2"])
    assert not cast_jobs
    wgv, wuv, wdv = WGB, WUB, WDB

    regs = {}

    def bc_reg(e):
        if "bc" not in regs:
            regs["bc"] = e.alloc_register("bcreg")
            e.reg_mov(regs["bc"], 4095)
        return regs["bc"]

    def bc_reg2(e):
        if "bc2" not in regs:
            regs["bc2"] = e.alloc_register("bcreg2")
            e.reg_mov(regs["bc2"], 2 * NT - 1)
        return regs["bc2"]

    def sp_L(j):
        w = j % NW
        for h in range(2):
            xi = (j % 3) * 2 + h
            P.dma("sp", lambda e, xi=xi, h=h: e.dma_start(out=XSB[xi], in_=XS[(2 * j + h) * 128:(2 * j + h + 1) * 128, :]), ("XSB", xi),
                  reads=XS_ALL, writes=[("XSB", xi)])
        for (Wt, wres, wv) in ((WG[w], ("WG", w), wgv), (WU[w], ("WU", w), wuv), (WD[w], ("WD", w), wdv)):
            P.dma("pool", lambda e, Wt=Wt, wv=wv: e.indirect_dma_start(
                out=(Wt.rearrange("p c f -> p (c f)")), out_offset=None, in_=wv,
                in_offset=bass.IndirectOffsetOnAxis(ap=IDXW[:, j:j + 1], axis=0), bounds_check=bc_reg(e), oob_is_err=False),
                wres, reads=["IDXW", "WCAST"], writes=[wres])

    def sp_T(j):
        x = j % 2
        for h in range(2):
            xi = (j % 3) * 2 + h
            bank = 6 + h
            for c in range(8):
                P.op("pe", lambda e, c=c, xi=xi, bank=bank: e.transpose(psb[bank][:, c * 128:(c + 1) * 128], XSB[xi][:, c:1024:8], ident),
                     reads=[("XSB", xi), "ident"], writes=[PS(bank)])
            if h == 0:
                P.op("act", lambda e, bank=bank, h=h: e.copy(out=XST[x][:, :, h * 128:(h + 1) * 128], in_=psb[bank].rearrange("p (c n) -> p c n", n=128)),
                     reads=[PS(bank)], writes=[("XST", x, h)])
            else:
                P.op("dve", lambda e, bank=bank, h=h: e.tensor_copy(out=XST[x][:, :, h * 128:(h + 1) * 128], in_=psb[bank].rearrange("p (c n) -> p c n", n=128)),
                     reads=[PS(bank)], writes=[("XST", x, h)])

    def sp_U(j):
        w = j % NW
        x = j % 2
        bA, bU = 0, 1
        for (Wt, wres, bank) in ((WG[w], ("WG", w), bA), (WU[w], ("WU", w), bU)):
            for fc in range(2):
                for c in range(8):
                    P.op("pe", lambda e, Wt=Wt, fc=fc, c=c, bank=bank: e.matmul(psf[bank][:, fc * 256:(fc + 1) * 256], lhsT=Wt[:, c, fc:256:2], rhs=XST[x][:, c, :],
                                                                                 start=(c == 0), stop=(c == 7)),
                         reads=[wres, ("XST", x, 0), ("XST", x, 1)], writes=[PS(bank)])
        P.op("act", lambda e: e.activation(out=SLs[x], in_=psf[bA], func=AF.Silu), reads=[PS(bA)], writes=[("SLs", x)])
        P.op("dve", lambda e: e.tensor_tensor(out=HIDT[x], in0=psf[bU], in1=SLs[x], op=ALU.mult), reads=[PS(bU), ("SLs", x)], writes=[("HIDT", x)])

    def sp_D(j):
        w = j % NW
        x = j % 2
        for h in range(2):
            xi = (j % 3) * 2 + h
            yi = x * 2 + h
            gate = XSB[xi][:, 1026:1028].bitcast(F32)
            for half in range(2):
                bank = 2 + 2 * h + half
                for fc in range(2):
                    P.op("pe", lambda e, fc=fc, h=h, half=half, bank=bank: e.matmul(
                        psf[bank], lhsT=HIDT[x][:, fc * 256 + h * 128:fc * 256 + (h + 1) * 128], rhs=WD[w][:, fc, half * 512:(half + 1) * 512],
                        start=(fc == 0), stop=(fc == 1)), reads=[("HIDT", x), ("WD", w)], writes=[PS(bank)])
                if half == 0:
                    P.op("act", lambda e, yi=yi, bank=bank, gate=gate: e.activation(out=YSB[yi][:, 0:512], in_=psf[bank], func=AF.Copy, scale=gate),
                         reads=[PS(bank), ("XSB", xi)], writes=[("YSB", yi)])
                else:
                    P.op("dve", lambda e, yi=yi, bank=bank, gate=gate: e.tensor_scalar(out=YSB[yi][:, 512:1024], in0=psf[bank], scalar1=gate, scalar2=None, op0=ALU.mult),
                         reads=[PS(bank), ("XSB", xi)], writes=[("YSB", yi)])
            P.dma("pool", lambda e, xi=xi, yi=yi: e.indirect_dma_start(
                out=YK, out_offset=bass.IndirectOffsetOnAxis(ap=XSB[xi][:, 1024:1026].bitcast(I32), axis=0), in_=YSB[yi], in_offset=None,
                bounds_check=bc_reg2(e), oob_is_err=False), ("st", "YSB", yi), reads=[("YSB", yi), ("XSB", xi)], writes=[("YKs", j, h)])

    sp_L(0)
    sp_L(1)
    for st_ in range(NS + 1):
        if st_ < NS:
            sp_T(st_)
        if st_ >= 1:
            sp_D(st_ - 1)
        if st_ < NS:
            sp_U(st_)
        if st_ + 2 < NS:
            sp_L(st_ + 2)
    YK_ALL = [("YKs", j, h) for j in range(NS) for h in range(2)]

    P.barrier()
    YO = [XST[0].rearrange("p c n -> p (c n)").bitcast(F32), XST[1].rearrange("p c n -> p (c n)").bitcast(F32)]
    def racc(t, k):
        P.dma("pool", lambda e: e.dma_start(out=R[:, t, :], in_=YK[k * NT + t * 128:k * NT + (t + 1) * 128, :], accum_op=ALU.add), ("RACC", t),
              reads=YK_ALL + [("R", t)], writes=[("R", t)])

    racc(0, 0)
    for t in range(18):
        if t + 1 < 18:
            racc(t + 1, 0)
        racc(t, 1)
    for t in range(18):
        s = t % 2
        layernorm("ln2", R[:, t, :], ("R", t), YO[s], ("YO", s), LNG2, LNB2, ["LNG2", "LNB2"])
        dst = yp[t * 128:(t + 1) * 128, :] if t < 16 else ys[(t - 16) * 128:(t - 15) * 128, :]
        P.dma("sp", lambda e, s=s, dst=dst: e.dma_start(out=dst, in_=YO[s]), ("st", "YO", s), reads=[("YO", s)])

    P.emit()
    return nc


_CACHE = {}
STOP = 99


def kernel(**inp):
    f = lambda a: np.ascontiguousarray(np.asarray(a, dtype=np.float32))
    if "nc" not in _CACHE:
        nc = bass.Bass("TRN2", target_bir_lowering=False)
        build(nc, STOP)
        _CACHE["nc"] = nc
    nc = _CACHE["nc"]
    rep = {}
    for k in ["ln0_g", "ln0_b"]:
        rep[k] = f(inp[k])
    for k in ["w_in", "w_dw", "b_dw", "lnc_g", "lnc_b", "w_cpw", "w_mk", "w_mv", "w_out", "ln1_g", "ln1_b", "w_rg", "b_rg",
              "w_re", "b_re", "w_eg", "w_eu", "w_ed", "ln2_g", "ln2_b"]:
        rep[k] = f(inp[k][0])
    x_prompt = f(inp["x_prompt"]); x_sample = f(inp["x_sample"]); mem_prompt = f(inp["mem_prompt"])
    cK = inp["cache_sba_k"]; cV = inp["cache_sba_v"]
    in_maps = []
    for c in range(NCORES):
        m = dict(rep)
        m["xp"] = x_prompt[c]
        m["xs"] = x_sample[4 * c:4 * c + 4].reshape(256, 1024)
        m["memp"] = mem_prompt[c]
        m["ck"] = f(cK[0, 4 * c:4 * c + 4]).reshape(4, 4096, 512)
        m["cv"] = f(cV[0, 4 * c:4 * c + 4]).reshape(4, 4096, 512)
        m["cconv"] = f(inp["cache_conv"][0, 4 * c:4 * c + 4])
        m["cmk"] = f(inp["cache_mem_k"][0, 4 * c:4 * c + 4]).reshape(4, 256, 256)
        m["cmv"] = f(inp["cache_mem_v"][0, 4 * c:4 * c + 4]).reshape(4, 256, 256)
        in_maps.append(m)
    res = run_bass_kernel_spmd(nc, in_maps, core_ids=list(range(NCORES)))
    rs = res.results
    cat = lambda k: np.stack([np.asarray(r[k], dtype=np.float32) for r in rs], 0)
    y_prompt = cat("yp")
    y_sample = cat("ys").reshape(32, 64, 1024)
    kpo = cat("kp").reshape(1, 8, 2048, 8, 64)
    vpo = cat("vp").reshape(1, 8, 2048, 8, 64)
    cpo = cat("convp").reshape(1, 8, 30, 256)
    mko = cat("mkp").reshape(1, 8, 256, 4, 64)
    mvo = cat("mvp").reshape(1, 8, 256, 4, 64)
    kso = cat("ksn").reshape(1, 32, 64, 8, 64)
    vso = cat("vsn").reshape(1, 32, 64, 8, 64)
    cso = cat("convs").reshape(1, 32, 30, 256)
    return (y_prompt, y_sample, kpo, vpo, cpo, mko, mvo, kso, vso, cso)
```

```python
import numpy as np
from contextlib import ExitStack
import concourse.bass as bass
import concourse.mybir as mybir
from concourse.bass_utils import run_bass_kernel_spmd

F32 = mybir.dt.float32
BF16 = mybir.dt.bfloat16
I32 = mybir.dt.int32
AF = mybir.ActivationFunctionType
ALU = mybir.AluOpType

ENGS = ["pe", "act", "dve", "pool", "sp"]
NCORES = 8
ALPHA = 2.0 ** 0.25
EPS = 1e-5
SCALE = 0.125
NT = 2304
NP = 2048
BIG = 1.0e30


class Prog:
    def __init__(self, nc):
        self.nc = nc
        self.ins = {e: [] for e in ENGS}
        self.last_w = {}
        self.readers = {}
        self.dma_cnt = {}

    def _tok_deps(self, reads, writes):
        deps = []
        for r in reads:
            t = self.last_w.get(r)
            if t is not None:
                deps.append(t)
        for w in writes:
            t = self.last_w.get(w)
            if t is not None:
                deps.append(t)
            deps.extend(self.readers.get(w, ()))
        return deps

    def _add_waits(self, eng, deps):
        waits = []
        for t in deps:
            if t[0] == "dma":
                waits.append((("dma", t[1]), t[2] * 16))
            else:
                _, e2, idx = t
                if e2 == eng and eng == "pe":
                    continue
                self.ins[e2][idx]["flag"] = True
                waits.append((("eng", e2), idx))
        return waits

    def op(self, eng, fn, reads=(), writes=()):
        deps = self._tok_deps(reads, writes)
        waits = self._add_waits(eng, deps)
        idx = len(self.ins[eng])
        self.ins[eng].append(dict(fn=fn, waits=waits, flag=False, dma=None))
        tok = ("eng", eng, idx)
        for r in reads:
            lst = self.readers.setdefault(r, [])
            lst[:] = [t for t in lst if not (t[0] == "eng" and t[1] == eng)]
            lst.append(tok)
        for w in writes:
            self.last_w[w] = tok
            self.readers[w] = []
        return tok

    def dma(self, eng, fn, semkey, reads=(), writes=()):
        deps = self._tok_deps(reads, writes)
        waits = self._add_waits(eng, deps)
        cnt = self.dma_cnt.get(semkey, 0) + 1
        self.dma_cnt[semkey] = cnt
        self.ins[eng].append(dict(fn=fn, waits=waits, flag=False, dma=semkey))
        tok = ("dma", semkey, cnt)
        for r in reads:
            self.readers.setdefault(r, []).append(tok)
        for w in writes:
            self.last_w[w] = tok
            self.readers[w] = []
        return tok

    def barrier(self):
        lasts = {}
        for e in ENGS:
            for idx in range(len(self.ins[e]) - 1, -1, -1):
                ins = self.ins[e][idx]
                if ins["fn"] is not None and ins["dma"] is None:
                    lasts[e] = idx
                    break
        dmas = dict(self.dma_cnt)
        for e in ENGS:
            waits = []
            for e2, idx in lasts.items():
                if e2 != e:
                    self.ins[e2][idx]["flag"] = True
                    waits.append((("eng", e2), idx))
            for k, c in dmas.items():
                waits.append((("dma", k), c * 16))
            self.ins[e].append(dict(fn=None, waits=waits, flag=False, dma=None))

    def emit(self):
        nc = self.nc
        with ExitStack() as st:
            sems = {}
            for e in ENGS:
                sems[("eng", e)] = st.enter_context(nc.semaphore("s_" + e))
            for i, k in enumerate(self.dma_cnt):
                sems[("dma", k)] = st.enter_context(nc.semaphore("d%d" % i))
            cnts = {}
            for e in ENGS:
                c = 0
                arr = []
                for ins in self.ins[e]:
                    if ins["flag"]:
                        c += 1
                    arr.append(c)
                cnts[e] = arr
            block = st.enter_context(nc.Block())
            final_dma = [(sems[("dma", k)], c * 16) for k, c in self.dma_cnt.items()]

            def run(e, engobj):
                waited = {}
                for ins in self.ins[e]:
                    for key, v in ins["waits"]:
                        if key[0] == "eng":
                            v = cnts[key[1]][v]
                        if waited.get(key, 0) >= v:
                            continue
                        waited[key] = v
                        engobj.wait_ge(sems[key], v)
                    if ins["fn"] is None:
                        continue
                    r = ins["fn"](engobj)
                    if ins["dma"] is not None:
                        r.then_inc(sems[("dma", ins["dma"])], 16)
                    elif ins["flag"]:
                        r.then_inc(sems[("eng", e)], 1)
                if e == "sp":
                    for s, v in final_dma:
                        engobj.wait_ge(s, v)

            @block.tensor
            def _(eng):
                run("pe", eng)

            @block.scalar
            def _(eng):
                run("act", eng)

            @block.vector
            def _(eng):
                run("dve", eng)

            @block.gpsimd
            def _(eng):
                run("pool", eng)

            @block.sync
            def _(eng):
                run("sp", eng)


def build(nc, stop_after=99):
    P = Prog(nc)

    def din(name, shape):
        return nc.dram_tensor(name, list(shape), F32, kind="ExternalInput").ap()

    def dout(name, shape):
        return nc.dram_tensor(name, list(shape), F32, kind="ExternalOutput").ap()

    xp = din("xp", [NP, 1024]); xs = din("xs", [256, 1024]); memp = din("memp", [256, 1024])
    ck = din("ck", [4, 4096, 512]); cv = din("cv", [4, 4096, 512])
    cconv = din("cconv", [4, 30, 256]); cmk = din("cmk", [4, 256, 256]); cmv = din("cmv", [4, 256, 256])
    ln0_g = din("ln0_g", [1024]); ln0_b = din("ln0_b", [1024])
    w_in = din("w_in", [1024, 2304]); w_dw = din("w_dw", [31, 256]); b_dw = din("b_dw", [256])
    lnc_g = din("lnc_g", [256]); lnc_b = din("lnc_b", [256]); w_cpw = din("w_cpw", [256, 256])
    w_mk = din("w_mk", [1024, 256]); w_mv = din("w_mv", [1024, 256]); w_out = din("w_out", [1024, 1024])
    ln1_g = din("ln1_g", [1024]); ln1_b = din("ln1_b", [1024])
    w_rg = din("w_rg", [1024, 4]); b_rg = din("b_rg", [4]); w_re = din("w_re", [1024, 32]); b_re = din("b_re", [32])
    w_eg = din("w_eg", [32, 1024, 256]); w_eu = din("w_eu", [32, 1024, 256]); w_ed = din("w_ed", [32, 256, 1024])
    ln2_g = din("ln2_g", [1024]); ln2_b = din("ln2_b", [1024])

    yp = dout("yp", [NP, 1024]); ys = dout("ys", [256, 1024])
    kp = dout("kp", [NP, 512]); vp = dout("vp", [NP, 512]); convp = dout("convp", [30, 256])
    mkp = dout("mkp", [256, 256]); mvp = dout("mvp", [256, 256])
    ksn = dout("ksn", [256, 512]); vsn = dout("vsn", [256, 512]); convs = dout("convs", [4, 30, 256])
    XS = nc.dram_tensor("XS_scr", [50 * 256, 1028], BF16, kind="Internal").ap()
    YK = nc.dram_tensor("YK_scr", [2 * NT, 1024], F32, kind="Internal").ap()
    XNS = nc.dram_tensor("XN_scr", [NT, 1024], F32, kind="Internal").ap()
    WGB = nc.dram_tensor("WGB_scr", [4096, 2048], BF16, kind="Internal").ap()
    WUB = nc.dram_tensor("WUB_scr", [4096, 2048], BF16, kind="Internal").ap()
    WDB = nc.dram_tensor("WDB_scr", [4096, 2048], BF16, kind="Internal").ap()

    class Arena:
        def __init__(self, base, size):
            self.base, self.size, self.off = base, size, 0

        def t(self, name, shape, dtype):
            el = 2 if dtype == BF16 else 4
            n = 1
            for s in shape[1:]:
                n *= s
            sz = (n * el + 31) // 32 * 32
            assert self.off + sz <= self.size, (name, self.off, sz, self.size)
            h = nc.alloc_sbuf_tensor_at(name, list(shape), dtype, offset=self.base + self.off)
            self.off += sz
            return h.ap()

        def reset(self):
            self.off = 0

    BASE = 16640
    aXT = Arena(BASE, 36864)
    aMIX = Arena(BASE + 36864, 36864)
    CSZ = 11776
    aCONST = Arena(BASE + 73728, CSZ)
    aR = Arena(BASE + 73728 + CSZ, 73728)
    aFREE = Arena(BASE + 73728 + CSZ + 73728, 228640 - (BASE + 73728 + CSZ + 73728))

    XT = aXT.t("XT", [128, 8, NT], BF16)
    MIXT = aMIX.t("MIXT", [128, 8, NT], BF16)
    R = aR.t("R", [128, 18, 1024], F32)
    GATE = aCONST.t("GATE", [128, 18, 32], F32)
    ident = aCONST.t("ident", [128, 128], BF16)
    identF = aCONST.t("identF", [128, 128], F32)
    Uinc = aCONST.t("Uinc", [128, 128], BF16)
    ones = aCONST.t("ones", [128, 128], BF16)
    onesF = aCONST.t("onesF", [128, 128], F32)
    tri = aCONST.t("tri", [128, 128], F32)
    tri8 = aCONST.t("tri8", [128, 8, 64], F32)
    wdw = aCONST.t("wdw", [128, 2, 31], F32)
    cpar = aCONST.t("cpar", [128, 3, 2], F32)
    W_CPW = aCONST.t("W_CPW", [128, 2, 256], BF16)
    W_R = aCONST.t("W_R", [128, 8, 36], BF16)
    RB = aCONST.t("RB", [128, 36], F32)
    small = aCONST.t("small", [128, 64], F32)
    small2 = aCONST.t("small2", [128, 64], F32)
    IDXW = aCONST.t("IDXW", [128, 50], I32)

    ps = [nc.alloc_psum_tensor("ps%d" % i, [128, 512], F32) for i in range(8)]
    psf = [p.ap() for p in ps]
    psb = [p.bitcast(BF16).ap() for p in ps]

    def PS(i):
        return ("ps", i)

    def pool(fn, reads=(), writes=()):
        return P.op("pool", fn, reads, writes)

    pool(lambda e: e.memset(ident, 0.0), writes=["ident"])
    pool(lambda e: e.affine_select(out=ident, in_=ident, pattern=[[-1, 128]], compare_op=ALU.not_equal,
                                   fill=1.0, base=0, channel_multiplier=1), reads=["ident"], writes=["ident"])
    pool(lambda e: e.memset(identF, 0.0), writes=["identF"])
    pool(lambda e: e.affine_select(out=identF, in_=identF, pattern=[[-1, 128]], compare_op=ALU.not_equal,
                                   fill=1.0, base=0, channel_multiplier=1), reads=["identF"], writes=["identF"])
    aR.reset()
    W_IN = aR.t("W_IN", [128, 8, 2304], BF16)
    FILLR = aR.t("FILLR", [128, 1028], BF16)
    for c in range(8):
        P.dma("pool", lambda e, c=c: e.dma_start(out=W_IN[:, c, :], in_=w_in[c * 128:(c + 1) * 128, :]), "W_IN", writes=["W_IN"])
    WIN_ALL = [("W_IN", c) for c in range(8)]

    pool(lambda e: e.memset(ones, 1.0), writes=["ones"])
    pool(lambda e: e.memset(onesF, 1.0), writes=["onesF"])
    pool(lambda e: e.memset(Uinc, 1.0), writes=["Uinc"])
    pool(lambda e: e.affine_select(out=Uinc, in_=Uinc, pattern=[[-1, 128]], compare_op=ALU.is_ge,
                                   fill=0.0, base=0, channel_multiplier=1), reads=["Uinc"], writes=["Uinc"])
    pool(lambda e: e.memset(tri, 1.0), writes=["tri"])
    pool(lambda e: e.affine_select(out=tri, in_=tri, pattern=[[1, 128]], compare_op=ALU.is_gt,
                                   fill=0.0, base=0, channel_multiplier=-1), reads=["tri"], writes=["tri"])
    pool(lambda e: e.memset(tri8, 1.0), writes=["tri8"])
    pool(lambda e: e.affine_select(out=tri8[0:64], in_=tri8[0:64], pattern=[[0, 8], [1, 64]], compare_op=ALU.is_gt,
                                   fill=0.0, base=0, channel_multiplier=-1), reads=["tri8"], writes=["tri8"])
    P.dma("pool", lambda e: e.dma_start(out=tri8[64:128], in_=tri8[0:64]), "tri8", reads=["tri8"], writes=["tri8"])

    def xrows(t):
        return xp[t * 128:(t + 1) * 128, :] if t < 16 else xs[(t - 16) * 128:(t - 15) * 128, :]

    def tcols(t):
        return slice(t * 128, (t + 1) * 128)

    ln_ctr = [0]

    def layernorm(tag, src, src_res, dst, dst_res, G, B, gres, out_scale=None):
        k = ln_ctr[0] % 2
        ln_ctr[0] += 1
        sm = small if k == 0 else small2
        smr = ("small", k)
        st6 = sm[:, 0:12].rearrange("p (a b) -> p a b", b=6)
        mv = sm[:, 12:14]
        rs = sm[:, 14:15]
        P.op("dve", lambda e: e.bn_stats(out=st6[:, 0, :], in_=src[:, 0:512]), reads=[src_res], writes=[smr])
        P.op("dve", lambda e: e.bn_stats(out=st6[:, 1, :], in_=src[:, 512:1024]), reads=[src_res], writes=[smr])
        P.op("dve", lambda e: e.bn_aggr(out=mv, in_=st6), reads=[smr], writes=[smr])
        P.op("dve", lambda e: e.tensor_scalar(out=rs, in0=mv[:, 1:2], scalar1=EPS, scalar2=None, op0=ALU.add),
             reads=[smr], writes=[smr])
        P.op("act", lambda e: e.activation(out=rs, in_=rs, func=AF.Ln), reads=[smr], writes=[smr])
        P.op("act", lambda e: e.activation(out=rs, in_=rs, func=AF.Exp, scale=-0.5), reads=[smr], writes=[smr])
        P.op("dve", lambda e: e.scalar_tensor_tensor(out=src, in0=src, scalar=mv[:, 0:1], in1=G, op0=ALU.subtract, op1=ALU.mult),
             reads=[smr, src_res] + gres, writes=[src_res])
        P.op("dve", lambda e: e.scalar_tensor_tensor(out=dst, in0=src, scalar=rs, in1=B, op0=ALU.mult, op1=ALU.add),
             reads=[smr, src_res] + gres, writes=[dst_res])

    aFREE.reset()
    aMIX.reset()
    XIN = [aMIX.t("XIN%d" % i, [128, 1024], F32) for i in range(3)]
    XNB = [aMIX.t("XNB%d" % i, [128, 1024], BF16) for i in range(2)]
    LNG = aMIX.t("LNG", [128, 1024], F32)
    LNB = aMIX.t("LNB", [128, 1024], F32)
    P.dma("sp", lambda e: e.dma_start(out=LNG, in_=ln0_g.partition_broadcast(128)), "LNG", writes=["LNG"])
    P.dma("sp", lambda e: e.dma_start(out=LNB, in_=ln0_b.partition_broadcast(128)), "LNB", writes=["LNB"])
    def transpose_to_XT(t, srcb, src_res, bank):
        for c in range(8):
            P.op("pe", lambda e, c=c: e.transpose(psb[bank][:, c * 128:(c + 1) * 128], srcb[:, c * 128:(c + 1) * 128], ident),
                 reads=[src_res, "ident"], writes=[PS(bank)])
        P.op("act", lambda e: e.copy(out=XT[:, :, tcols(t)], in_=psb[bank].rearrange("p (c n) -> p c n", n=128)),
             reads=[PS(bank)], writes=[("XT", t)])

    KST = [aMIX.t("KST%d" % i, [128, 512], F32) for i in range(2)]
    VST = [aMIX.t("VST%d" % i, [128, 512], F32) for i in range(2)]
    aFREE.reset()
    V_ALL = aFREE.t("V_ALL", [128, 18, 512], BF16)
    aA_base = aFREE.off

    def p1_load(t):
        s3 = t % 3
        P.dma("sp", lambda e: e.dma_start(out=XIN[s3], in_=xrows(t)), ("XIN", s3), writes=[("XIN", s3)])

    def p2a_mm(t):
        s = t % 2
        for (which, col0, bank) in (("k", 512, 0 + s), ("v", 1024, 2 + s)):
            for c in range(8):
                P.op("pe", lambda e, c=c, col0=col0, bank=bank: e.matmul(
                    psf[bank], lhsT=XT[:, c, tcols(t)], rhs=W_IN[:, c, col0:col0 + 512], start=(c == 0), stop=(c == 7)),
                    reads=[("XT", t), "W_IN"], writes=[PS(bank)])

    def p2a_evac(t):
        s = t % 2
        P.op("act", lambda e: e.copy(out=KST[s], in_=psf[0 + s]), reads=[PS(0 + s)], writes=[("KST", s)])
        P.op("act", lambda e: e.copy(out=VST[s], in_=psf[2 + s]), reads=[PS(2 + s)], writes=[("VST", s)])
        P.op("act", lambda e: e.copy(out=V_ALL[:, t, :], in_=psf[2 + s]), reads=[PS(2 + s)], writes=[("V_ALL", t)])
        kdst = kp[t * 128:(t + 1) * 128, :] if t < 16 else ksn[(t - 16) * 128:(t - 15) * 128, :]
        vdst = vp[t * 128:(t + 1) * 128, :] if t < 16 else vsn[(t - 16) * 128:(t - 15) * 128, :]
        P.dma("pool", lambda e: e.dma_start(out=kdst, in_=KST[s]), ("st", "KST", s), reads=[("KST", s)])
        P.dma("pool", lambda e: e.dma_start(out=vdst, in_=VST[s]), ("st", "VST", s), reads=[("VST", s)])

    p1_load(0)
    p1_load(1)
    WDWT = aCONST.t("WDWT", [31, 256], F32)
    P.dma("sp", lambda e: e.dma_start(out=WDWT, in_=w_dw), "WDWT", writes=["WDWT"])
    for cc in range(2):
        P.op("pe", lambda e, cc=cc: e.transpose(psf[0][:, cc * 32:cc * 32 + 31], WDWT[:, cc * 128:(cc + 1) * 128], identF[0:31, 0:31]),
             reads=["WDWT", "identF"], writes=[PS(0)])
    P.op("act", lambda e: e.copy(out=wdw, in_=psf[0][:, 0:64].rearrange("p (c n) -> p c n", n=32)[:, :, 0:31]),
         reads=[PS(0)], writes=["wdw"])
    for i, v in enumerate([b_dw, lnc_g, lnc_b]):
        P.dma("sp", lambda e, i=i, v=v: e.dma_start(out=cpar[:, i, :], in_=v.rearrange("(c p) -> p c", p=128),
                                                     allow_slow_non_contiguous=True), "cpar", writes=["cpar"])
    P.dma("pool", lambda e: e.dma_start(out=W_CPW, in_=w_cpw.rearrange("(c p) f -> p c f", p=128)), "W_CPW", writes=["W_CPW"])
    P.dma("pool", lambda e: e.dma_start(out=W_R[:, :, 0:4], in_=w_rg.rearrange("(c p) f -> p c f", p=128)), "W_R", writes=["W_R"])
    P.dma("pool", lambda e: e.dma_start(out=W_R[:, :, 4:36], in_=w_re.rearrange("(c p) f -> p c f", p=128)), "W_R", writes=["W_R"])
    P.dma("sp", lambda e: e.dma_start(out=RB[:, 0:4], in_=b_rg.partition_broadcast(128)), "RB", writes=["RB"])
    P.dma("sp", lambda e: e.dma_start(out=RB[:, 4:36], in_=b_re.partition_broadcast(128)), "RB", writes=["RB"])

    for t in range(20):
        if t < 18:
            s = t % 2
            s3 = t % 3
            layernorm("ln0", XIN[s3], ("XIN", s3), XIN[s3], ("XIN", s3), LNG, LNB, ["LNG", "LNB"])
        if t >= 2:
            p2a_evac(t - 2)
        if t < 18:
            P.op("act", lambda e, s=s, s3=s3: e.copy(out=XNB[s], in_=XIN[s3]), reads=[("XIN", s3)], writes=[("XNB", s)])
            P.dma("pool", lambda e, t=t, s3=s3: e.dma_start(out=XNS[t * 128:(t + 1) * 128, :], in_=XIN[s3]), ("st", "XIN", s3), reads=[("XIN", s3)], writes=[("XNS", t)])
            transpose_to_XT(t, XNB[s], ("XNB", s), 6 + s)
            if t + 2 < 18:
                p1_load(t + 2)
        if 1 <= t < 19:
            p2a_mm(t - 1)
    P.op("pool", lambda e: e.memset(FILLR, 0.0), writes=["FILLR"])
    P.op("pool", lambda e: e.memset(FILLR[:, 1024:1026].bitcast(I32), 1 << 24), reads=["FILLR"], writes=["FILLR"])
    for a_ in range(10):
        P.dma("sp", lambda e, a_=a_: e.dma_start(out=XS[a_ * 1280:(a_ + 1) * 1280, :].rearrange("(a p) f -> p a f", p=128),
                                                 in_=FILLR.unsqueeze(1).to_broadcast([128, 10, 1028])), "XSfill", reads=["FILLR"], writes=["XSfill"])
    XT_ALL = [("XT", t) for t in range(18)]
    if stop_after <= 2:
        P.emit(); return nc

    QT = aR.t("QT", [128, 4, NP], BF16)
    KT = aR.t("KT", [128, 4, NP], BF16)
    QS = aFREE.t("QS", [128, 4, 256], BF16)
    KS = aFREE.t("KS", [128, 4, 256], BF16)
    aA_base = aFREE.off
    GW = 2078 + 4 * 94
    GLU = aFREE.t("GLU", [128, 2, GW], F32)
    MQT = aFREE.t("MQT", [128, 2, NT], BF16)
    SIG = [aMIX.t("SIG%d" % i, [128, 512], F32) for i in range(2)]
    blocks = [(0, 512), (512, 1024), (1024, 1536), (1536, 2048), (2048, 2304)]
    bankrr = [0]

    def inproj_fm(j, n0, n1, bank):
        N = n1 - n0
        for c in range(8):
            P.op("pe", lambda e, c=c: e.matmul(psf[bank][:, 0:N], lhsT=W_IN[:, c, j * 128:(j + 1) * 128], rhs=XT[:, c, n0:n1],
                                               start=(c == 0), stop=(c == 7)),
                 reads=["W_IN"] + [("XT", t) for t in range(n0 // 128, n1 // 128)], writes=[PS(bank)])

    def sample_glu_view(cc):
        return GLU[:, cc, 2078:2078 + 376].rearrange("p (s w) -> p s w", w=94)[:, :, 30:94]

    for bi, (n0, n1) in enumerate(blocks):
        N = n1 - n0
        for j in list(range(8)) + [16, 17]:
            bank = bankrr[0] % 4
            bankrr[0] += 1
            inproj_fm(j, n0, n1, bank)
            if j < 4:
                dst, dres = (QT[:, j, n0:n1], ("QT", j, bi)) if bi < 4 else (QS[:, j, :], ("QS", j))
            elif j < 8:
                dst, dres = (KT[:, j - 4, n0:n1], ("KT", j - 4, bi)) if bi < 4 else (KS[:, j - 4, :], ("KS", j - 4))
            else:
                dst, dres = MQT[:, j - 16, n0:n1], ("MQT", j - 16, bi)
            if (bankrr[0] % 2) == 0:
                P.op("act", lambda e, dst=dst, bank=bank, N=N: e.copy(out=dst, in_=psf[bank][:, 0:N]), reads=[PS(bank)], writes=[dres])
            else:
                P.op("dve", lambda e, dst=dst, bank=bank, N=N: e.tensor_copy(out=dst, in_=psf[bank][:, 0:N]), reads=[PS(bank)], writes=[dres])
        for cc in range(2):
            b1, b2 = 4 + 2 * (cc % 2), 5 + 2 * (cc % 2)
            inproj_fm(12 + cc, n0, n1, b1)
            inproj_fm(14 + cc, n0, n1, b2)
            sg = SIG[cc]
            P.op("act", lambda e, sg=sg, b2=b2, N=N: e.activation(out=sg[:, 0:N], in_=psf[b2][:, 0:N], func=AF.Sigmoid),
                 reads=[PS(b2)], writes=[("SIG", cc)])
            if bi < 4:
                P.op("dve", lambda e, sg=sg, b1=b1, cc=cc, n0=n0, N=N: e.tensor_tensor(
                    out=GLU[:, cc, 30 + n0:30 + n0 + N], in0=psf[b1][:, 0:N], in1=sg[:, 0:N], op=ALU.mult),
                    reads=[PS(b1), ("SIG", cc)], writes=[("GLU", cc)])
            else:
                P.op("dve", lambda e, sg=sg, b1=b1, cc=cc: e.tensor_tensor(
                    out=sample_glu_view(cc), in0=psf[b1][:, 0:256].rearrange("p (s w) -> p s w", w=64),
                    in1=sg[:, 0:256].rearrange("p (s w) -> p s w", w=64), op=ALU.mult),
                    reads=[PS(b1), ("SIG", cc)], writes=[("GLU", cc)])
    if stop_after <= 3:
        P.emit(); return nc

    P.barrier()
    aMIX.reset()
    DIAG = aMIX.t("DIAG", [128, 62, 128], BF16)
    assert aMIX.off <= 18432, aMIX.off
    for cc in range(2):
        for j in range(31):
            P.op("dve", lambda e, cc=cc, j=j: e.tensor_scalar(out=DIAG[:, cc * 31 + j, :], in0=identF, scalar1=wdw[:, cc, j:j + 1],
                                                              scalar2=None, op0=ALU.mult), reads=["identF", "wdw"], writes=["DIAG"])
    aW = Arena(aR.base, 36864)
    MEMB = aW.t("MEMB", [128, 2, 1024], BF16)
    MEMT = aW.t("MEMT", [128, 8, 256], BF16)
    W_MK = aW.t("W_MK", [128, 8, 256], BF16)
    W_MV = aW.t("W_MV", [128, 8, 256], BF16)
    MKT = aW.t("MKT", [128, 2, 256], BF16)
    MV = aW.t("MV", [128, 2, 256], BF16)
    MST = [aW.t("MST%d" % i, [128, 256], F32) for i in range(2)]
    PT = [aW.t("PT%d" % i, [128, 512], BF16) for i in range(2)]
    RDEN = aW.t("RDEN", [128, 512], F32)
    P.dma("pool", lambda e: e.dma_start(out=MEMB, in_=memp.rearrange("(m p) d -> p m d", p=128)), "MEMB", writes=["MEMB"])
    P.dma("pool", lambda e: e.dma_start(out=W_MK, in_=w_mk.rearrange("(c p) f -> p c f", p=128)), "W_MK", writes=["W_MK"])
    P.dma("pool", lambda e: e.dma_start(out=W_MV, in_=w_mv.rearrange("(c p) f -> p c f", p=128)), "W_MV", writes=["W_MV"])
    for mt in range(2):
        for c in range(8):
            P.op("pe", lambda e, mt=mt, c=c: e.transpose(psb[6][:, c * 128:(c + 1) * 128], MEMB[:, mt, c * 128:(c + 1) * 128], ident),
                 reads=["MEMB", "ident"], writes=[PS(6)])
        P.op("act", lambda e, mt=mt: e.copy(out=MEMT[:, :, mt * 128:(mt + 1) * 128], in_=psb[6].rearrange("p (c n) -> p c n", n=128)),
             reads=[PS(6)], writes=["MEMT"])
    for wi, (W, wres, dstout) in enumerate(((W_MK, "W_MK", mkp), (W_MV, "W_MV", mvp))):
        for mt in range(2):
            bank = (wi * 2 + mt) % 4
            for c in range(8):
                P.op("pe", lambda e, W=W, mt=mt, c=c, bank=bank: e.matmul(psf[bank][:, 0:256], lhsT=MEMT[:, c, mt * 128:(mt + 1) * 128],
                                                                           rhs=W[:, c, :], start=(c == 0), stop=(c == 7)),
                     reads=["MEMT", wres], writes=[PS(bank)])
            s = mt
            P.op("act", lambda e, s=s, bank=bank: e.copy(out=MST[s], in_=psf[bank][:, 0:256]), reads=[PS(bank)], writes=[("MST", s)])
            if wi == 1:
                P.op("pool", lambda e, mt=mt, s=s: e.tensor_copy(out=MV[:, mt, :], in_=MST[s]), reads=[("MST", s)], writes=["MV"])
            P.dma("sp", lambda e, s=s, dstout=dstout, mt=mt: e.dma_start(out=dstout[mt * 128:(mt + 1) * 128, :], in_=MST[s]),
                  ("st", "MST", s), reads=[("MST", s)])
    for fc in range(2):
        bank = 4 + fc
        for c in range(8):
            P.op("pe", lambda e, fc=fc, c=c, bank=bank: e.matmul(psf[bank][:, 0:256], lhsT=W_MK[:, c, fc * 128:(fc + 1) * 128],
                                                                  rhs=MEMT[:, c, :], start=(c == 0), stop=(c == 7)),
                 reads=["MEMT", "W_MK"], writes=[PS(bank)])
        P.op("act", lambda e, fc=fc, bank=bank: e.copy(out=MKT[:, fc, :], in_=psf[bank][:, 0:256]), reads=[PS(bank)], writes=["MKT"])

    def mem_attn(mkt, mkt_res, mvv, mv_res, n0, N, mq_res):
        for fc in range(2):
            bO, bD = 4 + fc, 6 + fc
            for hh in range(2):
                h = 2 * fc + hh
                pr = slice(64 * hh, 64 * hh + 64)
                for mt in range(2):
                    bS = mt
                    P.op("pe", lambda e, mt=mt, bS=bS, pr=pr, fc=fc: e.matmul(psf[bS][:, 0:N], lhsT=mkt[pr, fc, mt * 128:(mt + 1) * 128],
                                                                       rhs=MQT[pr, fc, n0:n0 + N], start=True, stop=True),
                         reads=[mkt_res] + mq_res, writes=[PS(bS)])
                    P.op("act", lambda e, mt=mt, bS=bS: e.activation(out=PT[mt][:, 0:N], in_=psf[bS][:, 0:N], func=AF.Exp, scale=SCALE),
                         reads=[PS(bS)], writes=[("PT", mt)])
                for mt in range(2):
                    P.op("pe", lambda e, mt=mt, pr=pr, h=h, bO=bO: e.matmul(psf[bO][pr, 0:N], lhsT=mvv[:, mt, h * 64:(h + 1) * 64], rhs=PT[mt][:, 0:N],
                                                                     start=(mt == 0), stop=(mt == 1)),
                         reads=[mv_res, ("PT", mt)], writes=[PS(bO)])
                for mt in range(2):
                    P.op("pe", lambda e, mt=mt, pr=pr, bD=bD: e.matmul(psf[bD][pr, 0:N], lhsT=ones[:, 0:64], rhs=PT[mt][:, 0:N],
                                                                start=(mt == 0), stop=(mt == 1)),
                         reads=["ones", ("PT", mt)], writes=[PS(bD)])
            P.op("act", lambda e, bD=bD: e.activation(out=RDEN[:, 0:N], in_=psf[bD][:, 0:N], func=AF.Ln), reads=[PS(bD)], writes=["RDEN"])
            P.op("act", lambda e: e.activation(out=RDEN[:, 0:N], in_=RDEN[:, 0:N], func=AF.Exp, scale=-1.0), reads=["RDEN"], writes=["RDEN"])
            P.op("dve", lambda e, bO=bO, fc=fc: e.tensor_tensor(out=MIXT[:, 6 + fc, n0:n0 + N], in0=psf[bO][:, 0:N], in1=RDEN[:, 0:N], op=ALU.mult),
                 reads=[PS(bO), "RDEN"], writes=[("MIXT", 6 + fc, n0)])

    for bi in range(4):
        mem_attn(MKT, "MKT", MV, "MV", bi * 512, 512, [("MQT", 0, bi), ("MQT", 1, bi)])
    if stop_after <= 3.2:
        P.emit(); return nc
    CMK = [aW.t("CMK%d" % i, [128, 2, 256], BF16) for i in range(2)]
    CMV = [aW.t("CMV%d" % i, [128, 2, 256], BF16) for i in range(2)]
    CMKT = [aW.t("CMKT%d" % i, [128, 2, 256], BF16) for i in range(2)]
    for s in range(4):
        sl = s % 2
        P.dma("pool", lambda e, s=s, sl=sl: e.dma_start(out=CMK[sl], in_=cmk[s].rearrange("(m p) f -> p m f", p=128)), ("CMK", sl), writes=[("CMK", sl)])
        P.dma("pool", lambda e, s=s, sl=sl: e.dma_start(out=CMV[sl], in_=cmv[s].rearrange("(m p) f -> p m f", p=128)), ("CMV", sl), writes=[("CMV", sl)])
        for mt in range(2):
            for fc in range(2):
                P.op("pe", lambda e, mt=mt, fc=fc, sl=sl: e.transpose(psb[3][:, (mt * 2 + fc) * 128:(mt * 2 + fc + 1) * 128],
                                                                      CMK[sl][:, mt, fc * 128:(fc + 1) * 128], ident),
                     reads=[("CMK", sl), "ident"], writes=[PS(3)])
        P.op("act", lambda e, sl=sl: e.copy(out=CMKT[sl].rearrange("p f (m n) -> p m f n", n=128),
                                            in_=psb[3][:, 0:512].rearrange("p (m f n) -> p m f n", f=2, n=128)),
             reads=[PS(3)], writes=[("CMKT", sl)])
        mem_attn(CMKT[sl], ("CMKT", sl), CMV[sl], ("CMV", sl), 2048 + 64 * s, 64, [("MQT", 0, 4), ("MQT", 1, 4)])

    if stop_after <= 3.4:
        P.emit(); return nc
    P.barrier()
    aW.reset()
    GLUB = aW.t("GLUB", [128, 2, GW], BF16)
    CT = aW.t("CT", [30, 256], F32)
    CST = [aW.t("CST%d" % i, [30, 256], F32) for i in range(2)]
    DW = [aW.t("DW%d" % i, [128, 512], F32) for i in range(4)]
    SQ = [aW.t("SQ%d" % i, [128, 512], F32) for i in range(2)]
    MEANT = aW.t("MEANT", [128, 512], F32)
    RSTD = aW.t("RSTD", [128, 512], F32)
    SU = [aW.t("SU%d" % i, [128, 512], BF16) for i in range(4)]
    cblk = [0]
    segs = [(0, 2048)] + [(2078 + 94 * s, 64) for s in range(4)]
    P.op("pool", lambda e: e.memset(GLU[:, :, 0:30], 0.0), writes=[("GLU", 0), ("GLU", 1)])
    for s in range(4):
        P.dma("sp", lambda e, s=s: e.dma_start(out=CT, in_=cconv[s]), "CT", writes=["CT"])
        for cc in range(2):
            P.op("pe", lambda e, cc=cc: e.transpose(psf[0][:, cc * 32:cc * 32 + 30], CT[:, cc * 128:(cc + 1) * 128], identF[0:30, 0:30]),
                 reads=["CT", "identF"], writes=[PS(0)])
        P.op("act", lambda e, s=s: e.copy(out=GLU[:, :, 2078 + 94 * s:2078 + 94 * s + 30],
                                          in_=psf[0][:, 0:64].rearrange("p (c n) -> p c n", n=32)[:, :, 0:30]),
             reads=[PS(0)], writes=[("GLU", 0), ("GLU", 1)])
    P.op("act", lambda e: e.copy(out=GLUB[:, 0, :], in_=GLU[:, 0, :]), reads=[("GLU", 0)], writes=[("GLUB", 0)])
    P.op("pool", lambda e: e.tensor_copy(out=GLUB[:, 1, :], in_=GLU[:, 1, :]), reads=[("GLU", 1)], writes=[("GLUB", 1)])
    for si, (gb, ln) in enumerate(segs):
        sl = si % 2
        for cc in range(2):
            P.op("pe", lambda e, cc=cc, gb=gb, ln=ln: e.transpose(psf[1][0:30, cc * 128:(cc + 1) * 128], GLU[:, cc, gb + ln:gb + ln + 30], identF),
                 reads=[("GLU", cc), "identF"], writes=[PS(1)])
        P.op("act", lambda e, sl=sl: e.copy(out=CST[sl], in_=psf[1][0:30, 0:256]), reads=[PS(1)], writes=[("CST", sl)])
        dst = convp if si == 0 else convs[si - 1]
        P.dma("sp", lambda e, sl=sl, dst=dst: e.dma_start(out=dst, in_=CST[sl]), ("st", "CST", sl), reads=[("CST", sl)])

    if stop_after <= 3.6:
        P.emit(); return nc

    def conv_s1(gb, t0, N, tok0):
        pb = cblk[0] % 2
        cblk[0] += 1
        DWl = [DW[2 * pb], DW[2 * pb + 1]]
        SUl = [SU[2 * pb], SU[2 * pb + 1]]
        for cc in range(2):
            dw = DWl[cc]
            for j in range(31):
                P.op("pe", lambda e, cc=cc, j=j: e.matmul(psf[cc][:, 0:N], lhsT=DIAG[:, cc * 31 + j, :], rhs=GLUB[:, cc, gb + t0 + j:gb + t0 + j + N],
                                                          start=(j == 0), stop=(j == 30)), reads=["DIAG", ("GLUB", cc)], writes=[PS(cc)])
            P.op("act", lambda e, cc=cc, dw=dw: e.activation(out=dw[:, 0:N], in_=psf[cc][:, 0:N], func=AF.Identity, bias=cpar[:, 0, cc:cc + 1]),
                 reads=[PS(cc), "cpar"], writes=[("DW", cc, pb)])
            P.op("act", lambda e, cc=cc, dw=dw: e.activation(out=SQ[cc][:, 0:N], in_=dw[:, 0:N], func=AF.Square),
                 reads=[("DW", cc, pb)], writes=[("SQ", cc)])
        return (pb, DWl, SUl)

    def conv_s2(ctx, gb, t0, N, tok0):
        pb, DWl, SUl = ctx
        for cc in range(2):
            P.op("pe", lambda e, cc=cc: e.matmul(psf[2][:, 0:N], lhsT=onesF, rhs=DWl[cc][:, 0:N], start=(cc == 0), stop=(cc == 1)),
                 reads=["onesF", ("DW", cc, pb)], writes=[PS(2)])
        for cc in range(2):
            P.op("pe", lambda e, cc=cc: e.matmul(psf[3][:, 0:N], lhsT=onesF, rhs=SQ[cc][:, 0:N], start=(cc == 0), stop=(cc == 1)),
                 reads=["onesF", ("SQ", cc)], writes=[PS(3)])
        P.op("dve", lambda e: e.tensor_scalar(out=MEANT[:, 0:N], in0=psf[2][:, 0:N], scalar1=1.0 / 256, scalar2=None, op0=ALU.mult),
             reads=[PS(2)], writes=["MEANT"])
        P.op("dve", lambda e: e.tensor_tensor(out=RSTD[:, 0:N], in0=MEANT[:, 0:N], in1=MEANT[:, 0:N], op=ALU.mult),
             reads=["MEANT"], writes=["RSTD"])
        P.op("dve", lambda e: e.scalar_tensor_tensor(out=RSTD[:, 0:N], in0=psf[3][:, 0:N], scalar=1.0 / 256, in1=RSTD[:, 0:N],
                                                     op0=ALU.mult, op1=ALU.subtract), reads=[PS(3), "RSTD"], writes=["RSTD"])
        P.op("dve", lambda e: e.tensor_scalar(out=RSTD[:, 0:N], in0=RSTD[:, 0:N], scalar1=EPS, scalar2=None, op0=ALU.add),
             reads=["RSTD"], writes=["RSTD"])
        P.op("act", lambda e: e.activation(out=RSTD[:, 0:N], in_=RSTD[:, 0:N], func=AF.Ln), reads=["RSTD"], writes=["RSTD"])
        P.op("act", lambda e: e.activation(out=RSTD[:, 0:N], in_=RSTD[:, 0:N], func=AF.Exp, scale=-0.5), reads=["RSTD"], writes=["RSTD"])
        for cc in range(2):
            dw = DWl[cc]
            P.op("dve", lambda e, dw=dw: e.tensor_tensor(out=dw[:, 0:N], in0=dw[:, 0:N], in1=MEANT[:, 0:N], op=ALU.subtract),
                 reads=[("DW", cc, pb), "MEANT"], writes=[("DW", cc, pb)])
            P.op("pool", lambda e, dw=dw: e.tensor_tensor(out=dw[:, 0:N], in0=dw[:, 0:N], in1=RSTD[:, 0:N], op=ALU.mult),
                 reads=[("DW", cc, pb), "RSTD"], writes=[("DW", cc, pb)])
            P.op("act", lambda e, dw=dw, cc=cc: e.activation(out=SUl[cc][:, 0:N], in_=dw[:, 0:N], func=AF.Silu,
                                                             scale=cpar[:, 1, cc:cc + 1], bias=cpar[:, 2, cc:cc + 1]),
                 reads=[("DW", cc, pb), "cpar"], writes=[("SU", cc, pb)])

    def conv_s3(ctx, gb, t0, N, tok0):
        pb, DWl, SUl = ctx
        for oc in range(2):
            bank = 4 + oc
            for cc in range(2):
                P.op("pe", lambda e, oc=oc, cc=cc, bank=bank: e.matmul(psf[bank][:, 0:N], lhsT=W_CPW[:, cc, oc * 128:(oc + 1) * 128],
                                                                        rhs=SUl[cc][:, 0:N], start=(cc == 0), stop=(cc == 1)),
                     reads=["W_CPW", ("SU", cc, pb)], writes=[PS(bank)])
            P.op("act", lambda e, oc=oc, bank=bank: e.copy(out=MIXT[:, 4 + oc, tok0:tok0 + N], in_=psf[bank][:, 0:N]),
                 reads=[PS(bank)], writes=[("MIXT", 4 + oc, tok0)])

    cblocks = [(0, b_ * 512, 512, b_ * 512) for b_ in range(4)] + [(2078 + 94 * s_, 0, 64, 2048 + 64 * s_) for s_ in range(4)]
    cctx = {}
    for k_ in range(len(cblocks) + 2):
        if 0 <= k_ - 1 < len(cblocks):
            conv_s2(cctx[k_ - 1], *cblocks[k_ - 1])
        if k_ < len(cblocks):
            cctx[k_] = conv_s1(*cblocks[k_])
        if 0 <= k_ - 2 < len(cblocks):
            conv_s3(cctx[k_ - 2], *cblocks[k_ - 2])
    if stop_after <= 4:
        P.emit(); return nc

    P.barrier()
    WOUT_OFF = 24576
    assert WOUT_OFF >= aA_base and WOUT_OFF + 16384 <= aFREE.off
    W_OUT = nc.alloc_sbuf_tensor_at("W_OUT", [128, 8, 1024], BF16, offset=aFREE.base + WOUT_OFF).ap()
    for c in range(8):
        P.dma("pool", lambda e, c=c: e.dma_start(out=W_OUT[:, c, :], in_=w_out[c * 128:(c + 1) * 128, :]), "W_OUT", writes=["W_OUT"])
    aA = Arena(aR.base, 36864)
    NSL = 4
    E = [aA.t("E%d" % i, [128, 512], BF16) for i in range(NSL)]
    SPb = [aA.t("SP%d" % i, [128, 512], BF16) for i in range(NSL)]
    Xb = [aA.t("X%d" % i, [128, 512], BF16) for i in range(NSL)]
    Wb = Xb
    SACC = [aA.t("SACC%d" % i, [128, 512], BF16) for i in range(4)]
    KC = [aA.t("KC%d" % i, [128, 512], BF16) for i in range(5)]
    VC = [aA.t("VC%d" % i, [128, 512], BF16) for i in range(10)]
    KCT = [aA.t("KCT%d" % i, [128, 4, 128], BF16) for i in range(2)]
    QBD = [aA.t("QBD%d" % i, [128, 4, 128], BF16) for i in range(2)]
    FSRC = aA.t("FSRC", [128, 512], BF16)
    P.op("dve", lambda e: e.memset(FSRC, 1.0), writes=["FSRC"])

    items = []
    gi = 0
    for hp in range(4):
        for Q in range(4):
            for hh in range(2):
                nkb = 4 * Q + 4
                for kb in range(nkb - 1, -1, -1):
                    items.append(dict(kind="p", hp=hp, Q=Q, hh=hh, kb=kb, first=(kb == nkb - 1), last=(kb == 0),
                                      grp=gi, sacc=(gi * 2 + hh) % 2, obank=4 + (gi % 2), endgrp=(hh == 1 and kb == 0)))
            gi += 1
    for s in range(4):
        for kb in range(32, -1, -1):
            items.append(dict(kind="s", s=s, kb=kb, first=(kb == 32), last=(kb == 0), grp=gi, sacc=gi % 2,
                              obank=4 + (gi % 2), endgrp=(kb == 0)))
        gi += 1
    gcount = {}
    for it in items:
        n = gcount.get((it["grp"], it.get("hh", 0)), 0)
        gcount[(it["grp"], it.get("hh", 0))] = n + 1
        gpar = it["hh"] if it["kind"] == "p" else it["grp"] % 2
        it["sa_cur"] = gpar * 2 + (n % 2)
        it["sa_nxt"] = gpar * 2 + ((n + 1) % 2)
        it["gpar"] = gpar
    for i, it in enumerate(items):
        it["i"] = i
        it["sl"] = i % NSL
        it["zb"] = i % 2
        it["sb"] = 2 + (i % 2)

    def build_qbd(s_):
        q = QBD[s_ % 2]
        P.op("dve", lambda e: e.memset(q, 0.0), writes=[("QBD", s_ % 2)])
        for hh in range(2):
            pr = slice(64 * hh, 64 * hh + 64)
            P.op("dve", lambda e, pr=pr, hh=hh: e.tensor_copy(out=q[pr, :, 64 * hh:64 * hh + 64], in_=QS[pr, :, 64 * s_:64 * s_ + 64]),
                 reads=[("QS", j) for j in range(4)] + [("QBD", s_ % 2)], writes=[("QBD", s_ % 2)])

    def rng(it):
        if it["kind"] == "p":
            c0 = max(0, it["kb"] - 4 * it["Q"]) * 128
            return slice(0, 128), c0, 512
        if it["kb"] == 32:
            o = 64 * (it["s"] % 2)
            return slice(o, o + 64), 0, 512
        return slice(0, 128), 0, 512

    wgv0 = w_eg.rearrange("e (p c) f -> (e p) (c f)", c=8)
    wuv0 = w_eu.rearrange("e (p c) f -> (e p) (c f)", c=8)
    wdv0 = w_ed.rearrange("e (p c) d -> (e p) (c d)", c=2)
    cast_jobs = [(src, dst, g) for g in range(8) for (src, dst) in ((wgv0, WGB), (wuv0, WUB), (wdv0, WDB))]

    def st_load(it):
        if it["kind"] == "p" and it["i"] % 12 == 0 and cast_jobs:
            src, dst, g = cast_jobs.pop(0)
            P.dma("pool", lambda e: e.dma_start(out=dst[g * 512:(g + 1) * 512, :], in_=src[g * 512:(g + 1) * 512, :]), "WCAST", writes=["WCAST"])
        if it["kind"] != "s" or it["kb"] == 32:
            return
        s, kb = it["s"], it["kb"]
        sl3 = it["i"] % 5
        P.dma("pool", lambda e: e.dma_start(out=KC[sl3], in_=ck[s, kb * 128:(kb + 1) * 128, :]), ("KC", sl3), writes=[("KC", sl3)])
        sl8 = it["i"] % 10
        P.dma("pool", lambda e: e.dma_start(out=VC[sl8], in_=cv[s, kb * 128:(kb + 1) * 128, :]), ("VC", sl8), writes=[("VC", sl8)])

    def st_T(it):
        if it["kind"] != "s" or it["kb"] == 32:
            return
        sl3 = it["i"] % 5
        s2 = it["i"] % 2
        for hp in range(4):
            P.op("pe", lambda e, hp=hp: e.transpose(psb[6][:, hp * 128:(hp + 1) * 128], KC[sl3][:, hp * 128:(hp + 1) * 128], ident),
                 reads=[("KC", sl3), "ident"], writes=[PS(6)])
        P.op("dve", lambda e: e.tensor_copy(out=KCT[s2], in_=psb[6][:, 0:512].rearrange("p (h n) -> p h n", n=128)),
             reads=[PS(6)], writes=[("KCT", s2)])

    def st_A(it):
        pr, c0, ce = rng(it)
        zb = it["zb"]
        if it["kind"] == "p":
            hp, Q, hh, kb = it["hp"], it["Q"], it["hh"], it["kb"]
            hr = slice(64 * hh, 64 * hh + 64)
            P.op("pe", lambda e: e.matmul(psf[zb][:, c0:512], lhsT=KT[hr, hp, kb * 128:(kb + 1) * 128], rhs=QT[hr, hp, Q * 512 + c0:Q * 512 + 512],
                                          start=True, stop=True),
                 reads=[("KT", hp, kb // 4), ("QT", hp, Q)], writes=[PS(zb)])
        else:
            s, kb = it["s"], it["kb"]
            if kb == 32:
                build_qbd(s)
            for hp in range(4):
                if kb == 32:
                    P.op("pe", lambda e, hp=hp: e.matmul(psf[zb][pr, hp * 128:(hp + 1) * 128], lhsT=KS[:, hp, 64 * s:64 * s + 64], rhs=QBD[s % 2][:, hp, :],
                                                         start=True, stop=True),
                         reads=[("KS", hp), ("QBD", s % 2)], writes=[PS(zb)])
                else:
                    s2 = it["i"] % 2
                    P.op("pe", lambda e, hp=hp: e.matmul(psf[zb][:, hp * 128:(hp + 1) * 128], lhsT=KCT[s2][:, hp, :], rhs=QBD[s % 2][:, hp, :],
                                                         start=True, stop=True),
                         reads=[("KCT", s2), ("QBD", s % 2)], writes=[PS(zb)])

    def st_B(it):
        pr, c0, ce = rng(it)
        zb, sl = it["zb"], it["sl"]
        P.op("act", lambda e: e.activation(out=E[sl][pr, c0:512], in_=psf[zb][pr, c0:512], func=AF.Exp, scale=SCALE),
             reads=[PS(zb)], writes=[("E", sl)])
        if it["kind"] == "p" and it["kb"] >= 4 * it["Q"]:
            P.op("dve", lambda e: e.tensor_tensor(out=E[sl][:, c0:c0 + 128], in0=E[sl][:, c0:c0 + 128], in1=tri, op=ALU.mult),
                 reads=[("E", sl), "tri"], writes=[("E", sl)])
        if it["kind"] == "s" and it["kb"] == 32:
            P.op("dve", lambda e: e.tensor_tensor(out=E[sl][pr, :], in0=E[sl][pr, :], in1=tri8[pr].rearrange("p h c -> p (h c)"), op=ALU.mult),
                 reads=[("E", sl), "tri8"], writes=[("E", sl)])
        P.op("act", lambda e: e.activation(out=SPb[sl][pr, c0:512], in_=E[sl][pr, c0:512], func=AF.Ln, bias=1.0),
             reads=[("E", sl)], writes=[("SP", sl)])

    def st_C(it):
        pr, c0, ce = rng(it)
        sb, sl, sa, sn = it["sb"], it["sl"], it["sa_cur"], it["sa_nxt"]
        if it["first"]:
            P.op("dve", lambda e: e.memset(SACC[sa], 0.0), writes=[("SACC", sa)])
            P.op("dve", lambda e: e.memset(SACC[sn], 0.0), writes=[("SACC", sn)])
        P.op("pe", lambda e: e.matmul(psf[sb][pr, c0:512], lhsT=Uinc[pr, pr], rhs=SPb[sl][pr, c0:512], start=True, stop=it["first"]),
             reads=["Uinc", ("SP", sl)], writes=[PS(sb)])
        if not it["first"]:
            P.op("pe", lambda e: e.matmul(psf[sb][pr, c0:512], lhsT=ones[:, pr], rhs=SACC[sa][:, c0:512], start=False, stop=True),
                 reads=["ones", ("SACC", sa)], writes=[PS(sb)])
        if not it["last"]:
            P.op("dve", lambda e: e.tensor_tensor(out=SACC[sn][pr, c0:512], in0=SACC[sa][pr, c0:512], in1=SPb[sl][pr, c0:512], op=ALU.add),
                 reads=[("SACC", sa), ("SP", sl)], writes=[("SACC", sn)])

    def st_D(it):
        pr, c0, ce = rng(it)
        sb, sl, sa = it["sb"], it["sl"], it["sacc"]
        P.op("act", lambda e: e.activation(out=Xb[sl][pr, c0:512], in_=psf[sb][pr, c0:512], func=AF.Exp, scale=-1.0),
             reads=[PS(sb)], writes=[("X", sl), ("W", sl)])
        P.op("dve", lambda e: e.tensor_tensor(out=Wb[sl][pr, c0:512], in0=E[sl][pr, c0:512], in1=Xb[sl][pr, c0:512], op=ALU.mult),
             reads=[("E", sl), ("X", sl)], writes=[("W", sl)])

    def st_E(it):
        pr, c0, ce = rng(it)
        sl, ob = it["sl"], it["obank"]
        if it["kind"] == "p":
            hp, Q, hh, kb = it["hp"], it["Q"], it["hh"], it["kb"]
            h = 2 * hp + hh
            hr = slice(64 * hh, 64 * hh + 64)
            P.op("pe", lambda e: e.matmul(psf[ob][hr, c0:512], lhsT=V_ALL[:, kb, h * 64:(h + 1) * 64], rhs=Wb[sl][:, c0:512],
                                          start=it["first"], stop=it["last"], skip_group_check=True),
                 reads=[("V_ALL", kb), ("W", sl)], writes=[PS(ob)])
            if it["endgrp"]:
                P.op("act", lambda e: e.copy(out=MIXT[:, hp, Q * 512:(Q + 1) * 512], in_=psf[ob]), reads=[PS(ob)], writes=[("MIXT", hp, Q * 512)])
        else:
            s, kb = it["s"], it["kb"]
            sl3 = it["i"] % 5
            for h in range(8):
                hp, hh = h // 2, h % 2
                hr = slice(64 * hh, 64 * hh + 64)
                if kb == 32:
                    vt = V_ALL[pr, 16 + s // 2, h * 64:(h + 1) * 64]
                    vres = ("V_ALL", 16 + s // 2)
                else:
                    vt = VC[it["i"] % 10][:, h * 64:(h + 1) * 64]
                    vres = ("VC", it["i"] % 10)
                P.op("pe", lambda e, h=h, hp=hp, hr=hr, vt=vt: e.matmul(psf[ob][hr, hp * 64:(hp + 1) * 64], lhsT=vt, rhs=Wb[sl][pr, h * 64:(h + 1) * 64],
                                                                          start=(it["first"] and hp == 0), stop=it["last"], skip_group_check=True),
                     reads=[vres, ("W", sl)], writes=[PS(ob)])
            if it["endgrp"]:
                P.op("act", lambda e: e.copy(out=MIXT[:, 0:4, 2048 + 64 * s:2048 + 64 * s + 64],
                                             in_=psf[ob][:, 0:256].rearrange("p (h n) -> p h n", n=64)),
                     reads=[PS(ob)], writes=[("MIXT", "s", s)])

    n_it = len(items)
    NFILL = 1
    for step in range(n_it + 10):
        def g(k):
            j = step - k
            return items[j] if 0 <= j < n_it else None
        for k, fnst in ((0, st_load), (4, st_T), (5, st_A), (6, st_B), (8, st_C), (8, st_D), (9, st_E)):
            it = g(k)
            if it is not None:
                fnst(it)
            if k in (5, 8) and fnst is not st_D and NFILL and it is not None and it["kind"] == "p":
                P.op("pe", lambda e: e.matmul(psf[7], lhsT=ones, rhs=FSRC, start=True, stop=True), reads=["FSRC", "ones"])
    if stop_after <= 5:
        P.emit(); return nc

    P.barrier()
    aFREE.reset()
    RT = aFREE.t("RT", [128, 18, 88], F32)
    off_rt = aFREE.off
    LNG1 = aFREE.t("LNG1", [128, 1024], F32)
    LNB1 = aFREE.t("LNB1", [128, 1024], F32)
    XIN5 = [aFREE.t("XIN5_%d" % i, [128, 1024], F32) for i in range(2)]
    assert aFREE.off <= WOUT_OFF
    aFREE.off = WOUT_OFF + 16384
    XIN5.append(aFREE.t("XIN5_2", [128, 1024], F32))
    H5 = XIN5
    X1B = [aFREE.t("X1B_%d" % i, [128, 1024], BF16) for i in range(2)]
    for (tl, src) in ((LNG1, ln1_g), (LNB1, ln1_b)):
        P.dma("sp", lambda e, tl=tl, src=src: e.dma_start(out=tl, in_=src.partition_broadcast(128)), ("LNP", id(tl)), writes=[("LNP", id(tl))])
    LN1R = [("LNP", id(LNG1)), ("LNP", id(LNB1))]

    def mix_res(t):
        n0 = t * 128
        if t < 16:
            q = (n0 // 512) * 512
            return [("MIXT", c, q) for c in range(8)]
        r = []
        for s in (2 * (t - 16), 2 * (t - 16) + 1):
            r += [("MIXT", "s", s)] + [("MIXT", c, 2048 + 64 * s) for c in range(4, 8)]
        return r

    def router_mm(t):
        bank = 4 + (t % 2)
        for c in range(8):
            P.op("pe", lambda e, c=c: e.matmul(psf[bank][:, 0:36], lhsT=XT[:, c, tcols(t)], rhs=W_R[:, c, :], start=(c == 0), stop=(c == 7)),
                 reads=[("XT", t), "W_R"], writes=[PS(bank)])
        P.op("dve", lambda e: e.tensor_tensor(out=RT[:, t, 84:88], in0=psf[bank][:, 0:4], in1=RB[:, 0:4], op=ALU.add),
             reads=[PS(bank), "RB"], writes=["LG"])
        P.op("dve", lambda e: e.tensor_tensor(out=GATE[:, t, :], in0=psf[bank][:, 4:36], in1=RB[:, 4:36], op=ALU.add),
             reads=[PS(bank), "RB"], writes=["LG"])

    def p5_load(t):
        s = t % 3
        P.dma("sp", lambda e: e.dma_start(out=XIN5[s], in_=XNS[t * 128:(t + 1) * 128, :]), ("XIN5", s), reads=[("XNS", t)], writes=[("XIN5", s)])

    def p5_mm(t):
        s = t % 3
        s2 = t % 2
        for half in range(2):
            bank = 2 * s2 + half
            for c in range(8):
                P.op("pe", lambda e, c=c, half=half, bank=bank: e.matmul(psf[bank], lhsT=MIXT[:, c, tcols(t)], rhs=W_OUT[:, c, half * 512:(half + 1) * 512],
                                                                         start=(c == 0), stop=(c == 7)),
                     reads=mix_res(t) + ["W_OUT"], writes=[PS(bank)])
            P.op("dve", lambda e, half=half, bank=bank: e.scalar_tensor_tensor(
                out=H5[s][:, half * 512:(half + 1) * 512], in0=XIN5[s][:, half * 512:(half + 1) * 512], scalar=ALPHA, in1=psf[bank],
                op0=ALU.mult, op1=ALU.add), reads=[("XIN5", s), PS(bank)], writes=[("XIN5", s)])

    def p5_ln(t):
        s = t % 3
        s2 = t % 2
        layernorm("ln1", H5[s], ("XIN5", s), H5[s], ("XIN5", s), LNG1, LNB1, LN1R)
        P.op("act", lambda e: e.mul(out=R[:, t, :], in_=H5[s], mul=ALPHA), reads=[("XIN5", s)], writes=[("R", t)])
        P.op("act", lambda e: e.copy(out=X1B[s2], in_=H5[s]), reads=[("XIN5", s)], writes=[("X1B", s2)])

    def p5_T(t):
        s2 = t % 2
        transpose_to_XT(t, X1B[s2], ("X1B", s2), 6 + s2)

    p5_load(0)
    p5_load(1)
    for t in range(20):
        if t < 18:
            p5_mm(t)
        if 1 <= t < 19:
            p5_T(t - 1)
        if t < 18:
            p5_ln(t)
            if t + 2 < 18:
                p5_load(t + 2)
        if t >= 2:
            router_mm(t - 2)
    P.barrier()
    aF2 = Arena(aFREE.base + off_rt, aFREE.size - off_rt)
    X_ = mybir.AxisListType.X
    NS = 50
    TS = 256
    SELB = aF2.t("SELB", [128, 18, 32], BF16)
    RANK = aF2.t("RANK", [128, 18, 32], F32)
    TMP = aF2.t("TMP", [128, 18, 32], F32)
    CNT = aF2.t("CNT", [128, 32], F32)
    NTL = aF2.t("NTL", [128, 32], F32)
    SCA = aF2.t("SCA", [128, 32], F32)
    SCB = aF2.t("SCB", [128, 32], F32)
    CMP9 = aF2.t("CMP9", [128, 32, 9], F32)
    THR = aF2.t("THR", [128, 32, 9], F32)
    S1F = aF2.t("S1F", [128, 18], F32)
    S2F = aF2.t("S2F", [128, 18], F32)
    S1I = aF2.t("S1I", [128, 18], I32)
    S2I = aF2.t("S2I", [128, 18], I32)
    DV1 = aF2.t("DV1", [128, 18], I32)
    DV2 = aF2.t("DV2", [128, 18], I32)
    JCI = aF2.t("JCI", [128, NS, 32], I32)
    JC = aF2.t("JC", [128, NS, 32], F32)
    CMPJ = aF2.t("CMPJ", [128, NS, 32], F32)
    EJF = aF2.t("EJF", [128, NS], F32)
    PIDI = aF2.t("PIDI", [128, 1], I32)
    PIDF = aF2.t("PIDF", [128, 1], F32)
    XB = [aF2.t("XB%d" % i, [128, 1028], BF16) for i in range(8)]
    TRIB = aF2.t("TRIB", [128, 128], BF16)

    gm = RT[:, :, 0]
    ngs = RT[:, :, 1]
    gw = RT[:, :, 2]
    m1 = RT[:, :, 3]
    m2 = RT[:, :, 4]
    d21 = RT[:, :, 5]
    w1 = RT[:, :, 6]
    w2 = RT[:, :, 7]
    gmask = RT[:, :, 8:12]
    pen = RT[:, :, 12:16]
    ge = RT[:, :, 16:20]
    em = GATE[:, :, :]
    mk1 = RT[:, :, 20:52]
    mk2 = RT[:, :, 52:84]
    lg4 = RT[:, :, 84:88]

    def bc(a_, n):
        return a_.unsqueeze(2).to_broadcast([128, 18, n])

    def D(fn):
        P.op("dve", fn, reads=["RT", "LG"], writes=["RT"])
    D(lambda e: e.tensor_reduce(out=gm, in_=lg4, axis=X_, op=ALU.max))
    D(lambda e: e.tensor_tensor(out=gmask, in0=lg4, in1=bc(gm, 4), op=ALU.is_ge))
    D(lambda e: e.tensor_scalar(out=pen, in0=gmask, scalar1=-1.0, scalar2=BIG, op0=ALU.add, op1=ALU.mult))
    D(lambda e: e.tensor_tensor(out=ge, in0=lg4, in1=bc(gm, 4), op=ALU.subtract))
    P.op("act", lambda e: e.activation(out=ge, in_=ge, func=AF.Exp), reads=["RT"], writes=["RT"])
    D(lambda e: e.tensor_reduce(out=ngs, in_=ge, axis=X_, op=ALU.add))
    D(lambda e: e.reciprocal(out=gw, in_=ngs))
    D(lambda e: e.tensor_tensor(out=em.rearrange("p t (g x) -> p t g x", x=8), in0=em.rearrange("p t (g x) -> p t g x", x=8),
                                in1=pen.unsqueeze(3).to_broadcast([128, 18, 4, 8]), op=ALU.add))
    D(lambda e: e.tensor_reduce(out=m1, in_=em, axis=X_, op=ALU.max))
    D(lambda e: e.tensor_tensor(out=mk1, in0=em, in1=bc(m1, 32), op=ALU.is_ge))
    D(lambda e: e.scalar_tensor_tensor(out=em, in0=mk1, scalar=-BIG, in1=em, op0=ALU.mult, op1=ALU.add))
    D(lambda e: e.tensor_reduce(out=m2, in_=em, axis=X_, op=ALU.max))
    D(lambda e: e.tensor_tensor(out=mk2, in0=em, in1=bc(m2, 32), op=ALU.is_ge))
    D(lambda e: e.tensor_tensor(out=d21, in0=m2, in1=m1, op=ALU.subtract))
    P.op("act", lambda e: e.activation(out=d21, in_=d21, func=AF.Exp), reads=["RT"], writes=["RT"])
    D(lambda e: e.tensor_scalar(out=w1, in0=d21, scalar1=1.0, scalar2=None, op0=ALU.add))
    D(lambda e: e.reciprocal(out=w1, in_=w1))
    D(lambda e: e.tensor_tensor(out=w2, in0=d21, in1=w1, op=ALU.mult))
    D(lambda e: e.tensor_tensor(out=w1, in0=w1, in1=gw, op=ALU.mult))
    D(lambda e: e.tensor_tensor(out=w2, in0=w2, in1=gw, op=ALU.mult))
    D(lambda e: e.tensor_tensor(out=SELB, in0=mk1, in1=mk2, op=ALU.add))
    P.op("act", lambda e: e.copy(out=TRIB, in_=tri), reads=["tri"], writes=["TRIB"])
    for t in range(18):
        bank = 0 if t < 9 else 1
        cs = slice((t % 9) * 32, (t % 9) * 32 + 32)
        for t2 in range(t + 1):
            P.op("pe", lambda e, t2=t2, t=t, bank=bank, cs=cs: e.matmul(psf[bank][:, cs], lhsT=(TRIB if t2 == t else ones), rhs=SELB[:, t2, :],
                                                                      start=(t2 == 0), stop=(t2 == t)),
                 reads=["RT", "TRIB", "ones"], writes=[PS(bank)])
    for t2 in range(18):
        P.op("pe", lambda e, t2=t2: e.matmul(psf[2][:, 0:32], lhsT=ones, rhs=SELB[:, t2, :], start=(t2 == 0), stop=(t2 == 17)),
             reads=["RT", "ones"], writes=[PS(2)])
    P.op("dve", lambda e: e.tensor_copy(out=RANK[:, 0:9, :], in_=psf[0][:, 0:288].rearrange("p (t x) -> p t x", x=32)), reads=[PS(0)], writes=["RANK"])
    P.op("dve", lambda e: e.tensor_copy(out=RANK[:, 9:18, :], in_=psf[1][:, 0:288].rearrange("p (t x) -> p t x", x=32)), reads=[PS(1), "RANK"], writes=["RANK"])
    P.op("dve", lambda e: e.tensor_copy(out=CNT, in_=psf[2][:, 0:32]), reads=[PS(2)], writes=["CNT"])
    for k in range(9):
        P.op("pool", lambda e, k=k: e.memset(THR[:, :, k:k + 1], float(TS * k)), writes=["THR"])
    P.op("dve", lambda e: e.tensor_tensor(out=CMP9, in0=CNT.unsqueeze(2).to_broadcast([128, 32, 9]), in1=THR, op=ALU.is_gt),
         reads=["CNT", "THR"], writes=["CMP9"])
    P.op("dve", lambda e: e.tensor_reduce(out=NTL, in_=CMP9, axis=X_, op=ALU.add), reads=["CMP9"], writes=["NTL"])
    src_, dst_ = NTL, SCA
    for sh in (1, 2, 4, 8, 16):
        P.op("dve", lambda e, src_=src_, dst_=dst_, sh=sh: e.tensor_copy(out=dst_[:, 0:sh], in_=src_[:, 0:sh]), reads=["NTL", "SC"], writes=["SC"])
        P.op("dve", lambda e, src_=src_, dst_=dst_, sh=sh: e.tensor_tensor(out=dst_[:, sh:32], in0=src_[:, sh:32], in1=src_[:, 0:32 - sh], op=ALU.add),
             reads=["NTL", "SC"], writes=["SC"])
        src_, dst_ = dst_, (SCB if dst_ is SCA else SCA)
    INCL = src_
    EXCL = dst_
    P.op("dve", lambda e: e.tensor_tensor(out=EXCL, in0=INCL, in1=NTL, op=ALU.subtract), reads=["SC", "NTL"], writes=["SC"])
    P.op("dve", lambda e: e.tensor_scalar(out=EXCL, in0=EXCL, scalar1=float(TS), scalar2=None, op0=ALU.mult), reads=["SC"], writes=["SC"])
    P.op("dve", lambda e: e.tensor_tensor(out=RANK, in0=RANK, in1=EXCL.unsqueeze(1).to_broadcast([128, 18, 32]), op=ALU.add),
         reads=["RANK", "SC"], writes=["RANK"])
    for (mk_, SF_, SI_) in ((mk1, S1F, S1I), (mk2, S2F, S2I)):
        P.op("dve", lambda e, mk_=mk_: e.tensor_tensor(out=TMP, in0=RANK, in1=mk_, op=ALU.mult), reads=["RANK", "RT"], writes=["TMP"])
        P.op("dve", lambda e, SF_=SF_: e.tensor_reduce(out=SF_, in_=TMP, axis=X_, op=ALU.add), reads=["TMP"], writes=["SF"])
        P.op("dve", lambda e, SF_=SF_, SI_=SI_: e.tensor_copy(out=SI_, in_=SF_), reads=["SF"], writes=["SI"])
    P.op("pool", lambda e: e.iota(DV1, pattern=[[128, 18]], base=0, channel_multiplier=1), writes=["DV"])
    P.op("pool", lambda e: e.iota(DV2, pattern=[[128, 18]], base=NT, channel_multiplier=1), writes=["DV"])
    P.op("pool", lambda e: e.iota(JCI, pattern=[[1, NS], [0, 32]], base=0, channel_multiplier=0), writes=["JCI"])
    P.op("dve", lambda e: e.tensor_copy(out=JC, in_=JCI), reads=["JCI"], writes=["JC"])
    P.op("dve", lambda e: e.tensor_tensor(out=CMPJ, in0=INCL.unsqueeze(1).to_broadcast([128, NS, 32]), in1=JC, op=ALU.is_le),
         reads=["SC", "JC"], writes=["CMPJ"])
    P.op("dve", lambda e: e.tensor_reduce(out=EJF, in_=CMPJ, axis=X_, op=ALU.add), reads=["CMPJ"], writes=["EJF"])
    P.op("pool", lambda e: e.iota(PIDI, pattern=[[0, 1]], base=0, channel_multiplier=1), writes=["PIDI"])
    P.op("dve", lambda e: e.tensor_copy(out=PIDF, in_=PIDI), reads=["PIDI"], writes=["PIDF"])
    P.op("dve", lambda e: e.tensor_scalar(out=EJF, in0=EJF, scalar1=128.0, scalar2=None, op0=ALU.mult), reads=["EJF"], writes=["EJF"])
    P.op("dve", lambda e: e.tensor_scalar(out=EJF, in0=EJF, scalar1=PIDF[:, 0:1], scalar2=None, op0=ALU.add), reads=["EJF", "PIDF"], writes=["EJF"])
    P.op("dve", lambda e: e.tensor_copy(out=IDXW, in_=EJF), reads=["EJF"], writes=["IDXW"])
    for t in range(18):
        for k in range(2):
            xb = XB[(t % 4) * 2 + k]
            xr = ("XB", (t % 4) * 2 + k)
            DV_, SI_, wk = (DV1, S1I, w1) if k == 0 else (DV2, S2I, w2)
            P.op("act", lambda e, xb=xb, t=t: e.mul(out=xb[:, 0:1024], in_=R[:, t, :], mul=1.0 / ALPHA), reads=[("R", t)], writes=[xr])
            P.op("dve", lambda e, xb=xb, t=t, DV_=DV_: e.tensor_copy(out=xb[:, 1024:1026].bitcast(I32), in_=DV_[:, t:t + 1]),
                 reads=["DV", xr], writes=[xr])
            P.op("dve", lambda e, xb=xb, t=t, wk=wk: e.tensor_copy(out=xb[:, 1026:1028].bitcast(F32), in_=wk[:, t:t + 1]),
                 reads=["RT", xr], writes=[xr])
            P.dma("pool", lambda e, xb=xb, t=t, SI_=SI_: e.indirect_dma_start(
                out=XS, out_offset=bass.IndirectOffsetOnAxis(ap=SI_[:, t:t + 1], axis=0), in_=xb, in_offset=None), "XSsc", reads=[xr, "SI", "XSfill"], writes=[("XSs", t, k)])
    XS_ALL = [("XSs", t, k) for t in range(18) for k in range(2)]
    if stop_after <= 6:
        P.emit(); return nc

    P.barrier()
    aMIX.reset()
    NW = 3
    WG = [aMIX.t("WG%d" % i, [128, 8, 256], BF16) for i in range(NW)]
    WU = [aMIX.t("WU%d" % i, [128, 8, 256], BF16) for i in range(NW)]
    WD = [aMIX.t("WD%d" % i, [128, 2, 1024], BF16) for i in range(NW)]
    aFREE.reset()
    XSB = [aFREE.t("XSB%d" % i, [128, 1028], BF16) for i in range(6)]
    XST = [aFREE.t("XST%d" % i, [128, 8, 256], BF16) for i in range(2)]
    SLs = [aFREE.t("SLs%d" % i, [128, 512], BF16) for i in range(2)]
    HIDT = [aFREE.t("HIDT%d" % i, [128, 512], BF16) for i in range(2)]
    YSB = [aFREE.t("YSB%d" % i, [128, 1024], F32) for i in range(4)]
    LNG2 = aFREE.t("LNG2", [128, 1024], F32)
    LNB2 = aFREE.t("LNB2", [128, 1024], F32)
    P.dma("sp", lambda e: e.dma_start(out=LNG2, in_=ln2_g.partition_broadcast(128)), "LNG2", writes=["LNG2"])
    P.dma("sp", lambda e: e.dma_start(out=LNB2, in_=ln2_b.partition_broadcast(128)), "LNB2", writes=["LNB2"])
    assert not cast_jobs
    wgv, wuv, wdv = WGB, WUB, WDB

    regs = {}

    def bc_reg(e):
        if "bc" not in regs:
            regs["bc"] = e.alloc_register("bcreg")
            e.reg_mov(regs["bc"], 4095)
        return regs["bc"]

    def bc_reg2(e):
        if "bc2" not in regs:
            regs["bc2"] = e.alloc_register("bcreg2")
            e.reg_mov(regs["bc2"], 2 * NT - 1)
        return regs["bc2"]

    def sp_L(j):
        w = j % NW
        for h in range(2):
            xi = (j % 3) * 2 + h
            P.dma("sp", lambda e, xi=xi, h=h: e.dma_start(out=XSB[xi], in_=XS[(2 * j + h) * 128:(2 * j + h + 1) * 128, :]), ("XSB", xi),
                  reads=XS_ALL, writes=[("XSB", xi)])
        for (Wt, wres, wv) in ((WG[w], ("WG", w), wgv), (WU[w], ("WU", w), wuv), (WD[w], ("WD", w), wdv)):
            P.dma("pool", lambda e, Wt=Wt, wv=wv: e.indirect_dma_start(
                out=(Wt.rearrange("p c f -> p (c f)")), out_offset=None, in_=wv,
                in_offset=bass.IndirectOffsetOnAxis(ap=IDXW[:, j:j + 1], axis=0), bounds_check=bc_reg(e), oob_is_err=False),
                wres, reads=["IDXW", "WCAST"], writes=[wres])

    def sp_T(j):
        x = j % 2
        for h in range(2):
            xi = (j % 3) * 2 + h
            bank = 6 + h
            for c in range(8):
                P.op("pe", lambda e, c=c, xi=xi, bank=bank: e.transpose(psb[bank][:, c * 128:(c + 1) * 128], XSB[xi][:, c:1024:8], ident),
                     reads=[("XSB", xi), "ident"], writes=[PS(bank)])
            if h == 0:
                P.op("act", lambda e, bank=bank, h=h: e.copy(out=XST[x][:, :, h * 128:(h + 1) * 128], in_=psb[bank].rearrange("p (c n) -> p c n", n=128)),
                     reads=[PS(bank)], writes=[("XST", x, h)])
            else:
                P.op("dve", lambda e, bank=bank, h=h: e.tensor_copy(out=XST[x][:, :, h * 128:(h + 1) * 128], in_=psb[bank].rearrange("p (c n) -> p c n", n=128)),
                     reads=[PS(bank)], writes=[("XST", x, h)])

    def sp_U(j):
        w = j % NW
        x = j % 2
        bA, bU = 0, 1
        for (Wt, wres, bank) in ((WG[w], ("WG", w), bA), (WU[w], ("WU", w), bU)):
            for fc in range(2):
                for c in range(8):
                    P.op("pe", lambda e, Wt=Wt, fc=fc, c=c, bank=bank: e.matmul(psf[bank][:, fc * 256:(fc + 1) * 256], lhsT=Wt[:, c, fc:256:2], rhs=XST[x][:, c, :],
                                                                                 start=(c == 0), stop=(c == 7)),
                         reads=[wres, ("XST", x, 0), ("XST", x, 1)], writes=[PS(bank)])
        P.op("act", lambda e: e.activation(out=SLs[x], in_=psf[bA], func=AF.Silu), reads=[PS(bA)], writes=[("SLs", x)])
        P.op("dve", lambda e: e.tensor_tensor(out=HIDT[x], in0=psf[bU], in1=SLs[x], op=ALU.mult), reads=[PS(bU), ("SLs", x)], writes=[("HIDT", x)])

    def sp_D(j):
        w = j % NW
        x = j % 2
        for h in range(2):
            xi = (j % 3) * 2 + h
            yi = x * 2 + h
            gate = XSB[xi][:, 1026:1028].bitcast(F32)
            for half in range(2):
                bank = 2 + 2 * h + half
                for fc in range(2):
                    P.op("pe", lambda e, fc=fc, h=h, half=half, bank=bank: e.matmul(
                        psf[bank], lhsT=HIDT[x][:, fc * 256 + h * 128:fc * 256 + (h + 1) * 128], rhs=WD[w][:, fc, half * 512:(half + 1) * 512],
                        start=(fc == 0), stop=(fc == 1)), reads=[("HIDT", x), ("WD", w)], writes=[PS(bank)])
                if half == 0:
                    P.op("act", lambda e, yi=yi, bank=bank, gate=gate: e.activation(out=YSB[yi][:, 0:512], in_=psf[bank], func=AF.Copy, scale=gate),
                         reads=[PS(bank), ("XSB", xi)], writes=[("YSB", yi)])
                else:
                    P.op("dve", lambda e, yi=yi, bank=bank, gate=gate: e.tensor_scalar(out=YSB[yi][:, 512:1024], in0=psf[bank], scalar1=gate, scalar2=None, op0=ALU.mult),
                         reads=[PS(bank), ("XSB", xi)], writes=[("YSB", yi)])
            P.dma("pool", lambda e, xi=xi, yi=yi: e.indirect_dma_start(
                out=YK, out_offset=bass.IndirectOffsetOnAxis(ap=XSB[xi][:, 1024:1026].bitcast(I32), axis=0), in_=YSB[yi], in_offset=None,
                bounds_check=bc_reg2(e), oob_is_err=False), ("st", "YSB", yi), reads=[("YSB", yi), ("XSB", xi)], writes=[("YKs", j, h)])

    sp_L(0)
    sp_L(1)
    for st_ in range(NS + 1):
        if st_ < NS:
            sp_T(st_)
        if st_ >= 1:
            sp_D(st_ - 1)
        if st_ < NS:
            sp_U(st_)
        if st_ + 2 < NS:
            sp_L(st_ + 2)
    YK_ALL = [("YKs", j, h) for j in range(NS) for h in range(2)]

    P.barrier()
    YO = [XST[0].rearrange("p c n -> p (c n)").bitcast(F32), XST[1].rearrange("p c n -> p (c n)").bitcast(F32)]
    def racc(t, k):
        P.dma("pool", lambda e: e.dma_start(out=R[:, t, :], in_=YK[k * NT + t * 128:k * NT + (t + 1) * 128, :], accum_op=ALU.add), ("RACC", t),
              reads=YK_ALL + [("R", t)], writes=[("R", t)])

    racc(0, 0)
    for t in range(18):
        if t + 1 < 18:
            racc(t + 1, 0)
        racc(t, 1)
    for t in range(18):
        s = t % 2
        layernorm("ln2", R[:, t, :], ("R", t), YO[s], ("YO", s), LNG2, LNB2, ["LNG2", "LNB2"])
        dst = yp[t * 128:(t + 1) * 128, :] if t < 16 else ys[(t - 16) * 128:(t - 15) * 128, :]
        P.dma("sp", lambda e, s=s, dst=dst: e.dma_start(out=dst, in_=YO[s]), ("st", "YO", s), reads=[("YO", s)])

    P.emit()
    return nc


_CACHE = {}
STOP = 99


def kernel(**inp):
    f = lambda a: np.ascontiguousarray(np.asarray(a, dtype=np.float32))
    if "nc" not in _CACHE:
        nc = bass.Bass("TRN2", target_bir_lowering=False)
        build(nc, STOP)
        _CACHE["nc"] = nc
    nc = _CACHE["nc"]
    rep = {}
    for k in ["ln0_g", "ln0_b"]:
        rep[k] = f(inp[k])
    for k in ["w_in", "w_dw", "b_dw", "lnc_g", "lnc_b", "w_cpw", "w_mk", "w_mv", "w_out", "ln1_g", "ln1_b", "w_rg", "b_rg",
              "w_re", "b_re", "w_eg", "w_eu", "w_ed", "ln2_g", "ln2_b"]:
        rep[k] = f(inp[k][0])
    x_prompt = f(inp["x_prompt"]); x_sample = f(inp["x_sample"]); mem_prompt = f(inp["mem_prompt"])
    cK = inp["cache_sba_k"]; cV = inp["cache_sba_v"]
    in_maps = []
    for c in range(NCORES):
        m = dict(rep)
        m["xp"] = x_prompt[c]
        m["xs"] = x_sample[4 * c:4 * c + 4].reshape(256, 1024)
        m["memp"] = mem_prompt[c]
        m["ck"] = f(cK[0, 4 * c:4 * c + 4]).reshape(4, 4096, 512)
        m["cv"] = f(cV[0, 4 * c:4 * c + 4]).reshape(4, 4096, 512)
        m["cconv"] = f(inp["cache_conv"][0, 4 * c:4 * c + 4])
        m["cmk"] = f(inp["cache_mem_k"][0, 4 * c:4 * c + 4]).reshape(4, 256, 256)
        m["cmv"] = f(inp["cache_mem_v"][0, 4 * c:4 * c + 4]).reshape(4, 256, 256)
        in_maps.append(m)
    res = run_bass_kernel_spmd(nc, in_maps, core_ids=list(range(NCORES)))
    rs = res.results
    cat = lambda k: np.stack([np.asarray(r[k], dtype=np.float32) for r in rs], 0)
    y_prompt = cat("yp")
    y_sample = cat("ys").reshape(32, 64, 1024)
    kpo = cat("kp").reshape(1, 8, 2048, 8, 64)
    vpo = cat("vp").reshape(1, 8, 2048, 8, 64)
    cpo = cat("convp").reshape(1, 8, 30, 256)
    mko = cat("mkp").reshape(1, 8, 256, 4, 64)
    mvo = cat("mvp").reshape(1, 8, 256, 4, 64)
    kso = cat("ksn").reshape(1, 32, 64, 8, 64)
    vso = cat("vsn").reshape(1, 32, 64, 8, 64)
    cso = cat("convs").reshape(1, 32, 30, 256)
    return (y_prompt, y_sample, kpo, vpo, cpo, mko, mvo, kso, vso, cso)
```
